# Optimizing a Trainium2 kernel written in Bass

```python
import math
import jax
import jax.numpy as jnp
from jax import lax
import numpy as np

D_MODEL = 1024
BATCH = 8
SEQ = 4096
DEPTH = 2

CTX_LEN = 256
GRID_W = 64
N_MOD = 6
EPS = 1e-6
NEG_INF = -1e30

HEAD_DIM = 64
ATTN_WIDTH = D_MODEL // 2
N_HEADS = ATTN_WIDTH // HEAD_DIM
N_KV_HEADS = N_HEADS // 4
KV_WIDTH = N_KV_HEADS * HEAD_DIM
WINDOW = 128
ATT_BLOCK = 128
ROPE_BASE = 10000.0

SSM_INNER = D_MODEL - ATTN_WIDTH
SSM_HEAD_DIM = 64
SSM_HEADS = SSM_INNER // SSM_HEAD_DIM
SSM_GROUPS = 2
SSM_STATE = 64
CONV_W = 5
CONV_CH = SSM_INNER + 2 * SSM_GROUPS * SSM_STATE
CHUNK = 128
N_DIRS = 2

MIX_WIDTH = ATTN_WIDTH + SSM_INNER
IN_WIDTH = ATTN_WIDTH + 2 * KV_WIDTH + SSM_INNER + CONV_CH + N_DIRS * SSM_HEADS

N_EXPERTS = 32
TOP_K = 4
D_FF = D_MODEL
SWIGLU_LIMIT = 7.0
SWIGLU_ALPHA = 1.702
MOE_BLOCK = 128

kernel_name = 'hybrid_attn_ssd_moe_diffusion_block'


def rms_norm(x, w):
    xf = x.astype(jnp.float32)
    y = xf * lax.rsqrt(jnp.mean(xf * xf, axis=-1, keepdims=True) + EPS)
    return (y * w.astype(jnp.float32)).astype(x.dtype)


def modulate(h, shift, scale):
    return h * (1.0 + scale) + shift


def axial_rope_tables(row_pos, col_pos):
    n_freq = HEAD_DIM // 4
    inv_freq = ROPE_BASE ** (-jnp.arange(n_freq, dtype=jnp.float32) / n_freq)
    ang = jnp.stack([row_pos.astype(jnp.float32)[:, None] * inv_freq,
                     col_pos.astype(jnp.float32)[:, None] * inv_freq], axis=1)
    return jnp.cos(ang)[None, :, None], jnp.sin(ang)[None, :, None]


def apply_rope(t, cos, sin):
    tr = t.astype(jnp.float32).reshape(*t.shape[:-1], 2, 2, HEAD_DIM // 4)
    t1, t2 = tr[..., 0, :], tr[..., 1, :]
    out = jnp.stack([t1 * cos - t2 * sin, t2 * cos + t1 * sin], axis=-2)
    return out.reshape(t.shape).astype(t.dtype)


def split_in_proj(p):
    b, L = p.shape[0], p.shape[1]
    s1 = ATTN_WIDTH
    s2 = s1 + KV_WIDTH
    s3 = s2 + KV_WIDTH
    s4 = s3 + SSM_INNER
    s5 = s4 + CONV_CH
    q, k, v, z, xbc, dt = jnp.split(p, [s1, s2, s3, s4, s5], axis=-1)
    return (q.reshape(b, L, N_HEADS, HEAD_DIM), k.reshape(b, L, N_KV_HEADS, HEAD_DIM),
            v.reshape(b, L, N_KV_HEADS, HEAD_DIM), z, xbc, dt.reshape(b, L, N_DIRS, SSM_HEADS))


def latent_window_attention(q, k, v, k_c, v_c, sinks):
    b, S = q.shape[0], q.shape[1]
    nb = S // ATT_BLOCK
    rep = N_HEADS // N_KV_HEADS
    scale = HEAD_DIM ** -0.5
    qb = q.reshape(b, nb, ATT_BLOCK, N_KV_HEADS, rep, HEAD_DIM)

    def band(t):
        tp = jnp.pad(t, ((0, 0), (ATT_BLOCK, ATT_BLOCK), (0, 0), (0, 0)))
        tp = tp.reshape(b, nb + 2, ATT_BLOCK, N_KV_HEADS, HEAD_DIM)
        return jnp.concatenate([tp[:, :-2], tp[:, 1:-1], tp[:, 2:]], axis=2)

    kb, vb = band(k), band(v)
    s_win = jnp.einsum('bnqgrd,bnkgd->bngrqk', qb, kb).astype(jnp.float32) * scale
    qpos = jnp.arange(nb)[:, None] * ATT_BLOCK + jnp.arange(ATT_BLOCK)[None, :]
    kpos = jnp.arange(nb)[:, None] * ATT_BLOCK - ATT_BLOCK + jnp.arange(3 * ATT_BLOCK)[None, :]
    valid = ((jnp.abs(qpos[:, :, None] - kpos[:, None, :]) <= WINDOW)
             & (kpos[:, None, :] >= 0) & (kpos[:, None, :] < S))
    s_win = jnp.where(valid[None, :, None, None], s_win, NEG_INF)
    s_ctx = jnp.einsum('bnqgrd,bcgd->bngrqc', qb, k_c).astype(jnp.float32) * scale
    s_sink = jnp.broadcast_to(sinks.astype(jnp.float32).reshape(N_KV_HEADS, rep, 1, 1),
                              s_ctx.shape[:-1] + (1,))
    probs = jax.nn.softmax(jnp.concatenate([s_ctx, s_win, s_sink], axis=-1), axis=-1).astype(v.dtype)
    n_ctx = k_c.shape[1]
    out = (jnp.einsum('bngrqc,bcgd->bnqgrd', probs[..., :n_ctx], v_c)
           + jnp.einsum('bngrqk,bnkgd->bnqgrd', probs[..., n_ctx:n_ctx + 3 * ATT_BLOCK], vb))
    return out.reshape(b, S, ATTN_WIDTH)


def context_attention(q_c, k_c, v_c, sinks):
    b, n_ctx = q_c.shape[0], q_c.shape[1]
    rep = N_HEADS // N_KV_HEADS
    qcb = q_c.reshape(b, n_ctx, N_KV_HEADS, rep, HEAD_DIM)
    s = jnp.einsum('bqgrd,bkgd->bgrqk', qcb, k_c).astype(jnp.float32) * HEAD_DIM ** -0.5
    s_sink = jnp.broadcast_to(sinks.astype(jnp.float32).reshape(N_KV_HEADS, rep, 1, 1), s.shape[:-1] + (1,))
    probs = jax.nn.softmax(jnp.concatenate([s, s_sink], axis=-1), axis=-1).astype(v_c.dtype)
    out = jnp.einsum('bgrqk,bkgd->bqgrd', probs[..., :n_ctx], v_c)
    return out.reshape(b, n_ctx, ATTN_WIDTH)


def centred_dwconv(u, w, bias):
    out = lax.conv_general_dilated(
        u, w[:, None, :].astype(u.dtype), window_strides=(1,),
        padding=[(CONV_W // 2, CONV_W // 2)], dimension_numbers=('NWC', 'WIO', 'NWC'),
        feature_group_count=u.shape[-1])
    return out + bias.astype(u.dtype)


def conv_split(xbc, conv_w, conv_b):
    u = jax.nn.silu(centred_dwconv(xbc, conv_w, conv_b)).astype(jnp.float32)
    b, L = u.shape[0], u.shape[1]
    gs = SSM_GROUPS * SSM_STATE
    xs = u[..., :SSM_INNER].reshape(b, L, SSM_HEADS, SSM_HEAD_DIM)
    bm = u[..., SSM_INNER:SSM_INNER + gs].reshape(b, L, SSM_GROUPS, SSM_STATE)
    cm = u[..., SSM_INNER + gs:].reshape(b, L, SSM_GROUPS, SSM_STATE)
    return xs, bm, cm


def ssd_chunked(x, dt, a, bm, cm, h0):
    b, L, H, P = x.shape
    nc = L // CHUNK
    rep = H // SSM_GROUPS
    xc = x.reshape(b, nc, CHUNK, H, P)
    dtc = dt.reshape(b, nc, CHUNK, H)
    bh = jnp.repeat(bm, rep, axis=2).reshape(b, nc, CHUNK, H, SSM_STATE)
    ch = jnp.repeat(cm, rep, axis=2).reshape(b, nc, CHUNK, H, SSM_STATE)
    cs = jnp.cumsum(dtc * a, axis=2)
    lower = jnp.tril(jnp.ones((CHUNK, CHUNK), dtype=bool))[None, None, :, :, None]
    seg = cs[:, :, :, None, :] - cs[:, :, None, :, :]
    decay = jnp.where(lower, jnp.exp(jnp.where(lower, seg, 0.0)), 0.0)
    scores = jnp.einsum('bcihn,bcjhn->bcijh', ch, bh) * decay * dtc[:, :, None, :, :]
    y_diag = jnp.einsum('bcijh,bcjhp->bcihp', scores, xc)
    to_end = jnp.exp(cs[:, :, -1:, :] - cs) * dtc
    states = jnp.einsum('bcjhn,bcjh,bcjhp->bchpn', bh, to_end, xc)
    chunk_decay = jnp.exp(cs[:, :, -1, :])

    def step(h, inp):
        s, dcy = inp
        return dcy[:, :, None, None] * h + s, h

    h_final, h_prev = lax.scan(step, h0, (jnp.moveaxis(states, 1, 0), jnp.moveaxis(chunk_decay, 1, 0)))
    h_prev = jnp.moveaxis(h_prev, 0, 1)
    y_off = jnp.einsum('bcihn,bchpn,bcih->bcihp', ch, h_prev, jnp.exp(cs))
    return (y_diag + y_off).reshape(b, L, H, P), h_final


def gated_rms_norm(y, z, w):
    b, L = y.shape[0], y.shape[1]
    g = y.reshape(b, L, SSM_INNER) * jax.nn.silu(z.astype(jnp.float32))
    g = g.reshape(b, L, SSM_GROUPS, SSM_INNER // SSM_GROUPS)
    g = g * lax.rsqrt(jnp.mean(g * g, axis=-1, keepdims=True) + EPS)
    return (g.reshape(b, L, SSM_INNER) * w.astype(jnp.float32)).astype(z.dtype)


def bidirectional_ssd(xbc, xbc_c, z, z_c, dt_raw, dt_raw_c, conv_w, conv_b, dt_bias, a_log, d_skip, norm_w):
    xs, bm, cm = conv_split(xbc, conv_w, conv_b)
    xs_c, bm_c, cm_c = conv_split(xbc_c, conv_w, conv_b)
    skip = d_skip.astype(jnp.float32)[:, None]
    y = skip * xs
    y_c = skip * xs_c
    h0 = jnp.zeros((xs.shape[0], SSM_HEADS, SSM_HEAD_DIM, SSM_STATE), jnp.float32)
    for d in range(N_DIRS):
        if d == 0:
            orient = lambda t: t
        else:
            orient = lambda t: jnp.flip(t, axis=1)
        a = -jnp.exp(a_log[d].astype(jnp.float32))
        dt = jax.nn.softplus(dt_raw[:, :, d].astype(jnp.float32) + dt_bias[d].astype(jnp.float32))
        dt_c = jax.nn.softplus(dt_raw_c[:, :, d].astype(jnp.float32) + dt_bias[d].astype(jnp.float32))
        yc_d, h_ctx = ssd_chunked(orient(xs_c), orient(dt_c), a, orient(bm_c), orient(cm_c), h0)
        y_d, _ = ssd_chunked(orient(xs), orient(dt), a, orient(bm), orient(cm), h_ctx)
        y = y + orient(y_d)
        y_c = y_c + orient(yc_d)
    return gated_rms_norm(y, z, norm_w), gated_rms_norm(y_c, z_c, norm_w)


def hybrid_mixer(h, hc, cos, sin, w_in, conv_w, conv_b, dt_bias, a_log, d_skip,
                 ssm_norm_w, attn_sinks, attn_norm_w, w_out, with_ctx_out):
    q, k, v, z, xbc, dt_raw = split_in_proj(h @ w_in)
    q_c, k_c, v_c, z_c, xbc_c, dt_raw_c = split_in_proj(hc @ w_in)
    q = apply_rope(q, cos, sin)
    k = apply_rope(k, cos, sin)
    attn = rms_norm(latent_window_attention(q, k, v, k_c, v_c, attn_sinks), attn_norm_w)
    ssm, ssm_c = bidirectional_ssd(xbc, xbc_c, z, z_c, dt_raw, dt_raw_c, conv_w, conv_b,
                                   dt_bias, a_log, d_skip, ssm_norm_w)
    out = jnp.concatenate([attn, ssm], axis=-1) @ w_out
    if not with_ctx_out:
        return out, None
    attn_c = rms_norm(context_attention(q_c, k_c, v_c, attn_sinks), attn_norm_w)
    out_c = jnp.concatenate([attn_c, ssm_c], axis=-1) @ w_out
    return out, out_c


def clamped_swiglu(gu):
    glu, lin = jnp.split(gu, 2, axis=-1)
    glu = jnp.minimum(glu, SWIGLU_LIMIT)
    lin = jnp.clip(lin, -SWIGLU_LIMIT, SWIGLU_LIMIT)
    return glu * jax.nn.sigmoid(SWIGLU_ALPHA * glu) * (lin + 1.0)


def moe_ffn(h, w_router, b_router, w_gate_up, b_gate_up, w_down, b_down):
    n, d = h.shape
    nk = n * TOP_K
    logits = (h @ w_router + b_router).astype(jnp.float32)
    top_logit, top_idx = lax.top_k(logits, TOP_K)
    gates = jax.nn.softmax(top_logit, axis=-1).astype(h.dtype)
    flat_e = top_idx.reshape(nk).astype(jnp.int32)
    flat_tok = jnp.arange(nk, dtype=jnp.int32) // TOP_K
    order = jnp.argsort(flat_e)
    s_e, s_tok, s_gate = flat_e[order], flat_tok[order], gates.reshape(nk)[order]
    counts = jnp.bincount(flat_e, length=N_EXPERTS).astype(jnp.int32)
    start = jnp.cumsum(counts) - counts
    padded = (counts + MOE_BLOCK - 1) // MOE_BLOCK * MOE_BLOCK
    pad_end = jnp.cumsum(padded)
    pad_start = pad_end - padded
    dest = pad_start[s_e] + jnp.arange(nk, dtype=jnp.int32) - start[s_e]
    n_blocks = -(-nk // MOE_BLOCK) + N_EXPERTS
    buf = jnp.zeros((n_blocks * MOE_BLOCK, d), h.dtype).at[dest].set(h[s_tok])
    block_e = jnp.minimum(
        jnp.searchsorted(pad_end, jnp.arange(n_blocks, dtype=jnp.int32) * MOE_BLOCK, side='right'),
        N_EXPERTS - 1)

    def expert_block(args):
        xb, e = args
        return clamped_swiglu(xb @ w_gate_up[e] + b_gate_up[e]) @ w_down[e] + b_down[e]

    out_buf = lax.map(expert_block, (buf.reshape(n_blocks, MOE_BLOCK, d), block_e))
    contrib = out_buf.reshape(n_blocks * MOE_BLOCK, d)[dest] * s_gate[:, None]
    return jax.ops.segment_sum(contrib, s_tok, num_segments=n)


def setup_inputs(seed: int = 0) -> dict:
    key = jax.random.key(seed)
    ks = jax.random.split(key, 26)

    def nrm(k, shape, s):
        return jax.random.normal(k, shape, jnp.float32) * s

    dt0 = jnp.exp(jax.random.uniform(ks[11], (DEPTH, N_DIRS, SSM_HEADS), jnp.float32,
                                     math.log(1e-3), math.log(1e-1)))
    return {
        'x': nrm(ks[0], (BATCH, SEQ, D_MODEL), 1.0),
        'c': nrm(ks[1], (BATCH, D_MODEL), 1.0),
        'ctx': nrm(ks[2], (BATCH, CTX_LEN, D_MODEL), 1.0),
        'c_ctx': nrm(ks[3], (D_MODEL,), 1.0),
        'w_ada': nrm(ks[4], (DEPTH, D_MODEL, N_MOD * D_MODEL), 0.5 * D_MODEL ** -0.5),
        'b_ada': nrm(ks[5], (DEPTH, N_MOD * D_MODEL), 0.02),
        'norm_mix_w': 1.0 + nrm(ks[6], (DEPTH, D_MODEL), 0.1),
        'norm_ffn_w': 1.0 + nrm(ks[7], (DEPTH, D_MODEL), 0.1),
        'w_in': nrm(ks[8], (DEPTH, D_MODEL, IN_WIDTH), D_MODEL ** -0.5),
        'conv_w': nrm(ks[9], (DEPTH, CONV_W, CONV_CH), CONV_W ** -0.5),
        'conv_b': nrm(ks[10], (DEPTH, CONV_CH), 0.02),
        'dt_bias': dt0 + jnp.log(-jnp.expm1(-dt0)),
        'a_log': jnp.log(jax.random.uniform(ks[12], (DEPTH, N_DIRS, SSM_HEADS), jnp.float32, 1.0, 16.0)),
        'd_skip': 1.0 + nrm(ks[13], (DEPTH, SSM_HEADS), 0.1),
        'ssm_norm_w': 1.0 + nrm(ks[14], (DEPTH, SSM_INNER), 0.1),
        'attn_sinks': nrm(ks[15], (DEPTH, N_HEADS), 0.5),
        'attn_norm_w': 1.0 + nrm(ks[16], (DEPTH, ATTN_WIDTH), 0.1),
        'w_out': nrm(ks[17], (DEPTH, MIX_WIDTH, D_MODEL), MIX_WIDTH ** -0.5),
        'w_router': nrm(ks[18], (DEPTH, D_MODEL, N_EXPERTS), D_MODEL ** -0.5),
        'b_router': nrm(ks[19], (DEPTH, N_EXPERTS), 0.01),
        'w_gate_up': nrm(ks[20], (DEPTH, N_EXPERTS, D_MODEL, 2 * D_FF), D_MODEL ** -0.5),
        'b_gate_up': nrm(ks[21], (DEPTH, N_EXPERTS, 2 * D_FF), 0.01),
        'w_down': nrm(ks[22], (DEPTH, N_EXPERTS, D_FF, D_MODEL), D_FF ** -0.5),
        'b_down': nrm(ks[23], (DEPTH, N_EXPERTS, D_MODEL), 0.01),
        'final_norm_w': 1.0 + nrm(ks[24], (D_MODEL,), 0.1),
    }


def reference(x, c, ctx, c_ctx, w_ada, b_ada, norm_mix_w, norm_ffn_w, w_in, conv_w, conv_b,
              dt_bias, a_log, d_skip, ssm_norm_w, attn_sinks, attn_norm_w, w_out,
              w_router, b_router, w_gate_up, b_gate_up, w_down, b_down, final_norm_w):
    b, S, D = x.shape
    n_ctx = ctx.shape[1]
    rows = S // GRID_W
    row_pos = jnp.repeat(jnp.arange(rows, dtype=jnp.int32), GRID_W)
    col_pos = jnp.tile(jnp.arange(GRID_W, dtype=jnp.int32), rows)
    cos, sin = axial_rope_tables(row_pos, col_pos)
    xc = ctx
    for l in range(DEPTH):
        last = l == DEPTH - 1
        mod = jax.nn.silu(c) @ w_ada[l] + b_ada[l]
        mod_c = jax.nn.silu(c_ctx) @ w_ada[l] + b_ada[l]
        sh1, sc1, g1, sh2, sc2, g2 = jnp.split(mod[:, None, :], N_MOD, axis=-1)
        sh1c, sc1c, g1c, sh2c, sc2c, g2c = jnp.split(mod_c, N_MOD, axis=-1)
        h = modulate(rms_norm(x, norm_mix_w[l]), sh1, sc1)
        hc = modulate(rms_norm(xc, norm_mix_w[l]), sh1c, sc1c)
        mix, mix_c = hybrid_mixer(h, hc, cos, sin, w_in[l], conv_w[l], conv_b[l], dt_bias[l], a_log[l],
                                  d_skip[l], ssm_norm_w[l], attn_sinks[l], attn_norm_w[l], w_out[l],
                                  not last)
        x = x + g1 * mix
        if not last:
            xc = xc + g1c * mix_c
            h2 = modulate(rms_norm(x, norm_ffn_w[l]), sh2, sc2)
            h2c = modulate(rms_norm(xc, norm_ffn_w[l]), sh2c, sc2c)
            tokens = jnp.concatenate([h2.reshape(b * S, D), h2c.reshape(b * n_ctx, D)], axis=0)
            f = moe_ffn(tokens, w_router[l], b_router[l], w_gate_up[l], b_gate_up[l], w_down[l], b_down[l])
            x = x + g2 * f[:b * S].reshape(b, S, D)
            xc = xc + g2c * f[b * S:].reshape(b, n_ctx, D)
        else:
            h2 = modulate(rms_norm(x, norm_ffn_w[l]), sh2, sc2)
            f = moe_ffn(h2.reshape(b * S, D), w_router[l], b_router[l], w_gate_up[l], b_gate_up[l],
                        w_down[l], b_down[l])
            x = x + g2 * f.reshape(b, S, D)
    return rms_norm(x, final_norm_w)
```

```python
import numpy as np
import concourse.bass as bass
import concourse.mybir as mybir

F32 = mybir.dt.float32
BF16 = mybir.dt.bfloat16
I32 = mybir.dt.int32
AF = mybir.ActivationFunctionType
ALU = mybir.AluOpType
AX = mybir.AxisListType

ENGS = ("pe", "act", "dve", "pool", "sp")
NRING = 12


def _is_psum(ap):
    return type(ap.tensor).__name__ == "PSumTensorHandle"


def _region(ap):
    shape = list(ap.tensor.shape)
    if _is_psum(ap):
        return tuple((0, int(d) - 1) for d in shape)
    lo = int(ap.offset)
    hi = lo
    for step, cnt in ap.ap:
        step = int(step); cnt = int(cnt)
        if step >= 0:
            hi += step * (cnt - 1)
        else:
            lo += step * (cnt - 1)
    strides = [1] * len(shape)
    for i in range(len(shape) - 2, -1, -1):
        strides[i] = strides[i + 1] * int(shape[i + 1])
    reg = []
    l, h = lo, hi
    for i, s in enumerate(strides):
        a, l = divmod(l, s)
        b, h = divmod(h, s)
        if b < a or (i > 0 and reg and reg[-1][0] != reg[-1][1] and False):
            a, b = 0, int(shape[i]) - 1
        reg.append((a, b))
    for i in range(len(reg)):
        a, b = reg[i]
        if b < a:
            reg[i] = (0, int(shape[i]) - 1)
    return tuple(reg)


def _overlap(r1, r2):
    for (a0, a1), (b0, b1) in zip(r1, r2):
        if a1 < b0 or b1 < a0:
            return False
    return True


def _contains(outer, inner):
    for (a0, a1), (b0, b1) in zip(outer, inner):
        if b0 < a0 or b1 > a1:
            return False
    return True


class Rec:
    __slots__ = ("reg", "writer", "readers")

    def __init__(self, reg, writer, readers):
        self.reg = reg
        self.writer = writer
        self.readers = readers


class K:
    def __init__(self, nc):
        self.nc = nc
        self.ops = {e: [] for e in ENGS}
        self.cnt = {e: 0 for e in ENGS}
        self.clock = {e: {} for e in ENGS}
        self.recs = {}
        self.sems = {}
        self._ctx = []
        for e in ENGS:
            self.sems[e] = self._enter(nc.semaphore("s_" + e))
        self.ring = {}
        self.ring_pos = {}
        self.ring_val = {}
        for q in ("sp", "pool", "act"):
            self.ring[q] = []
            for i in range(NRING):
                key = "d_%s_%d" % (q, i)
                self.sems[key] = self._enter(nc.semaphore(key))
                self.ring[q].append(key)
            self.ring_pos[q] = 0
            self.ring_val[q] = [0] * NRING
        self.n_wait = 0
        self.tcount = 0

    def _enter(self, guard):
        v = guard.__enter__()
        self._ctx.append(guard)
        return v

    def sbuf(self, name, shape, dtype):
        self.tcount += 1
        return self._enter(self.nc.sbuf_tensor("%s_%d" % (name, self.tcount), list(shape), dtype))

    def psum(self, name, shape, dtype=F32):
        self.tcount += 1
        return self._enter(self.nc.psum_tensor("%s_%d" % (name, self.tcount), list(shape), dtype))

    def dram(self, name, shape, dtype, kind="Internal"):
        return self.nc.dram_tensor(name, list(shape), dtype, kind=kind)

    def _need(self, eng, ev, skip_self_pe):
        if ev is None:
            return
        key, val = ev
        if key == eng and eng == "pe" and skip_self_pe:
            return
        if self.clock[eng].get(key, 0) >= val:
            return
        self.clock[eng][key] = val
        sem = self.sems[key]
        self.ops[eng].append(("w", sem, val))
        self.n_wait += 1

    def _deps(self, eng, reads, writes):
        need = []
        for ap in reads:
            name = ap.tensor.name
            reg = _region(ap)
            for r in self.recs.get(name, ()):
                if r.writer is not None and _overlap(r.reg, reg):
                    need.append(r.writer)
        for ap in writes:
            name = ap.tensor.name
            reg = _region(ap)
            for r in self.recs.get(name, ()):
                if _overlap(r.reg, reg):
                    if r.writer is not None:
                        need.append(r.writer)
                    for k, v in r.readers.items():
                        need.append((k, v))
        return need

    def _record(self, ev, reads, writes):
        key, val = ev
        for ap in reads:
            name = ap.tensor.name
            reg = _region(ap)
            lst = self.recs.setdefault(name, [])
            for r in lst:
                if r.reg == reg:
                    if r.readers.get(key, 0) < val:
                        r.readers[key] = val
                    break
            else:
                lst.append(Rec(reg, None, {key: val}))
        for ap in writes:
            name = ap.tensor.name
            reg = _region(ap)
            lst = self.recs.setdefault(name, [])
            lst[:] = [r for r in lst if not _contains(reg, r.reg)]
            lst.append(Rec(reg, ev, {}))

    def op(self, eng, fn, reads=(), writes=()):
        writes = list(writes) + [ap for ap in reads if _is_psum(ap)]
        reads = [ap for ap in reads if not _is_psum(ap)]
        for ev in self._deps(eng, reads, writes):
            self._need(eng, ev, True)
        self.cnt[eng] += 1
        ev = (eng, self.cnt[eng])
        self.ops[eng].append(("o", fn, self.sems[eng], 1))
        self._record(ev, reads, writes)
        return ev

    def dma(self, q, out, in_, **kw):
        for ev in self._deps(q, [in_], [out]):
            self._need(q, ev, False)
        i = self.ring_pos[q]
        self.ring_pos[q] = (i + 1) % NRING
        key = self.ring[q][i]
        prev = self.ring_val[q][i]
        if prev:
            self._need(q, (key, prev), False)
        val = prev + 16
        self.ring_val[q][i] = val
        ev = (key, val)

        def fn(e, out=out, in_=in_, kw=kw):
            return e.dma_start(out=out, in_=in_, **kw)
        self.ops[q].append(("o", fn, self.sems[key], 16))
        self._record(ev, [in_], [out])
        return ev

    def dma_custom(self, q, fn, reads, writes):
        for ev in self._deps(q, reads, writes):
            self._need(q, ev, False)
        i = self.ring_pos[q]
        self.ring_pos[q] = (i + 1) % NRING
        key = self.ring[q][i]
        prev = self.ring_val[q][i]
        if prev:
            self._need(q, (key, prev), False)
        val = prev + 16
        self.ring_val[q][i] = val
        ev = (key, val)
        self.ops[q].append(("o", fn, self.sems[key], 16))
        self._record(ev, reads, writes)
        return ev

    def mark(self):
        return len(self._ctx)

    def pe_drain(self):
        if self.cnt["pe"]:
            self.ops["pe"].append(("w", self.sems["pe"], self.cnt["pe"]))

    def barrier(self):
        for e in ENGS:
            self.wait_all(e)
        self.recs.clear()

    def release(self, m):
        self.barrier()
        while len(self._ctx) > m:
            self._ctx.pop().__exit__(None, None, None)

    def wait_all(self, eng):
        for e in ENGS:
            if e != eng and self.cnt[e]:
                self._need(eng, (e, self.cnt[e]), False)
        for q in self.ring:
            for i, key in enumerate(self.ring[q]):
                if self.ring_val[q][i]:
                    self._need(eng, (key, self.ring_val[q][i]), False)

    def emit(self):
        nc = self.nc
        engobj = {"pe": "tensor", "act": "scalar", "dve": "vector", "pool": "gpsimd", "sp": "sync"}
        with nc.Block() as block:
            for e in ENGS:
                lst = self.ops[e]
                if not lst:
                    continue

                def body(eng, lst=lst):
                    for it in lst:
                        if it[0] == "w":
                            eng.wait_ge(it[1], it[2])
                        else:
                            it[1](eng).then_inc(it[2], it[3])
                getattr(block, engobj[e])(body)

    def close(self):
        for g in reversed(self._ctx):
            g.__exit__(None, None, None)

    def mm(self, out, lhsT, rhs, start=True, stop=True, **kw):
        return self.op("pe", lambda e: e.matmul(out, lhsT, rhs, start=start, stop=stop, **kw),
                       reads=[lhsT, rhs], writes=[out])

    def tr(self, out, in_, ident):
        return self.op("pe", lambda e: e.transpose(out, in_, ident), reads=[in_, ident], writes=[out])

    def act(self, out, in_, func, bias=None, scale=None, accum_out=None, eng="act"):
        kw = {}
        reads = [in_]
        writes = [out]
        if bias is not None:
            kw["bias"] = bias
            if not isinstance(bias, (int, float)):
                reads.append(bias)
        if scale is not None:
            kw["scale"] = scale
            if not isinstance(scale, (int, float)):
                reads.append(scale)
        if accum_out is not None:
            kw["accum_out"] = accum_out
            writes.append(accum_out)
        return self.op(eng, lambda e: e.activation(out, in_, func, **kw), reads=reads, writes=writes)

    def tt(self, out, in0, in1, op, eng="dve"):
        return self.op(eng, lambda e: e.tensor_tensor(out, in0, in1, op), reads=[in0, in1], writes=[out])

    def ts(self, out, in0, s1, op0, s2=None, op1=None, eng="dve", accum_out=None):
        reads = [in0]
        if not isinstance(s1, (int, float)):
            reads.append(s1)
        if s2 is not None and not isinstance(s2, (int, float)):
            reads.append(s2)
        writes = [out]
        kw = {}
        if accum_out is not None:
            kw["accum_out"] = accum_out
            writes.append(accum_out)
        if op1 is None:
            return self.op(eng, lambda e: e.tensor_scalar(out, in0, s1, None, op0, **kw), reads=reads, writes=writes)
        return self.op(eng, lambda e: e.tensor_scalar(out, in0, s1, s2, op0, op1, **kw), reads=reads, writes=writes)

    def stt(self, out, in0, scalar, in1, op0, op1, eng="dve"):
        reads = [in0, in1]
        if not isinstance(scalar, (int, float)):
            reads.append(scalar)
        return self.op(eng, lambda e: e.scalar_tensor_tensor(out, in0, scalar, in1, op0, op1),
                       reads=reads, writes=[out])

    def copy(self, out, in_, eng="dve"):
        if eng == "act":
            return self.op("act", lambda e: e.copy(out, in_), reads=[in_], writes=[out])
        return self.op(eng, lambda e: e.tensor_copy(out, in_), reads=[in_], writes=[out])

    def memset(self, ap, val, eng="dve"):
        return self.op(eng, lambda e: e.memset(ap, val), reads=[], writes=[ap])


from concourse.bass_utils import run_bass_kernel_spmd

T = 4352
NCH = 34
CTX = 256
S_LAT = 4096
D = 1024
DEPTH = 2
NEXP = 32
EPS = 1e-6
ALPHA = 1.702
TILES = [(0, 256, 1)] + [(256 + 512 * i, 512, 0) for i in range(8)]
NBLK = (T * 4) // 128 + NEXP
V_NMW, V_NFW, V_BADA, V_CONVB, V_CONVW, V_SNW, V_ANW = 0, 8, 16, 64, 70, 100, 104
NVEC = 108
R_DTB, R_ALOG, R_DSKIP, R_SINK, R_BR = 0, 16, 32, 40, 48
R_CONVB = 80
NROW = 848
C_ID, C_PERM, C_U0, C_U1, C_L0, C_L1, C_ONE, C_E0 = 0, 128, 256, 384, 512, 640, 768, 896
NCST = 1024


def bc(ap, axis, n):
    shp = list(ap.shape)
    shp.insert(axis, n)
    return ap.unsqueeze(axis).broadcast_to(shp)


class Prog:
    def __init__(self, debug=None, nlayers=DEPTH, stop_after=None):
        self.debug = debug or ()
        self.nlayers = nlayers
        self.stop_after = stop_after
        nc = bass.Bass("TRN2", target_bir_lowering=False)
        self.nc = nc
        self.k = K(nc)
        k = self.k
        I = {}

        def inp(name, shape):
            I[name] = nc.dram_tensor(name, list(shape), F32, kind="ExternalInput").ap()
        inp("x", [S_LAT, D]); inp("ctx", [CTX, D])
        inp("w_ada", [DEPTH, D, 6 * D]); inp("w_in", [DEPTH, D, 2064]); inp("w_out", [DEPTH, D, D])
        inp("w_router", [DEPTH, D, NEXP]); inp("w_gate_up", [DEPTH * NEXP * D, 2 * D])
        inp("b_gate_up", [DEPTH * NEXP, 2 * D]); inp("w_down", [DEPTH * NEXP * D, D]); inp("b_down", [DEPTH * NEXP, D])
        inp("vec", [DEPTH, 128, NVEC]); inp("gvec", [128, 24]); inp("rowp", [DEPTH, 128, NROW])
        inp("cst", [128, NCST]); inp("rope", [128, 2, S_LAT]); inp("iot", [128, 257])
        self.I = I
        self.out = nc.dram_tensor("out", [S_LAT, D], F32, kind="ExternalOutput").ap()
        self.S = {}

        def scr(name, shape, dt):
            kind = "ExternalOutput" if name in self.debug else "Internal"
            self.S[name] = nc.dram_tensor("s_" + name, list(shape), dt, kind=kind).ap()
        scr("xT", [D, T], F32)
        scr("qT", [512, T], BF16); scr("kT", [128, T], BF16); scr("v", [T, 128], BF16)
        scr("zs", [T, 512], BF16); scr("dt", [T, 16], F32); scr("xbcT", [768, T], BF16)
        scr("xs", [T, 512], BF16); scr("Bt", [T, 128], BF16); scr("BT", [128, T], BF16); scr("CT", [128, T], BF16)
        scr("yf", [T, 512], F32); scr("mixT", [D, T], BF16)
        if "yb" in self.debug:
            scr("yb", [T, 512], F32)
        if "d_dsts" in self.debug:
            scr("d_dsts", [128, NCH * 4], I32); scr("d_blkf", [128, NBLK], F32); scr("d_base", [128, NEXP], F32)
            scr("d_gates", [128, NCH * 4], F32); scr("d_pend", [128, NEXP], F32)
        if "dg1" in self.debug:
            scr("dg1", [T, 512], F32); scr("dg2", [T, 512], F32); scr("dg3", [T, 512], BF16)
        scr("h2", [T, D], BF16); scr("gat", [T, 4], F32); scr("dst", [T, 4], I32)
        scr("buf", [NBLK * 128, D], BF16); scr("obuf", [NBLK * 128, D], BF16)
        scr("blke", [1, 2 * NBLK], I32)
        self.psum_gen = 0
        self.pool_regs = {}
        self.cf = k.sbuf("cstf", [128, NCST], F32)
        self.cb = k.sbuf("cstb", [128, NCST], BF16)
        self.gv = k.sbuf("gvec_sb", [128, 24], F32)
        k.dma("sp", self.cf[:], I["cst"])
        k.dma("pool", self.cb[:], I["cst"])
        k.dma("sp", self.gv[:], I["gvec"])

    def alloc_psum(self, nf, nt):
        k = self.k
        self.psum_gen += 1
        self.pf = [k.psum("pf%d_%d" % (self.psum_gen, i), [128, 512], F32) for i in range(nf)]
        self.pt = [k.psum("pt%d_%d" % (self.psum_gen, i), [128, 1024], BF16) for i in range(nt)]

    def cB(self, off, n=128, p=128):
        return self.cb[0:p, off:off + n]

    def cF(self, off, n=128, p=128):
        return self.cf[0:p, off:off + n]

    def phase_x0(self):
        k, I, S = self.k, self.I, self.S
        m = k.mark()
        self.alloc_psum(2, 0)
        xin = [k.sbuf("x0in%d" % i, [128, D], F32) for i in range(2)]
        xo = [k.sbuf("x0o%d" % i, [128, 8, 128], F32) for i in range(2)]
        xTv = S["xT"].rearrange("(k p) t -> p k t", p=128)
        for c in range(NCH):
            src = I["ctx"][c * 128:(c + 1) * 128, :] if c < 2 else I["x"][(c - 2) * 128:(c - 1) * 128, :]
            xi = xin[c % 2]; o = xo[c % 2]
            k.dma("sp", xi[:], src)
            for h in range(2):
                p = self.pf[h]
                for j in range(4):
                    kk = h * 4 + j
                    k.mm(p[:, j * 128:(j + 1) * 128], xi[:, kk * 128:(kk + 1) * 128], self.cF(C_ID))
                k.copy(o[:, h * 4:(h + 1) * 4, :], p[:].rearrange("p (j t) -> p j t", j=4), eng=("dve" if h else "act"))
            k.dma("pool", xTv[:, :, c * 128:(c + 1) * 128], o[:])
        k.release(m)

    def phase_ada(self, l):
        k, I = self.k, self.I
        vec = self.vec
        m = k.mark()
        self.alloc_psum(1, 0)
        sc = k.sbuf("ada_sc", [128, 8, 2], F32)
        k.act(sc[:, :, 0], self.gv[:, 8:16], AF.Silu)
        k.act(sc[:, :, 1], self.gv[:, 16:24], AF.Silu)
        wa = [k.sbuf("ada_w%d" % i, [128, 8, 512], F32) for i in range(2)]
        pm = self.pf[0]
        pmv = pm[:, 0:96].rearrange("p (j s) -> p j s", s=2)
        wv = I["w_ada"][l].rearrange("(k p) n -> p k n", p=128)
        for g in range(12):
            w = wa[g % 2]
            k.dma("sp", w[:], wv[:, :, g * 512:(g + 1) * 512])
            for jj in range(4):
                j = g * 4 + jj
                for kk in range(8):
                    k.mm(pmv[:, j, :], w[:, kk, jj * 128:(jj + 1) * 128], sc[:, kk, :], start=(kk == 0), stop=(kk == 7))
        mod = self.mod
        k.tt(mod[:], pmv, bc(vec[:, V_BADA:V_BADA + 48], 2, 2), ALU.add)
        for (A, base, nw) in ((self.A1, 8, V_NMW), (self.A2, 32, V_NFW)):
            k.ts(A[:], mod[:, base:base + 8, :], 1.0, ALU.add)
            k.tt(A[:], A[:], bc(vec[:, nw:nw + 8], 2, 2), ALU.mult)
        k.release(m)

    def norm_mod(self, xt, n, s, A, Boff, hT, sq, rstd, ps):
        k = self.k
        k.act(sq[:, :, 0:n], xt[:, :, 0:n], AF.Square)
        for kk in range(8):
            k.mm(ps[:, 0:n], self.cB(C_ONE), sq[:, kk, 0:n], start=(kk == 0), stop=(kk == 7))
        k.act(rstd[:, 0:n], ps[:, 0:n], AF.Sqrt, bias=self.epsb[:, 0:1], scale=1.0 / D)
        k.op("dve", lambda e: e.reciprocal(rstd[:, 0:n], rstd[:, 0:n]), reads=[rstd[:, 0:n]], writes=[rstd[:, 0:n]])
        for kk in range(8):
            tp = self.nm_tmp[kk % 2]
            k.stt(tp[:, 0:n], xt[:, kk, 0:n], A[:, kk, s:s + 1], rstd[:, 0:n], ALU.mult, ALU.mult)
            k.act(hT[:, kk, 0:n], tp[:, 0:n], AF.Identity, bias=self.mod[:, Boff + kk, s:s + 1], scale=1.0)

    def phase_inproj(self, l):
        k, I, S = self.k, self.I, self.S
        m = k.mark()
        self.alloc_psum(6, 0)
        win = k.sbuf("win", [128, 8, 2064], BF16)
        wv = I["w_in"][l].rearrange("(k p) n -> p k n", p=128)
        for kk in range(8):
            for c0_ in (0, 1024, 2048):
                c1_ = min(c0_ + 1024, 2064)
                k.dma("pool", win[:, kk, c0_:c1_], wv[:, kk, c0_:c1_])
        rope = k.sbuf("rope_sb", [128, 2, S_LAT], F32)
        k.dma("sp", rope[:], I["rope"])
        xts = [k.sbuf("ip_x%d" % i, [128, 8, 512], F32) for i in range(2)]
        hTs = [k.sbuf("ip_h%d" % i, [128, 8, 512], BF16) for i in range(2)]
        sq = k.sbuf("ip_sq", [128, 8, 512], BF16)
        rstd = k.sbuf("ip_rstd", [128, 512], F32)
        qb = [k.sbuf("ip_qb%d" % i, [128, 512], BF16) for i in range(2)]
        t1 = [k.sbuf("ip_t1%d" % i, [128, 512], F32) for i in range(2)]
        t2 = [k.sbuf("ip_t2%d" % i, [128, 512], F32) for i in range(2)]
        ob = [k.sbuf("ip_ob%d" % i, [128, 512], BF16) for i in range(3)]
        zsb = [k.sbuf("ip_zs%d" % i, [128, 512], BF16) for i in range(2)]
        vsb = [k.sbuf("ip_v%d" % i, [128, 128], BF16) for i in range(2)]
        dtt = [k.sbuf("ip_dt%d" % i, [128, 16], F32) for i in range(2)]
        xTv = S["xT"].rearrange("(k p) t -> p k t", p=128)
        cnt = 0
        for ti, (t0, n, s) in enumerate(TILES):
            xt = xts[ti % 2]; hT = hTs[ti % 2]
            k.dma("sp", xt[:, :, 0:n], xTv[:, :, t0:t0 + n])
            self.norm_mod(xt, n, s, self.A1, 0, hT, sq, rstd, self.pf[5])
            fm = [("q", c, c * 128) for c in range(4)] + [("k", 0, 512)] + [("xbc", c, 1280 + c * 128) for c in range(6)]
            for (nm, c, col) in fm:
                ps = self.pf[cnt % 2]; cnt += 1
                for kk in range(8):
                    k.mm(ps[:, 0:n], win[:, kk, col:col + 128], hT[:, kk, 0:n], start=(kk == 0), stop=(kk == 7))
                o = ob[cnt % 3]
                if nm == "xbc":
                    k.copy(o[:, 0:n], ps[:, 0:n], eng="act")
                    k.dma("pool", S["xbcT"][c * 128:(c + 1) * 128, t0:t0 + n], o[:, 0:n])
                    continue
                dst = S["qT"][c * 128:(c + 1) * 128, t0:t0 + n] if nm == "q" else S["kT"][:, t0:t0 + n]
                if s == 1:
                    k.copy(o[:, 0:n], ps[:, 0:n], eng="act")
                else:
                    q_ = qb[cnt % 2]; t_ = t1[cnt % 2]
                    p0 = t0 - CTX
                    k.copy(q_[:, 0:n], ps[:, 0:n], eng="act")
                    pp = self.pf[2 + cnt % 2]
                    k.mm(pp[:, 0:n], self.cB(C_PERM), q_[:, 0:n])
                    t2_ = t2[cnt % 2]
                    k.tt(t_[:, 0:n], q_[:, 0:n], rope[:, 0, p0:p0 + n], ALU.mult)
                    k.tt(t2_[:, 0:n], pp[:, 0:n], rope[:, 1, p0:p0 + n], ALU.mult)
                    k.tt(o[:, 0:n], t_[:, 0:n], t2_[:, 0:n], ALU.add)
                k.dma("pool", dst, o[:, 0:n])
            for j in range(n // 128):
                c0 = t0 + j * 128
                pz = self.pf[4]; pv = self.pf[2 + j % 2]
                for kk in range(8):
                    k.mm(pz[:, :], hT[:, kk, j * 128:(j + 1) * 128], win[:, kk, 768:1280], start=(kk == 0), stop=(kk == 7))
                for kk in range(8):
                    k.mm(pv[:, 0:128], hT[:, kk, j * 128:(j + 1) * 128], win[:, kk, 640:768], start=(kk == 0), stop=(kk == 7))
                for kk in range(8):
                    k.mm(pv[:, 128:144], hT[:, kk, j * 128:(j + 1) * 128], win[:, kk, 2048:2064], start=(kk == 0), stop=(kk == 7))
                z_ = zsb[j % 2]; v_ = vsb[j % 2]; d_ = dtt[j % 2]
                k.act(z_[:], pz[:], AF.Silu)
                k.dma("pool", S["zs"][c0:c0 + 128, :], z_[:])
                k.copy(v_[:], pv[:, 0:128], eng="dve")
                k.dma("pool", S["v"][c0:c0 + 128, :], v_[:])
                k.tt(d_[:], pv[:, 128:144], self.rowp[:, R_DTB:R_DTB + 16], ALU.add)
                k.act(d_[:], d_[:], AF.Exp)
                k.act(d_[:], d_[:], AF.Ln, bias=self.oneb[:, 0:1], scale=1.0)
                k.dma("pool", S["dt"][c0:c0 + 128, :], d_[:])
        k.release(m)

    def phase_conv(self, l):
        k, I, S = self.k, self.I, self.S
        vec = self.vec
        m = k.mark()
        self.alloc_psum(6, 0)
        diag = k.sbuf("cv_diag", [128, 6, 5, 128], BF16)
        for c in range(6):
            for j in range(5):
                k.ts(diag[:, c, j, :], self.cF(C_ID), vec[:, V_CONVW + c * 5 + j:V_CONVW + c * 5 + j + 1], ALU.mult,
                     eng="dve")
        cbrow = k.sbuf("cv_cbrow", [128, 768], BF16)
        k.copy(cbrow[:], self.rowp[:, R_CONVB:R_CONVB + 768])
        xins = [k.sbuf("cv_xin%d" % i, [128, 6, 516], BF16) for i in range(2)]
        ofm = [k.sbuf("cv_ofm%d" % i, [128, 512], BF16) for i in range(2)]
        oxs = [k.sbuf("cv_oxs%d" % i, [128, 512], BF16) for i in range(2)]
        obt = [k.sbuf("cv_obt%d" % i, [128, 128], BF16) for i in range(2)]
        xv = S["xbcT"].rearrange("(c p) t -> p c t", p=128)
        cnt = 0
        for ti, (t0, n, s) in enumerate(TILES):
            xin = xins[ti % 2]
            s0, s1 = (0, CTX) if s == 1 else (CTX, T)
            lo, hi = t0 - 2, t0 + n + 2
            clo, chi = max(lo, s0), min(hi, s1)
            if lo < s0:
                k.memset(xin[:, :, 0:2], 0.0)
            if hi > s1:
                k.memset(xin[:, :, n + 2:n + 4], 0.0)
            k.dma("sp", xin[:, :, clo - lo:chi - lo], xv[:, :, clo:chi])
            for c in (4, 5):
                ps = self.pf[cnt % 2]; o = ofm[cnt % 2]; cnt += 1
                for j in range(5):
                    k.mm(ps[:, 0:n], diag[:, c, j, :], xin[:, c, j:j + n], start=(j == 0), stop=(j == 4))
                k.act(o[:, 0:n], ps[:, 0:n], AF.Silu, bias=vec[:, V_CONVB + c:V_CONVB + c + 1], scale=1.0)
                k.dma("pool", (S["BT"] if c == 4 else S["CT"])[:, t0:t0 + n], o[:, 0:n])
            for jt in range(n // 128):
                c0 = t0 + jt * 128
                pa = self.pf[2 + jt % 2]; pb = self.pf[4 + jt % 2]
                for c in range(5):
                    dstp = pa[:, c * 128:(c + 1) * 128] if c < 4 else pb[:, 0:128]
                    for j in range(5):
                        k.mm(dstp, xin[:, c, jt * 128 + j:jt * 128 + j + 128], diag[:, c, j, :], start=(j == 0), stop=False)
                    k.mm(dstp, self.cB(C_E0), cbrow[:, c * 128:(c + 1) * 128], start=False, stop=True)
                ox = oxs[jt % 2]; ob = obt[jt % 2]
                k.act(ox[:], pa[:], AF.Silu)
                k.dma("pool", S["xs"][c0:c0 + 128, :], ox[:])
                k.act(ob[:], pb[:, 0:128], AF.Silu)
                k.dma("pool", S["Bt"][c0:c0 + 128, :], ob[:])
        k.release(m)

    def rms_to_mixT(self, src, ngrp, gsz, nwoff, row0, c, tg):
        k, S = self.k, self.S
        ssq, rs, gn, mixc, junk = tg
        for g in range(ngrp):
            k.act(junk[:, 0:gsz], src[:, g * gsz:(g + 1) * gsz], AF.Square, accum_out=ssq[:, g:g + 1])
        k.act(rs[:, 0:ngrp], ssq[:, 0:ngrp], AF.Sqrt, bias=self.epsb[:, 0:1], scale=1.0 / gsz)
        k.op("dve", lambda e: e.reciprocal(rs[:, 0:ngrp], rs[:, 0:ngrp]), reads=[rs[:, 0:ngrp]], writes=[rs[:, 0:ngrp]])
        k.tt(gn[:].rearrange("p (g f) -> p g f", g=ngrp), src[:].rearrange("p (g f) -> p g f", g=ngrp),
             bc(rs[:, 0:ngrp], 2, gsz), ALU.mult)
        pt = self.pt[c % len(self.pt)]
        k.tr(pt[:, 512:640], gn[:, 0:128], self.cB(C_ID))
        for kk in range(4):
            k.tr(pt[:, kk * 128:(kk + 1) * 128], gn[:, kk * 128:(kk + 1) * 128], self.cB(C_ID))
        for kk in range(4):
            k.act(mixc[:, kk, :], pt[:, kk * 128:(kk + 1) * 128], AF.Copy, scale=self.vec[:, nwoff + kk:nwoff + kk + 1])
        mv = S["mixT"].rearrange("(k p) t -> p k t", p=128)
        k.dma("pool", mv[:, row0 // 128:row0 // 128 + 4, c * 128:(c + 1) * 128], mixc[:])

    def phase_attn(self, l, last):
        k, I, S = self.k, self.I, self.S
        m = k.mark()
        self.alloc_psum(4, 2)
        Kt = k.sbuf("at_K", [128, 2, T], BF16)
        Qt = k.sbuf("at_Q", [128, 8, T], BF16)
        k.memset(Kt[:], 0.0)
        k.memset(Qt[:], 0.0)
        Va = k.sbuf("at_V", [128, NCH, 2, 65], BF16)
        esk = k.sbuf("at_es", [128, 8], F32)
        for g in range(2):
            k.dma("sp", Kt[0:64, g, :], S["kT"][g * 64:(g + 1) * 64, :])
        for h in range(8):
            k.dma("sp", Qt[0:64, h, :], S["qT"][h * 64:(h + 1) * 64, :])
        k.memset(Va[:], 1.0)
        for c in range(NCH):
            k.dma("sp", Va[:, c, :, 0:64], S["v"][c * 128:(c + 1) * 128, :].rearrange("p (g d) -> p g d", g=2))
        k.act(esk[:], self.rowp[:, R_SINK:R_SINK + 8], AF.Exp)
        ET = [[k.sbuf("at_E%d_%d" % (i, j), [128, 4, 128], BF16) for j in range(5)] for i in range(2)]
        attn = [k.sbuf("at_o%d" % i, [128, 512], F32) for i in range(2)]
        den = k.sbuf("at_den", [128, 4], F32)
        tg = (k.sbuf("at_ssq", [128, 2], F32), k.sbuf("at_rs", [128, 2], F32), k.sbuf("at_gn", [128, 512], BF16),
              k.sbuf("at_mix", [128, 4, 128], BF16), k.sbuf("at_junk", [128, 512], F32))
        blocks = list(range(0 if not last else 2, NCH))
        it = 0
        for c in blocks:
            if c < 2:
                keys = [(0, None), (1, None)]
            else:
                keys = [(0, None), (1, None)]
                if c > 2:
                    keys.append((c - 1, C_U1))
                keys.append((c, None))
                if c < NCH - 1:
                    keys.append((c + 1, C_U0))
            at = attn[c % 2]
            for g in range(2):
                ets = ET[it % 2]
                pv = self.pf[2 + it % 2]
                pvv = pv[:].rearrange("p (r d) -> p r d", r=4)
                for idx, (kc, mk) in enumerate(keys):
                    ps = self.pf[idx % 2]
                    psv = ps[:].rearrange("p (r q) -> p r q", r=4)
                    k.mm(psv, Kt[:, g, kc * 128:(kc + 1) * 128], Qt[:, 4 * g:4 * g + 4, c * 128:(c + 1) * 128])
                    k.act(ets[idx][:], psv, AF.Exp, scale=0.125)
                    if mk is not None:
                        k.tt(ets[idx][:], ets[idx][:], bc(self.cB(mk), 1, 4), ALU.mult)
                for r in range(4):
                    for idx, (kc, mk) in enumerate(keys):
                        k.mm(pvv[:, r, 0:65], ets[idx][:, r, :], Va[:, kc, g, :], start=(idx == 0), stop=(idx == len(keys) - 1))
                k.tt(den[:], pvv[:, :, 64], esk[:, 4 * g:4 * g + 4], ALU.add)
                k.op("dve", lambda e: e.reciprocal(den[:], den[:]), reads=[den[:]], writes=[den[:]])
                k.tt(at[:, g * 256:(g + 1) * 256].rearrange("p (r d) -> p r d", r=4), pvv[:, :, 0:64], bc(den[:], 2, 64), ALU.mult)
                it += 1
            self.rms_to_mixT(at, 1, 512, V_ANW, 0, c, tg)
        k.release(m)

    def phase_ssd(self, l, last):
        k, I, S = self.k, self.I, self.S
        m = k.mark()
        self.alloc_psum(6, 2)
        rowp = self.rowp
        abc = k.sbuf("sd_a", [128, 16], F32)
        k.act(abc[:], rowp[:, R_ALOG:R_ALOG + 16], AF.Exp)
        k.ts(abc[:], abc[:], -1.0, ALU.mult)
        xsb = [k.sbuf("sd_xs%d" % i, [128, 512], BF16) for i in range(2)]
        btb = [k.sbuf("sd_bt%d" % i, [128, 128], BF16) for i in range(2)]
        BTc = [k.sbuf("sd_BT%d" % i, [128, 256], BF16) for i in range(2)]
        CTc = [k.sbuf("sd_CT%d" % i, [128, 256], BF16) for i in range(2)]
        for t_ in BTc + CTc:
            k.memset(t_[:], 0.0)
        dtb = [k.sbuf("sd_dt%d" % i, [128, 16], F32) for i in range(2)]
        yfb = [k.sbuf("sd_yf%d" % i, [128, 512], F32) for i in range(2)]
        zsb = [k.sbuf("sd_zs%d" % i, [128, 512], BF16) for i in range(2)]
        dta = k.sbuf("sd_dta", [128, 8], F32)
        DU = k.sbuf("sd_DU", [128, 1024], BF16)
        E = k.sbuf("sd_E", [128, 1024], BF16)
        GM = k.sbuf("sd_GM", [128, 256], BF16)
        ST = k.sbuf("sd_ST", [128, 1024], BF16)
        xdt = k.sbuf("sd_xdt", [128, 512], BF16)
        xw = k.sbuf("sd_xw", [128, 512], BF16)
        ecs = k.sbuf("sd_ecs", [128, 8], F32)
        dec = k.sbuf("sd_dec", [128, 8], F32)
        tmp = k.sbuf("sd_tmp", [128, 512], F32)
        ys = [k.sbuf("sd_y%d" % i, [128, 512], F32) for i in range(2)]
        H = k.sbuf("sd_H", [128, 256], F32)
        Hbf = k.sbuf("sd_Hbf", [128, 256], BF16)
        gg = k.sbuf("sd_g", [128, 512], F32)
        tg = (k.sbuf("sd_ssq", [128, 2], F32), k.sbuf("sd_rs", [128, 2], F32), k.sbuf("sd_gn", [128, 512], BF16),
              k.sbuf("sd_mix", [128, 4, 128], BF16), k.sbuf("sd_junk", [128, 512], F32))
        pE0, pE1, pG, pY, pYO, pS7 = self.pf
        pX = pG[:, 256:512]

        def v3(ap, h):
            return ap.rearrange("p (h q) -> p h q", h=h)

        def v4(ap):
            return ap.rearrange("p (g r i) -> p g r i", g=2, r=4)
        for d in range(2):
            k.memset(H[:], 0.0)
            k.memset(Hbf[:], 0.0)
            order = list(range(NCH)) if d == 0 else [1, 0] + list(range(NCH - 1, 1, -1))
            cU = C_U0 if d == 0 else C_U1
            cL = C_L0 if d == 0 else C_L1
            iend = 127 if d == 0 else 0
            for it, c in enumerate(order):
                c0 = c * 128
                xs_, bt_, BT_, CT_, dt_ = xsb[it % 2], btb[it % 2], BTc[it % 2], CTc[it % 2], dtb[it % 2]
                k.dma("sp", xs_[:], S["xs"][c0:c0 + 128, :])
                k.dma("sp", bt_[:], S["Bt"][c0:c0 + 128, :])
                for g in range(2):
                    k.dma("sp", BT_[g * 64:(g + 1) * 64, g * 128:(g + 1) * 128], S["BT"][g * 64:(g + 1) * 64, c0:c0 + 128])
                    k.dma("sp", CT_[g * 64:(g + 1) * 64, g * 128:(g + 1) * 128], S["CT"][g * 64:(g + 1) * 64, c0:c0 + 128])
                k.dma("sp", dt_[:], S["dt"][c0:c0 + 128, :])
                if d == 1:
                    yf_, zs_ = yfb[it % 2], zsb[it % 2]
                    k.dma("sp", yf_[:], S["yf"][c0:c0 + 128, :])
                    k.dma("sp", zs_[:], S["zs"][c0:c0 + 128, :])
                dtd = dt_[:, d * 8:(d + 1) * 8]
                k.tt(dta[:], dtd, abc[:, d * 8:(d + 1) * 8], ALU.mult)
                k.tt(v3(DU[:], 8), bc(self.cB(cU), 1, 8), bc(dta[:], 2, 128), ALU.mult)
                k.mm(pE0[:], self.cB(cL), DU[:, 0:512])
                k.mm(pE1[:], self.cB(cL), DU[:, 512:1024])
                k.act(E[:, 0:512], pE0[:], AF.Exp)
                k.act(E[:, 512:1024], pE1[:], AF.Exp)
                k.mm(pX[:, 0:8], self.cF(cU), dta[:])
                k.mm(pX[:, 8:16], self.cF(C_ONE), dta[:])
                k.act(ecs[:], pX[:, 0:8], AF.Exp)
                k.act(dec[:], pX[:, 8:16], AF.Exp)
                for g in range(2):
                    k.mm(pG[:, g * 128:(g + 1) * 128], BT_[:, g * 128:(g + 1) * 128], CT_[:, g * 128:(g + 1) * 128])
                k.tt(v3(GM[:], 2), v3(pG[:, 0:256], 2), bc(self.cB(cU), 1, 2), ALU.mult)
                k.tt(v4(ST[:]), v4(E[:]), bc(v3(GM[:], 2), 2, 4), ALU.mult)
                k.tt(v3(xdt[:], 8), v3(xs_[:], 8), bc(dtd, 2, 64), ALU.mult)
                k.tt(v3(xw[:], 8), v3(xdt[:], 8), bc(v3(E[:], 8)[:, :, iend], 2, 64), ALU.mult)
                for h in range(8):
                    k.mm(pY[:, h * 64:(h + 1) * 64], ST[:, h * 128:(h + 1) * 128], xdt[:, h * 64:(h + 1) * 64])
                for g in range(2):
                    k.mm(pYO[:, g * 256:(g + 1) * 256], CT_[:, g * 128:(g + 1) * 128], Hbf[:, :])
                y = ys[it % 2]
                k.tt(v3(tmp[:], 8), v3(pYO[:], 8), bc(ecs[:], 2, 64), ALU.mult)
                k.tt(y[:], pY[:], tmp[:], ALU.add)
                for g in range(2):
                    k.mm(pS7[:, g * 256:(g + 1) * 256], bt_[:, :], xw[:, g * 256:(g + 1) * 256])
                for g in range(2):
                    gs = slice(g * 64, (g + 1) * 64)
                    k.tt(v3(H[gs, :], 4), v3(H[gs, :], 4), bc(dec[gs, 4 * g:4 * g + 4], 2, 64), ALU.mult)
                    k.tt(H[gs, :], H[gs, :], pS7[gs, g * 256:(g + 1) * 256], ALU.add)
                k.copy(Hbf[:], H[:], eng="act")
                if d == 0:
                    k.dma("pool", S["yf"][c0:c0 + 128, :], y[:])
                else:
                    if "yb" in self.debug:
                        k.dma("pool", S["yb"][c0:c0 + 128, :], y[:])
                    k.tt(y[:], y[:], yf_[:], ALU.add)
                    k.tt(v3(tmp[:], 8), v3(xs_[:], 8), bc(rowp[:, R_DSKIP:R_DSKIP + 8], 2, 64), ALU.mult)
                    k.tt(y[:], y[:], tmp[:], ALU.add)
                    if "dg1" in self.debug:
                        k.dma("pool", S["dg1"][c0:c0 + 128, :], y[:])
                    k.tt(gg[:], y[:], zs_[:], ALU.mult)
                    if "dg1" in self.debug:
                        k.dma("pool", S["dg2"][c0:c0 + 128, :], gg[:])
                    self.rms_to_mixT(gg, 2, 256, V_SNW, 512, c, tg)
                    if "dg1" in self.debug:
                        k.dma("pool", S["dg3"][c0:c0 + 128, :], tg[2][:])
        k.release(m)

    def phase_outproj(self, l, last):
        k, I, S = self.k, self.I, self.S
        m = k.mark()
        self.alloc_psum(6, 2)
        wout = k.sbuf("op_w", [128, 8, D], BF16)
        wv = I["w_out"][l].rearrange("(k p) n -> p k n", p=128)
        for kk in range(8):
            k.dma("pool", wout[:, kk, :], wv[:, kk, :])
        wr = k.sbuf("op_wr", [128, 8, NEXP], F32)
        k.dma("sp", wr[:], I["w_router"][l].rearrange("(k p) e -> p k e", p=128))
        mixs = [k.sbuf("op_mix%d" % i, [128, 8, 512], BF16) for i in range(2)]
        xts = [k.sbuf("op_x%d" % i, [128, 8, 512], F32) for i in range(2)]
        h2f = k.sbuf("op_h2f", [128, 8, 512], F32)
        h2b = k.sbuf("op_h2b", [128, 8, 512], BF16)
        sq = k.sbuf("op_sq", [128, 8, 512], BF16)
        rstd = k.sbuf("op_rstd", [128, 512], F32)
        rows = [k.sbuf("op_rows%d" % i, [128, D], BF16) for i in range(2)]
        negm = k.sbuf("op_negm", [128, 1], F32)
        e4 = k.sbuf("op_e4", [128, 4], F32)
        gsum = k.sbuf("op_gsum", [128, 1], F32)
        maskb = k.sbuf("op_mask", [128, NEXP], BF16)
        xTv = S["xT"].rearrange("(k p) t -> p k t", p=128)
        mTv = S["mixT"].rearrange("(k p) t -> p k t", p=128)
        for ti, (t0, n, s) in enumerate(TILES):
            if last and s == 1:
                continue
            mix = mixs[ti % 2]; xt = xts[ti % 2]
            k.dma("sp", mix[:, :, 0:n], mTv[:, :, t0:t0 + n])
            k.dma("sp", xt[:, :, 0:n], xTv[:, :, t0:t0 + n])
            for mc in range(8):
                ps = self.pf[mc % 2]
                for kk in range(8):
                    k.mm(ps[:, 0:n], wout[:, kk, mc * 128:(mc + 1) * 128], mix[:, kk, 0:n], start=(kk == 0), stop=(kk == 7))
                k.stt(xt[:, mc, 0:n], ps[:, 0:n], self.mod[:, 16 + mc, s:s + 1], xt[:, mc, 0:n], ALU.mult, ALU.add)
            k.dma("pool", xTv[:, :, t0:t0 + n], xt[:, :, 0:n])
            self.norm_mod(xt, n, s, self.A2, 24, h2f, sq, rstd, self.pf[5])
            k.copy(h2b[:, :, 0:n], h2f[:, :, 0:n], eng="act")
            for j in range(n // 128):
                c = (t0 + j * 128) // 128
                pl = self.pf[2 + j % 2]
                for kk in range(8):
                    k.mm(pl[:, 0:NEXP], h2f[:, kk, j * 128:(j + 1) * 128], wr[:, kk, :], start=(kk == 0), stop=(kk == 7))
                lg = self.lgs[:, c, :]
                t8 = self.top8s[:, c, :]
                k.tt(lg, pl[:, 0:NEXP], self.rowp[:, R_BR:R_BR + NEXP], ALU.add)
                k.op("dve", lambda e, t8=t8, lg=lg: e.max(t8, lg), reads=[lg], writes=[t8])
                k.ts(negm[:], t8[:, 0:1], -1.0, ALU.mult)
                k.act(e4[:], t8[:, 0:4], AF.Exp, bias=negm[:, 0:1], scale=1.0, accum_out=gsum[:, 0:1])
                k.op("dve", lambda e: e.reciprocal(gsum[:], gsum[:]), reads=[gsum[:]], writes=[gsum[:]])
                k.ts(self.gates[:, c, :], e4[:], gsum[:, 0:1], ALU.mult)
                k.ts(maskb[:], lg, t8[:, 3:4], ALU.is_ge)
                pr = self.pf[4]
                k.mm(pr[:, 0:NEXP], self.cB(C_L1), maskb[:])
                k.mm(pr[:, NEXP:2 * NEXP], self.cB(C_ONE), maskb[:])
                k.tt(self.rks[:, c, :], pr[:, 0:NEXP], self.base[:], ALU.add)
                k.tt(self.base[:], self.base[:], pr[:, NEXP:2 * NEXP], ALU.add)
                pt = self.pt[j % 2]; rw = rows[j % 2]
                for kk in range(8):
                    k.tr(pt[:, kk * 128:(kk + 1) * 128], h2b[:, kk, j * 128:(j + 1) * 128], self.cB(C_ID))
                k.copy(rw[:, 0:512], pt[:, 0:512], eng="act")
                k.copy(rw[:, 512:1024], pt[:, 512:1024], eng="dve")
                k.dma("pool", S["h2"][c * 128:(c + 1) * 128, :], rw[:])
        k.release(m)

    def phase_route(self, l, last):
        k, I, S = self.k, self.I, self.S
        m = k.mark()
        chunks = list(range(2 if last else 0, NCH))
        NB = len(chunks) * 4 + NEXP
        self.NB = NB
        cm = k.sbuf("rt_cm", [128, NEXP], F32)
        gt = k.sbuf("rt_gt", [128, NEXP], F32)
        pd = k.sbuf("rt_pd", [128, NEXP], F32)
        ca = k.sbuf("rt_ca", [128, NEXP], F32)
        cbb = k.sbuf("rt_cb", [128, NEXP], F32)
        pstart = k.sbuf("rt_ps", [128, NEXP], F32)
        thr = k.sbuf("rt_thr", [128, 256], F32)
        k.ts(thr[:], self.iot[:, 0:256], 128.0, ALU.mult)
        cmp2 = k.sbuf("rt_cmp2", [128, NEXP, NCH], F32)
        k.tt(cmp2[:], bc(self.base[:], 2, NCH), bc(thr[:, 0:NCH], 1, NEXP), ALU.is_gt)
        k.op("dve", lambda e: e.reduce_sum(pd[:], cmp2[:], AX.X), reads=[cmp2[:]], writes=[pd[:]])
        k.ts(pd[:], pd[:], 128.0, ALU.mult)
        a, b = ca, cbb
        k.copy(a[:], pd[:])
        for sft in (1, 2, 4, 8, 16):
            k.copy(b[:, 0:sft], a[:, 0:sft])
            k.tt(b[:, sft:NEXP], a[:, sft:NEXP], a[:, 0:NEXP - sft], ALU.add)
            a, b = b, a
        pend = a
        k.tt(pstart[:], pend[:], pd[:], ALU.subtract)
        BIG = float(2 ** 30)
        cmp_ = k.sbuf("rt_cmp", [128, NB, NEXP], F32)
        blkf = k.sbuf("rt_blkf", [128, NB], F32)
        k.tt(cmp_[:], bc(pend[:, :], 1, NB), bc(thr[:, 0:NB], 2, NEXP), ALU.is_le)
        k.op("dve", lambda e: e.reduce_sum(blkf[:, 0:NB], cmp_[:], AX.X), reads=[cmp_[:]], writes=[blkf[:, 0:NB]])
        k.ts(blkf[:], blkf[:], float(NEXP - 1), ALU.min)
        k.copy(self.blkf[:, 0:NB], blkf[:, 0:NB])
        Dm = k.sbuf("rt_D", [128, NEXP], F32)
        oh = k.sbuf("rt_oh", [128, NEXP], F32)
        dstf = k.sbuf("rt_dstf", [128, 4], F32)
        rows = [k.sbuf("rt_rows%d" % i, [128, D], BF16) for i in range(2)]
        ixs = [k.sbuf("rt_ix%d" % i, [128, 1], I32) for i in range(8)]
        for ci, c in enumerate(chunks):
            rw = rows[ci % 2]
            k.dma("sp", rw[:], S["h2"][c * 128:(c + 1) * 128, :])
            k.tt(Dm[:], self.rks[:, c, :], pstart[:], ALU.add)
            for kq in range(4):
                k.ts(oh[:], self.lgs[:, c, :], self.top8s[:, c, kq:kq + 1], ALU.is_equal)
                k.tt(oh[:], oh[:], Dm[:], ALU.mult)
                k.op("dve", lambda e, kq=kq: e.reduce_sum(dstf[:, kq:kq + 1], oh[:], AX.X), reads=[oh[:]], writes=[dstf[:, kq:kq + 1]])
            k.copy(self.dsts[:, c, :], dstf[:])
            if "d_dsts" in self.debug:
                continue
            for kq in range(4):
                ixt = ixs[(ci * 4 + kq) % 8]
                k.copy(ixt[:], dstf[:, kq:kq + 1])
                ix = ixt[:, 0:1]
                k.dma_custom("pool", lambda e, ix=ix, rw=rw: e.indirect_dma_start(
                    out=S["buf"], out_offset=bass.IndirectOffsetOnAxis(ap=ix, axis=0), in_=rw[:], in_offset=None),
                    reads=[ix, rw[:]], writes=[S["buf"]])
        if "d_dsts" in self.debug:
            k.dma("sp", S["d_dsts"], self.dsts[:].rearrange("p c q -> p (c q)"))
            k.dma("sp", S["d_blkf"][:, 0:NB], self.blkf[:, 0:NB])
            k.dma("sp", S["d_base"], self.base[:])
            k.dma("sp", S["d_gates"], self.gates[:].rearrange("p c q -> p (c q)"))
            k.dma("sp", S["d_pend"], pend[:])
        k.release(m)

    def phase_moe(self, l, last):
        k, I, S = self.k, self.I, self.S
        m = k.mark()
        self.alloc_psum(6, 2)
        NB = self.NB
        wgu = k.sbuf("me_wgu", [128, 8, 2 * D], BF16)
        wd = k.sbuf("me_wd", [128, 8, D], BF16)
        bgu = k.sbuf("me_bgu", [128, 2 * D], BF16)
        bd = k.sbuf("me_bd", [128, D], BF16)
        rows = [k.sbuf("me_rows%d" % i, [128, D], BF16) for i in range(2)]
        rT = [k.sbuf("me_rT%d" % i, [128, 8, 128], BF16) for i in range(2)]
        gsb = k.sbuf("me_g", [128, D], F32)
        tsb = k.sbuf("me_t", [128, D], F32)
        lsb = k.sbuf("me_l", [128, D], F32)
        actb = k.sbuf("me_act", [128, D], BF16)
        aT = k.sbuf("me_aT", [128, 8, 128], BF16)
        osb = [k.sbuf("me_o%d" % i, [128, D], BF16) for i in range(2)]
        ones_row = self.cb[0:1, C_ONE:C_ONE + 128]
        wg2 = I["w_gate_up"]
        wd2 = I["w_down"]
        bg2 = I["b_gate_up"]
        bd2 = I["b_down"]
        sgu = [k.sbuf("me_sgu%d" % i, [128, 2 * D], F32) for i in range(2)]
        sdn = [k.sbuf("me_sdn%d" % i, [128, D], F32) for i in range(2)]
        c8 = k.sbuf("me_c8", [128, 8], F32)
        idf = k.sbuf("me_idf", [128, 9], F32)
        ixw = [k.sbuf("me_ixw%d" % i, [128, 1], I32) for i in range(18)]
        e1k = k.sbuf("me_e1k", [128, 1], F32)
        k.ts(idf[:, 0:1], self.iot[:, 256:257], 8.0, ALU.mult, float(l * NEXP * 1024), ALU.add)
        k.ts(c8[:], self.iot[:, 0:8], idf[:, 0:1], ALU.add)
        cnt = 0
        for b in range(NB):
            k.ts(e1k[:], self.blkf[:, b:b + 1], 1024.0, ALU.mult)
            k.ts(idf[:, 0:8], c8[:], e1k[:, 0:1], ALU.add)
            k.ts(idf[:, 8:9], self.blkf[:, b:b + 1], float(l * NEXP), ALU.add)
            sl = (b % 2) * 9
            for j in range(9):
                k.copy(ixw[sl + j][:], idf[:, j:j + 1])
            bix = ixw[sl + 8][:, 0:1]
            sg = sgu[cnt % 2]; cnt += 1
            k.dma_custom("pool", lambda e, bix=bix, sg=sg: e.indirect_dma_start(
                out=sg[:], out_offset=None, in_=bg2, in_offset=bass.IndirectOffsetOnAxis(ap=bix, axis=0)),
                reads=[bix, I["b_gate_up"]], writes=[sg[:]])
            k.copy(bgu[:], sg[:], eng="act")
            sd = sdn[cnt % 2]
            k.dma_custom("pool", lambda e, bix=bix, sd=sd: e.indirect_dma_start(
                out=sd[:], out_offset=None, in_=bd2, in_offset=bass.IndirectOffsetOnAxis(ap=bix, axis=0)),
                reads=[bix, I["b_down"]], writes=[sd[:]])
            k.copy(bd[:], sd[:], eng="dve")
            for kk in range(8):
                wix = ixw[sl + kk][:, 0:1]
                sg = sgu[cnt % 2]; sd = sdn[cnt % 2]; cnt += 1
                k.dma_custom("pool", lambda e, wix=wix, sg=sg: e.indirect_dma_start(
                    out=sg[:], out_offset=None, in_=wg2, in_offset=bass.IndirectOffsetOnAxis(ap=wix, axis=0)),
                    reads=[wix, I["w_gate_up"]], writes=[sg[:]])
                k.copy(wgu[:, kk, 0:D], sg[:, 0:D], eng="act")
                k.copy(wgu[:, kk, D:2 * D], sg[:, D:2 * D], eng="dve")
                k.dma_custom("pool", lambda e, wix=wix, sd=sd: e.indirect_dma_start(
                    out=sd[:], out_offset=None, in_=wd2, in_offset=bass.IndirectOffsetOnAxis(ap=wix, axis=0)),
                    reads=[wix, I["w_down"]], writes=[sd[:]])
                k.copy(wd[:, kk, :], sd[:], eng=("act" if kk % 2 else "dve"))
            rw = rows[b % 2]; rt = rT[b % 2]
            k.dma("sp", rw[:], S["buf"][b * 128:(b + 1) * 128, :])
            pt = self.pt[0]
            for kk in range(8):
                k.tr(pt[:, kk * 128:(kk + 1) * 128], rw[:].rearrange("p (c k) -> p k c", k=8)[:, kk, :], self.cB(C_ID))
            k.copy(rt[:, 0:4, :], pt[:, 0:512].rearrange("p (a t) -> p a t", a=4), eng="act")
            k.copy(rt[:, 4:8, :], pt[:, 512:1024].rearrange("p (a t) -> p a t", a=4), eng="dve")
            for nq in range(4):
                ps = self.pf[nq]
                for kk in range(8):
                    k.mm(ps[:], rt[:, kk, :], wgu[:, kk, nq * 512:(nq + 1) * 512], start=(kk == 0), stop=False)
                k.mm(ps[:], self.cB(C_E0), bgu[:, nq * 512:(nq + 1) * 512], start=False, stop=True)
            for h in range(2):
                k.ts(gsb[:, h * 512:(h + 1) * 512], self.pf[h][:], 7.0, ALU.min)
                k.ts(lsb[:, h * 512:(h + 1) * 512], self.pf[2 + h][:], 7.0, ALU.min, -7.0, ALU.max)
            k.act(tsb[:], gsb[:], AF.Silu, scale=ALPHA)
            k.act(lsb[:], lsb[:], AF.Identity, bias=self.ialb[:, 0:1], scale=1.0 / ALPHA)
            k.tt(actb[:], tsb[:], lsb[:], ALU.mult)
            pt2 = self.pt[1]
            for kk in range(8):
                k.tr(pt2[:, kk * 128:(kk + 1) * 128], actb[:].rearrange("p (c k) -> p k c", k=8)[:, kk, :], self.cB(C_ID))
            k.copy(aT[:, 0:4, :], pt2[:, 0:512].rearrange("p (a t) -> p a t", a=4), eng="act")
            k.copy(aT[:, 4:8, :], pt2[:, 512:1024].rearrange("p (a t) -> p a t", a=4), eng="dve")
            o = osb[b % 2]
            for nq in range(2):
                ps = self.pf[4 + nq]
                for kk in range(8):
                    k.mm(ps[:], aT[:, kk, :], wd[:, kk, nq * 512:(nq + 1) * 512], start=(kk == 0), stop=False)
                k.mm(ps[:], self.cB(C_E0), bd[:, nq * 512:(nq + 1) * 512], start=False, stop=True)
                k.copy(o[:, nq * 512:(nq + 1) * 512], ps[:], eng=("act" if nq == 0 else "dve"))
            k.dma("sp", S["obuf"][b * 128:(b + 1) * 128, :], o[:])
        k.release(m)

    def phase_combine(self, l, last):
        k, I, S = self.k, self.I, self.S
        m = k.mark()
        self.alloc_psum(0, 2)
        chunks = list(range(2 if last else 0, NCH))
        gk = [[k.sbuf("cb_g%d_%d" % (i, j), [128, D], BF16) for j in range(4)] for i in range(2)]
        cixs = [k.sbuf("cb_ix%d" % i, [128, 1], I32) for i in range(8)]
        f = k.sbuf("cb_f", [128, D], F32)
        fb = k.sbuf("cb_fb", [128, D], BF16)
        xts = [k.sbuf("cb_x%d" % i, [128, 8, 128], F32) for i in range(2)]
        tmp = k.sbuf("cb_tmp", [128, 8, 128], F32)
        xTv = S["xT"].rearrange("(k p) t -> p k t", p=128)
        for ci, c in enumerate(chunks):
            s = 1 if c < 2 else 0
            g4 = gk[ci % 2]
            xt = xts[ci % 2]
            k.dma("sp", xt[:], xTv[:, :, c * 128:(c + 1) * 128])
            for kq in range(4):
                ixt = cixs[(ci * 4 + kq) % 8]
                k.copy(ixt[:], self.dsts[:, c, kq:kq + 1])
                ix = ixt[:, 0:1]
                gt = g4[kq]
                k.dma_custom("pool", lambda e, ix=ix, gt=gt: e.indirect_dma_start(
                    out=gt[:], out_offset=None, in_=S["obuf"], in_offset=bass.IndirectOffsetOnAxis(ap=ix, axis=0)),
                    reads=[ix, S["obuf"]], writes=[gt[:]])
            k.ts(f[:], g4[0][:], self.gates[:, c, 0:1], ALU.mult)
            for kq in range(1, 4):
                k.stt(f[:] if kq < 3 else fb[:], g4[kq][:], self.gates[:, c, kq:kq + 1], f[:], ALU.mult, ALU.add)
            pt = self.pt[ci % 2]
            for kk in range(8):
                k.tr(pt[:, kk * 128:(kk + 1) * 128], fb[:, kk * 128:(kk + 1) * 128], self.cB(C_ID))
            k.tt(tmp[:], pt[:].rearrange("p (a t) -> p a t", a=8), bc(self.mod[:, 40:48, s], 2, 128), ALU.mult)
            k.tt(xt[:], xt[:], tmp[:], ALU.add)
            k.dma("sp", xTv[:, :, c * 128:(c + 1) * 128], xt[:])
        k.release(m)

    def phase_final(self):
        k, I, S = self.k, self.I, self.S
        m = k.mark()
        self.alloc_psum(4, 0)
        xts = [k.sbuf("fn_x%d" % i, [128, 8, 128], F32) for i in range(2)]
        sq = k.sbuf("fn_sq", [128, 8, 128], BF16)
        rstd = k.sbuf("fn_rstd", [128, 128], F32)
        xn = k.sbuf("fn_xn", [128, 8, 128], F32)
        ob = [k.sbuf("fn_o%d" % i, [128, D], F32) for i in range(2)]
        xTv = S["xT"].rearrange("(k p) t -> p k t", p=128)
        for c in range(2, NCH):
            xt = xts[c % 2]
            k.dma("sp", xt[:], xTv[:, :, c * 128:(c + 1) * 128])
            k.act(sq[:], xt[:], AF.Square)
            ps = self.pf[0]
            for kk in range(8):
                k.mm(ps[:, 0:128], self.cB(C_ONE), sq[:, kk, :], start=(kk == 0), stop=(kk == 7))
            k.act(rstd[:], ps[:, 0:128], AF.Sqrt, bias=self.epsb[:, 0:1], scale=1.0 / D)
            k.op("dve", lambda e: e.reciprocal(rstd[:], rstd[:]), reads=[rstd[:]], writes=[rstd[:]])
            for kk in range(8):
                k.stt(xn[:, kk, :], xt[:, kk, :], self.gv[:, kk:kk + 1], rstd[:], ALU.mult, ALU.mult,
                      eng="dve")
            o = ob[c % 2]
            for h in range(2):
                p = self.pf[1 + (c % 2) * 0 + h]
                for j in range(4):
                    kk = h * 4 + j
                    k.mm(p[:, j * 128:(j + 1) * 128], xn[:, kk, :], self.cF(C_ID))
                k.copy(o[:, h * 512:(h + 1) * 512], p[:], eng=("act" if h else "dve"))
            k.dma("pool", self.out[(c - 2) * 128:(c - 1) * 128, :], o[:])
        k.release(m)

    def build(self):
        k, I = self.k, self.I
        self.vec = k.sbuf("vec_sb", [128, NVEC], F32)
        self.rowp = k.sbuf("rowp_sb", [128, NROW], F32)
        self.mod = k.sbuf("mod", [128, 48, 2], F32)
        self.A1 = k.sbuf("A1", [128, 8, 2], F32)
        self.A2 = k.sbuf("A2", [128, 8, 2], F32)
        self.epsb = k.sbuf("epsb", [128, 1], F32)
        self.oneb = k.sbuf("oneb", [128, 1], F32)
        self.nm_tmp = [k.sbuf("nmtmp%d" % i, [128, 512], F32) for i in range(2)]
        self.iot = k.sbuf("iot_sb", [128, 257], F32)
        self.lgs = k.sbuf("lgs", [128, NCH, NEXP], F32)
        self.top8s = k.sbuf("top8s", [128, NCH, 8], F32)
        self.rks = k.sbuf("rks", [128, NCH, NEXP], F32)
        self.gates = k.sbuf("gates", [128, NCH, 4], F32)
        self.dsts = k.sbuf("dsts", [128, NCH, 4], I32)
        self.base = k.sbuf("base", [128, NEXP], F32)
        self.blkf = k.sbuf("blkf", [128, NBLK], F32)
        k.memset(self.epsb[:], EPS)
        k.memset(self.oneb[:], 1.0)
        self.ialb = k.sbuf("ialb", [128, 1], F32)
        k.memset(self.ialb[:], 1.0 / ALPHA)
        k.dma("sp", self.iot[:], I["iot"])
        stop = self.stop_after
        seq = [("x0", None)]
        for l in range(self.nlayers):
            for ph in ("ada", "inproj", "conv", "attn", "ssd", "outproj", "route", "moe", "combine"):
                seq.append((ph, l))
        seq.append(("final", None))
        for ph, l in seq:
            last = (l == DEPTH - 1)
            if ph == "x0":
                self.phase_x0()
            elif ph == "final":
                self.phase_final()
            elif ph == "ada":
                k.dma("sp", self.vec[:], I["vec"][l])
                k.dma("sp", self.rowp[:], I["rowp"][l])
                k.memset(self.base[:], 0.0)
                self.phase_ada(l)
            elif ph in ("inproj", "conv", "ada"):
                getattr(self, "phase_" + ph)(l)
            else:
                getattr(self, "phase_" + ph)(l, last)
            if stop is not None and (ph, l) == tuple(stop):
                break
        k.barrier()
        k.wait_all("sp")
        k.emit()
        k.close()
        return self.nc


def _host_consts():
    idn = np.eye(128, dtype=np.float32)
    perm = np.zeros((128, 128), np.float32)
    for p in range(128):
        perm[p, p + 16 if (p % 32) < 16 else p - 16] = 1.0
    kk = np.arange(128)
    U0 = (kk[:, None] <= kk[None, :]).astype(np.float32)
    U1 = (kk[:, None] >= kk[None, :]).astype(np.float32)
    L0 = (kk[:, None] > kk[None, :]).astype(np.float32)
    L1 = (kk[:, None] < kk[None, :]).astype(np.float32)
    ones = np.ones((128, 128), np.float32)
    e0 = np.zeros((128, 128), np.float32)
    e0[0, :] = 1.0
    cst = np.concatenate([idn, perm, U0, U1, L0, L1, ones, e0], axis=1)
    t = np.arange(S_LAT)
    pos = np.stack([t // 64, t % 64], axis=0).astype(np.float64)
    inv = 10000.0 ** (-np.arange(16, dtype=np.float64) / 16.0)
    rope = np.zeros((128, 2, S_LAT), np.float32)
    for p in range(128):
        d = p % 64
        a, r, f = d // 32, (d % 32) // 16, d % 16
        ang = pos[a] * inv[f]
        rope[p, 0] = np.cos(ang)
        rope[p, 1] = np.sin(ang) * (-1.0 if r == 0 else 1.0)
    iot = np.zeros((128, 257), np.float32)
    iot[:, 0:256] = np.arange(256, dtype=np.float32)[None, :]
    iot[:, 256] = np.arange(128, dtype=np.float32)
    return cst, rope, iot


def _fm(v, kcols):
    return np.ascontiguousarray(np.asarray(v, np.float32).reshape(kcols, 128).T)


def _host_layout(inputs):
    vec = np.zeros((DEPTH, 128, NVEC), np.float32)
    rowp = np.zeros((DEPTH, 128, NROW), np.float32)
    for l in range(DEPTH):
        vec[l, :, V_NMW:V_NMW + 8] = _fm(inputs["norm_mix_w"][l], 8)
        vec[l, :, V_NFW:V_NFW + 8] = _fm(inputs["norm_ffn_w"][l], 8)
        vec[l, :, V_BADA:V_BADA + 48] = _fm(inputs["b_ada"][l], 48)
        vec[l, :, V_CONVB:V_CONVB + 6] = _fm(inputs["conv_b"][l], 6)
        cw = np.asarray(inputs["conv_w"][l], np.float32)
        vec[l, :, V_CONVW:V_CONVW + 30] = cw.reshape(5, 6, 128).transpose(2, 1, 0).reshape(128, 30)
        vec[l, :, V_SNW:V_SNW + 4] = _fm(inputs["ssm_norm_w"][l], 4)
        vec[l, :, V_ANW:V_ANW + 4] = _fm(inputs["attn_norm_w"][l], 4)
        row = np.concatenate([np.asarray(inputs["dt_bias"][l], np.float32).reshape(16),
                              np.asarray(inputs["a_log"][l], np.float32).reshape(16),
                              np.asarray(inputs["d_skip"][l], np.float32).reshape(8),
                              np.asarray(inputs["attn_sinks"][l], np.float32).reshape(8),
                              np.asarray(inputs["b_router"][l], np.float32).reshape(32),
                              np.asarray(inputs["conv_b"][l], np.float32).reshape(768)])
        rowp[l] = np.broadcast_to(row[None, :], (128, NROW))
    return vec, rowp


_CACHE = {}


def kernel(**inputs):
    dbg = inputs.pop("_debug", None)
    inputs = {k_: np.asarray(v) for k_, v in inputs.items()}
    debug = tuple(dbg["dump"]) if dbg is not None else ()
    stop = dbg["stop"] if dbg is not None else None
    ncores = int(dbg.get("ncores", 8)) if dbg is not None else 8
    prog = Prog(debug=debug, stop_after=stop)
    nc = prog.build()
    cst, rope, iot = _host_consts()
    vec, rowp = _host_layout(inputs)
    f32 = lambda a: np.ascontiguousarray(np.asarray(a, np.float32))
    shared = {"w_ada": f32(inputs["w_ada"]), "w_in": f32(inputs["w_in"]), "w_out": f32(inputs["w_out"]),
              "w_router": f32(inputs["w_router"]), "w_gate_up": f32(inputs["w_gate_up"]).reshape(DEPTH * NEXP * D, 2 * D),
              "b_gate_up": f32(inputs["b_gate_up"]).reshape(DEPTH * NEXP, 2 * D),
              "w_down": f32(inputs["w_down"]).reshape(DEPTH * NEXP * D, D), "b_down": f32(inputs["b_down"]).reshape(DEPTH * NEXP, D),
              "vec": vec, "rowp": rowp, "cst": cst, "rope": rope, "iot": iot}
    in_maps = []
    for b in range(ncores):
        gvec = np.concatenate([_fm(inputs["final_norm_w"], 8), _fm(inputs["c"][b], 8), _fm(inputs["c_ctx"], 8)], axis=1)
        mp = dict(shared)
        mp["x"] = f32(inputs["x"][b]); mp["ctx"] = f32(inputs["ctx"][b]); mp["gvec"] = np.ascontiguousarray(gvec)
        in_maps.append(mp)
    res = run_bass_kernel_spmd(nc, in_maps, core_ids=list(range(ncores)))
    if dbg is not None:
        dbg["results"] = res.results
    out = np.stack([np.asarray(r["out"], np.float32) for r in res.results], axis=0)
    return out
```

```python
import numpy as np
import concourse.bass as bass
import concourse.mybir as mybir

F32 = mybir.dt.float32
BF16 = mybir.dt.bfloat16
I32 = mybir.dt.int32
AF = mybir.ActivationFunctionType
ALU = mybir.AluOpType
AX = mybir.AxisListType

ENGS = ("pe", "act", "dve", "pool", "sp")
NRING = 12


def _is_psum(ap):
    return type(ap.tensor).__name__ == "PSumTensorHandle"


def _region(ap):
    shape = list(ap.tensor.shape)
    if _is_psum(ap):
        return tuple((0, int(d) - 1) for d in shape)
    lo = int(ap.offset)
    hi = lo
    for step, cnt in ap.ap:
        step = int(step); cnt = int(cnt)
        if step >= 0:
            hi += step * (cnt - 1)
        else:
            lo += step * (cnt - 1)
    strides = [1] * len(shape)
    for i in range(len(shape) - 2, -1, -1):
        strides[i] = strides[i + 1] * int(shape[i + 1])
    reg = []
    l, h = lo, hi
    for i, s in enumerate(strides):
        a, l = divmod(l, s)
        b, h = divmod(h, s)
        if b < a or (i > 0 and reg and reg[-1][0] != reg[-1][1] and False):
            a, b = 0, int(shape[i]) - 1
        reg.append((a, b))
    for i in range(len(reg)):
        a, b = reg[i]
        if b < a:
            reg[i] = (0, int(shape[i]) - 1)
    return tuple(reg)


def _overlap(r1, r2):
    for (a0, a1), (b0, b1) in zip(r1, r2):
        if a1 < b0 or b1 < a0:
            return False
    return True


def _contains(outer, inner):
    for (a0, a1), (b0, b1) in zip(outer, inner):
        if b0 < a0 or b1 > a1:
            return False
    return True


class Rec:
    __slots__ = ("reg", "writer", "readers")

    def __init__(self, reg, writer, readers):
        self.reg = reg
        self.writer = writer
        self.readers = readers


class K:
    def __init__(self, nc):
        self.nc = nc
        self.ops = {e: [] for e in ENGS}
        self.cnt = {e: 0 for e in ENGS}
        self.clock = {e: {} for e in ENGS}
        self.recs = {}
        self.sems = {}
        self._ctx = []
        for e in ENGS:
            self.sems[e] = self._enter(nc.semaphore("s_" + e))
        self.ring = {}
        self.ring_pos = {}
        self.ring_val = {}
        for q in ("sp", "pool", "act"):
            self.ring[q] = []
            for i in range(NRING):
                key = "d_%s_%d" % (q, i)
                self.sems[key] = self._enter(nc.semaphore(key))
                self.ring[q].append(key)
            self.ring_pos[q] = 0
            self.ring_val[q] = [0] * NRING
        self.n_wait = 0
        self.tcount = 0

    def _enter(self, guard):
        v = guard.__enter__()
        self._ctx.append(guard)
        return v

    def sbuf(self, name, shape, dtype):
        self.tcount += 1
        return self._enter(self.nc.sbuf_tensor("%s_%d" % (name, self.tcount), list(shape), dtype))

    def psum(self, name, shape, dtype=F32):
        self.tcount += 1
        return self._enter(self.nc.psum_tensor("%s_%d" % (name, self.tcount), list(shape), dtype))

    def dram(self, name, shape, dtype, kind="Internal"):
        return self.nc.dram_tensor(name, list(shape), dtype, kind=kind)

    def _need(self, eng, ev, skip_self_pe):
        if ev is None:
            return
        key, val = ev
        if key == eng and eng == "pe" and skip_self_pe:
            return
        if self.clock[eng].get(key, 0) >= val:
            return
        self.clock[eng][key] = val
        sem = self.sems[key]
        self.ops[eng].append(("w", sem, val))
        self.n_wait += 1

    def _deps(self, eng, reads, writes):
        need = []
        for ap in reads:
            name = ap.tensor.name
            reg = _region(ap)
            for r in self.recs.get(name, ()):
                if r.writer is not None and _overlap(r.reg, reg):
                    need.append(r.writer)
        for ap in writes:
            name = ap.tensor.name
            reg = _region(ap)
            for r in self.recs.get(name, ()):
                if _overlap(r.reg, reg):
                    if r.writer is not None:
                        need.append(r.writer)
                    for k, v in r.readers.items():
                        need.append((k, v))
        return need

    def _record(self, ev, reads, writes):
        key, val = ev
        for ap in reads:
            name = ap.tensor.name
            reg = _region(ap)
            lst = self.recs.setdefault(name, [])
            for r in lst:
                if r.reg == reg:
                    if r.readers.get(key, 0) < val:
                        r.readers[key] = val
                    break
            else:
                lst.append(Rec(reg, None, {key: val}))
        for ap in writes:
            name = ap.tensor.name
            reg = _region(ap)
            lst = self.recs.setdefault(name, [])
            lst[:] = [r for r in lst if not _contains(reg, r.reg)]
            lst.append(Rec(reg, ev, {}))

    def op(self, eng, fn, reads=(), writes=()):
        writes = list(writes) + [ap for ap in reads if _is_psum(ap)]
        reads = [ap for ap in reads if not _is_psum(ap)]
        for ev in self._deps(eng, reads, writes):
            self._need(eng, ev, True)
        self.cnt[eng] += 1
        ev = (eng, self.cnt[eng])
        self.ops[eng].append(("o", fn, self.sems[eng], 1))
        self._record(ev, reads, writes)
        return ev

    def dma(self, q, out, in_, **kw):
        for ev in self._deps(q, [in_], [out]):
            self._need(q, ev, False)
        i = self.ring_pos[q]
        self.ring_pos[q] = (i + 1) % NRING
        key = self.ring[q][i]
        prev = self.ring_val[q][i]
        if prev:
            self._need(q, (key, prev), False)
        val = prev + 16
        self.ring_val[q][i] = val
        ev = (key, val)

        def fn(e, out=out, in_=in_, kw=kw):
            return e.dma_start(out=out, in_=in_, **kw)
        self.ops[q].append(("o", fn, self.sems[key], 16))
        self._record(ev, [in_], [out])
        return ev

    def dma_custom(self, q, fn, reads, writes):
        for ev in self._deps(q, reads, writes):
            self._need(q, ev, False)
        i = self.ring_pos[q]
        self.ring_pos[q] = (i + 1) % NRING
        key = self.ring[q][i]
        prev = self.ring_val[q][i]
        if prev:
            self._need(q, (key, prev), False)
        val = prev + 16
        self.ring_val[q][i] = val
        ev = (key, val)
        self.ops[q].append(("o", fn, self.sems[key], 16))
        self._record(ev, reads, writes)
        return ev

    def mark(self):
        return len(self._ctx)

    def pe_drain(self):
        if self.cnt["pe"]:
            self.ops["pe"].append(("w", self.sems["pe"], self.cnt["pe"]))

    def barrier(self):
        for e in ENGS:
            self.wait_all(e)
        self.recs.clear()

    def release(self, m):
        self.barrier()
        while len(self._ctx) > m:
            self._ctx.pop().__exit__(None, None, None)

    def wait_all(self, eng):
        for e in ENGS:
            if e != eng and self.cnt[e]:
                self._need(eng, (e, self.cnt[e]), False)
        for q in self.ring:
            for i, key in enumerate(self.ring[q]):
                if self.ring_val[q][i]:
                    self._need(eng, (key, self.ring_val[q][i]), False)

    def emit(self):
        nc = self.nc
        engobj = {"pe": "tensor", "act": "scalar", "dve": "vector", "pool": "gpsimd", "sp": "sync"}
        with nc.Block() as block:
            for e in ENGS:
                lst = self.ops[e]
                if not lst:
                    continue

                def body(eng, lst=lst):
                    for it in lst:
                        if it[0] == "w":
                            eng.wait_ge(it[1], it[2])
                        else:
                            it[1](eng).then_inc(it[2], it[3])
                getattr(block, engobj[e])(body)

    def close(self):
        for g in reversed(self._ctx):
            g.__exit__(None, None, None)

    def mm(self, out, lhsT, rhs, start=True, stop=True, **kw):
        return self.op("pe", lambda e: e.matmul(out, lhsT, rhs, start=start, stop=stop, **kw),
                       reads=[lhsT, rhs], writes=[out])

    def tr(self, out, in_, ident):
        return self.op("pe", lambda e: e.transpose(out, in_, ident), reads=[in_, ident], writes=[out])

    def act(self, out, in_, func, bias=None, scale=None, accum_out=None, eng="act"):
        kw = {}
        reads = [in_]
        writes = [out]
        if bias is not None:
            kw["bias"] = bias
            if not isinstance(bias, (int, float)):
                reads.append(bias)
        if scale is not None:
            kw["scale"] = scale
            if not isinstance(scale, (int, float)):
                reads.append(scale)
        if accum_out is not None:
            kw["accum_out"] = accum_out
            writes.append(accum_out)
        return self.op(eng, lambda e: e.activation(out, in_, func, **kw), reads=reads, writes=writes)

    def tt(self, out, in0, in1, op, eng="dve"):
        return self.op(eng, lambda e: e.tensor_tensor(out, in0, in1, op), reads=[in0, in1], writes=[out])

    def ts(self, out, in0, s1, op0, s2=None, op1=None, eng="dve", accum_out=None):
        reads = [in0]
        if not isinstance(s1, (int, float)):
            reads.append(s1)
        if s2 is not None and not isinstance(s2, (int, float)):
            reads.append(s2)
        writes = [out]
        kw = {}
        if accum_out is not None:
            kw["accum_out"] = accum_out
            writes.append(accum_out)
        if op1 is None:
            return self.op(eng, lambda e: e.tensor_scalar(out, in0, s1, None, op0, **kw), reads=reads, writes=writes)
        return self.op(eng, lambda e: e.tensor_scalar(out, in0, s1, s2, op0, op1, **kw), reads=reads, writes=writes)

    def stt(self, out, in0, scalar, in1, op0, op1, eng="dve"):
        reads = [in0, in1]
        if not isinstance(scalar, (int, float)):
            reads.append(scalar)
        return self.op(eng, lambda e: e.scalar_tensor_tensor(out, in0, scalar, in1, op0, op1),
                       reads=reads, writes=[out])

    def copy(self, out, in_, eng="dve"):
        if eng == "act":
            return self.op("act", lambda e: e.copy(out, in_), reads=[in_], writes=[out])
        return self.op(eng, lambda e: e.tensor_copy(out, in_), reads=[in_], writes=[out])

    def memset(self, ap, val, eng="dve"):
        return self.op(eng, lambda e: e.memset(ap, val), reads=[], writes=[ap])


from concourse.bass_utils import run_bass_kernel_spmd

T = 4352
NCH = 34
CTX = 256
S_LAT = 4096
D = 1024
DEPTH = 2
NEXP = 32
EPS = 1e-6
ALPHA = 1.702
TILES = [(0, 256, 1)] + [(256 + 512 * i, 512, 0) for i in range(8)]
NBLK = (T * 4) // 128 + NEXP
V_NMW, V_NFW, V_BADA, V_CONVB, V_CONVW, V_SNW, V_ANW = 0, 8, 16, 64, 70, 100, 104
NVEC = 108
R_DTB, R_ALOG, R_DSKIP, R_SINK, R_BR = 0, 16, 32, 40, 48
R_CONVB = 80
NROW = 848
C_ID, C_PERM, C_U0, C_U1, C_L0, C_L1, C_ONE, C_E0 = 0, 128, 256, 384, 512, 640, 768, 896
NCST = 1024


def bc(ap, axis, n):
    shp = list(ap.shape)
    shp.insert(axis, n)
    return ap.unsqueeze(axis).broadcast_to(shp)


class Prog:
    def __init__(self, debug=None, nlayers=DEPTH, stop_after=None):
        self.debug = debug or ()
        self.nlayers = nlayers
        self.stop_after = stop_after
        nc = bass.Bass("TRN2", target_bir_lowering=False)
        self.nc = nc
        self.k = K(nc)
        k = self.k
        I = {}

        def inp(name, shape):
            I[name] = nc.dram_tensor(name, list(shape), F32, kind="ExternalInput").ap()
        inp("x", [S_LAT, D]); inp("ctx", [CTX, D])
        inp("w_ada", [DEPTH, D, 6 * D]); inp("w_in", [DEPTH, D, 2064]); inp("w_out", [DEPTH, D, D])
        inp("w_router", [DEPTH, D, NEXP]); inp("w_gate_up", [DEPTH * NEXP * D, 2 * D])
        inp("b_gate_up", [DEPTH * NEXP, 2 * D]); inp("w_down", [DEPTH * NEXP * D, D]); inp("b_down", [DEPTH * NEXP, D])
        inp("vec", [DEPTH, 128, NVEC]); inp("gvec", [128, 24]); inp("rowp", [DEPTH, 128, NROW])
        inp("cst", [128, NCST]); inp("rope", [128, 2, S_LAT]); inp("iot", [128, 257])
        self.I = I
        self.out = nc.dram_tensor("out", [S_LAT, D], F32, kind="ExternalOutput").ap()
        self.S = {}

        def scr(name, shape, dt):
            kind = "ExternalOutput" if name in self.debug else "Internal"
            self.S[name] = nc.dram_tensor("s_" + name, list(shape), dt, kind=kind).ap()
        scr("xT", [D, T], F32)
        scr("qT", [512, T], BF16); scr("kT", [128, T], BF16); scr("v", [T, 128], BF16)
        scr("zs", [T, 512], BF16); scr("dt", [T, 16], F32); scr("xbcT", [768, T], BF16)
        scr("xs", [T, 512], BF16); scr("Bt", [T, 128], BF16); scr("BT", [128, T], BF16); scr("CT", [128, T], BF16)
        scr("yf", [T, 512], F32); scr("mixT", [D, T], BF16)
        if "yb" in self.debug:
            scr("yb", [T, 512], F32)
        if "d_dsts" in self.debug:
            scr("d_dsts", [128, NCH * 4], I32); scr("d_blkf", [128, NBLK], F32); scr("d_base", [128, NEXP], F32)
            scr("d_gates", [128, NCH * 4], F32); scr("d_pend", [128, NEXP], F32)
        if "dg1" in self.debug:
            scr("dg1", [T, 512], F32); scr("dg2", [T, 512], F32); scr("dg3", [T, 512], BF16)
        scr("h2", [T, D], BF16); scr("gat", [T, 4], F32); scr("dst", [T, 4], I32)
        scr("buf", [NBLK * 128, D], BF16); scr("obuf", [NBLK * 128, D], BF16)
        scr("blke", [1, 2 * NBLK], I32)
        self.psum_gen = 0
        self.pool_regs = {}
        self.cf = k.sbuf("cstf", [128, NCST], F32)
        self.cb = k.sbuf("cstb", [128, NCST], BF16)
        self.gv = k.sbuf("gvec_sb", [128, 24], F32)
        k.dma("sp", self.cf[:], I["cst"])
        k.dma("pool", self.cb[:], I["cst"])
        k.dma("sp", self.gv[:], I["gvec"])

    def alloc_psum(self, nf, nt):
        k = self.k
        self.psum_gen += 1
        self.pf = [k.psum("pf%d_%d" % (self.psum_gen, i), [128, 512], F32) for i in range(nf)]
        self.pt = [k.psum("pt%d_%d" % (self.psum_gen, i), [128, 1024], BF16) for i in range(nt)]

    def cB(self, off, n=128, p=128):
        return self.cb[0:p, off:off + n]

    def cF(self, off, n=128, p=128):
        return self.cf[0:p, off:off + n]

    def phase_x0(self):
        k, I, S = self.k, self.I, self.S
        m = k.mark()
        self.alloc_psum(2, 0)
        xin = [k.sbuf("x0in%d" % i, [128, D], F32) for i in range(2)]
        xo = [k.sbuf("x0o%d" % i, [128, 8, 128], F32) for i in range(2)]
        xTv = S["xT"].rearrange("(k p) t -> p k t", p=128)
        for c in range(NCH):
            src = I["ctx"][c * 128:(c + 1) * 128, :] if c < 2 else I["x"][(c - 2) * 128:(c - 1) * 128, :]
            xi = xin[c % 2]; o = xo[c % 2]
            k.dma("sp", xi[:], src)
            for h in range(2):
                p = self.pf[h]
                for j in range(4):
                    kk = h * 4 + j
                    k.mm(p[:, j * 128:(j + 1) * 128], xi[:, kk * 128:(kk + 1) * 128], self.cF(C_ID))
                k.copy(o[:, h * 4:(h + 1) * 4, :], p[:].rearrange("p (j t) -> p j t", j=4), eng=("dve" if h else "act"))
            k.dma("pool", xTv[:, :, c * 128:(c + 1) * 128], o[:])
        k.release(m)

    def phase_ada(self, l):
        k, I = self.k, self.I
        vec = self.vec
        m = k.mark()
        self.alloc_psum(1, 0)
        sc = k.sbuf("ada_sc", [128, 8, 2], F32)
        k.act(sc[:, :, 0], self.gv[:, 8:16], AF.Silu)
        k.act(sc[:, :, 1], self.gv[:, 16:24], AF.Silu)
        wa = [k.sbuf("ada_w%d" % i, [128, 8, 512], F32) for i in range(2)]
        pm = self.pf[0]
        pmv = pm[:, 0:96].rearrange("p (j s) -> p j s", s=2)
        wv = I["w_ada"][l].rearrange("(k p) n -> p k n", p=128)
        for g in range(12):
            w = wa[g % 2]
            k.dma("sp", w[:], wv[:, :, g * 512:(g + 1) * 512])
            for jj in range(4):
                j = g * 4 + jj
                for kk in range(8):
                    k.mm(pmv[:, j, :], w[:, kk, jj * 128:(jj + 1) * 128], sc[:, kk, :], start=(kk == 0), stop=(kk == 7))
        mod = self.mod
        k.tt(mod[:], pmv, bc(vec[:, V_BADA:V_BADA + 48], 2, 2), ALU.add)
        for (A, base, nw) in ((self.A1, 8, V_NMW), (self.A2, 32, V_NFW)):
            k.ts(A[:], mod[:, base:base + 8, :], 1.0, ALU.add)
            k.tt(A[:], A[:], bc(vec[:, nw:nw + 8], 2, 2), ALU.mult)
        k.release(m)

    def norm_mod(self, xt, n, s, A, Boff, hT, sq, rstd, ps):
        k = self.k
        k.act(sq[:, :, 0:n], xt[:, :, 0:n], AF.Square)
        for kk in range(8):
            k.mm(ps[:, 0:n], self.cB(C_ONE), sq[:, kk, 0:n], start=(kk == 0), stop=(kk == 7))
        k.act(rstd[:, 0:n], ps[:, 0:n], AF.Sqrt, bias=self.epsb[:, 0:1], scale=1.0 / D)
        k.op("dve", lambda e: e.reciprocal(rstd[:, 0:n], rstd[:, 0:n]), reads=[rstd[:, 0:n]], writes=[rstd[:, 0:n]])
        for kk in range(8):
            tp = self.nm_tmp[kk % 2]
            k.stt(tp[:, 0:n], xt[:, kk, 0:n], A[:, kk, s:s + 1], rstd[:, 0:n], ALU.mult, ALU.mult)
            k.act(hT[:, kk, 0:n], tp[:, 0:n], AF.Identity, bias=self.mod[:, Boff + kk, s:s + 1], scale=1.0)

    def phase_inproj(self, l):
        k, I, S = self.k, self.I, self.S
        m = k.mark()
        self.alloc_psum(6, 0)
        win = k.sbuf("win", [128, 8, 2064], BF16)
        wv = I["w_in"][l].rearrange("(k p) n -> p k n", p=128)
        for kk in range(8):
            for c0_ in (0, 1024, 2048):
                c1_ = min(c0_ + 1024, 2064)
                k.dma("pool", win[:, kk, c0_:c1_], wv[:, kk, c0_:c1_])
        rope = k.sbuf("rope_sb", [128, 2, S_LAT], F32)
        k.dma("sp", rope[:], I["rope"])
        xts = [k.sbuf("ip_x%d" % i, [128, 8, 512], F32) for i in range(2)]
        hTs = [k.sbuf("ip_h%d" % i, [128, 8, 512], BF16) for i in range(2)]
        sq = k.sbuf("ip_sq", [128, 8, 512], BF16)
        rstd = k.sbuf("ip_rstd", [128, 512], F32)
        qb = [k.sbuf("ip_qb%d" % i, [128, 512], BF16) for i in range(2)]
        t1 = [k.sbuf("ip_t1%d" % i, [128, 512], F32) for i in range(2)]
        t2 = [k.sbuf("ip_t2%d" % i, [128, 512], F32) for i in range(2)]
        ob = [k.sbuf("ip_ob%d" % i, [128, 512], BF16) for i in range(3)]
        zsb = [k.sbuf("ip_zs%d" % i, [128, 512], BF16) for i in range(2)]
        vsb = [k.sbuf("ip_v%d" % i, [128, 128], BF16) for i in range(2)]
        dtt = [k.sbuf("ip_dt%d" % i, [128, 16], F32) for i in range(2)]
        xTv = S["xT"].rearrange("(k p) t -> p k t", p=128)
        cnt = 0
        for ti, (t0, n, s) in enumerate(TILES):
            xt = xts[ti % 2]; hT = hTs[ti % 2]
            k.dma("sp", xt[:, :, 0:n], xTv[:, :, t0:t0 + n])
            self.norm_mod(xt, n, s, self.A1, 0, hT, sq, rstd, self.pf[5])
            fm = [("q", c, c * 128) for c in range(4)] + [("k", 0, 512)] + [("xbc", c, 1280 + c * 128) for c in range(6)]
            for (nm, c, col) in fm:
                ps = self.pf[cnt % 2]; cnt += 1
                for kk in range(8):
                    k.mm(ps[:, 0:n], win[:, kk, col:col + 128], hT[:, kk, 0:n], start=(kk == 0), stop=(kk == 7))
                o = ob[cnt % 3]
                if nm == "xbc":
                    k.copy(o[:, 0:n], ps[:, 0:n], eng="act")
                    k.dma("pool", S["xbcT"][c * 128:(c + 1) * 128, t0:t0 + n], o[:, 0:n])
                    continue
                dst = S["qT"][c * 128:(c + 1) * 128, t0:t0 + n] if nm == "q" else S["kT"][:, t0:t0 + n]
                if s == 1:
                    k.copy(o[:, 0:n], ps[:, 0:n], eng="act")
                else:
                    q_ = qb[cnt % 2]; t_ = t1[cnt % 2]
                    p0 = t0 - CTX
                    k.copy(q_[:, 0:n], ps[:, 0:n], eng="act")
                    pp = self.pf[2 + cnt % 2]
                    k.mm(pp[:, 0:n], self.cB(C_PERM), q_[:, 0:n])
                    t2_ = t2[cnt % 2]
                    k.tt(t_[:, 0:n], q_[:, 0:n], rope[:, 0, p0:p0 + n], ALU.mult)
                    k.tt(t2_[:, 0:n], pp[:, 0:n], rope[:, 1, p0:p0 + n], ALU.mult)
                    k.tt(o[:, 0:n], t_[:, 0:n], t2_[:, 0:n], ALU.add)
                k.dma("pool", dst, o[:, 0:n])
            for j in range(n // 128):
                c0 = t0 + j * 128
                pz = self.pf[4]; pv = self.pf[2 + j % 2]
                for kk in range(8):
                    k.mm(pz[:, :], hT[:, kk, j * 128:(j + 1) * 128], win[:, kk, 768:1280], start=(kk == 0), stop=(kk == 7))
                for kk in range(8):
                    k.mm(pv[:, 0:128], hT[:, kk, j * 128:(j + 1) * 128], win[:, kk, 640:768], start=(kk == 0), stop=(kk == 7))
                for kk in range(8):
                    k.mm(pv[:, 128:144], hT[:, kk, j * 128:(j + 1) * 128], win[:, kk, 2048:2064], start=(kk == 0), stop=(kk == 7))
                z_ = zsb[j % 2]; v_ = vsb[j % 2]; d_ = dtt[j % 2]
                k.act(z_[:], pz[:], AF.Silu)
                k.dma("pool", S["zs"][c0:c0 + 128, :], z_[:])
                k.copy(v_[:], pv[:, 0:128], eng="dve")
                k.dma("pool", S["v"][c0:c0 + 128, :], v_[:])
                k.tt(d_[:], pv[:, 128:144], self.rowp[:, R_DTB:R_DTB + 16], ALU.add)
                k.act(d_[:], d_[:], AF.Exp)
                k.act(d_[:], d_[:], AF.Ln, bias=self.oneb[:, 0:1], scale=1.0)
                k.dma("pool", S["dt"][c0:c0 + 128, :], d_[:])
        k.release(m)

    def phase_conv(self, l):
        k, I, S = self.k, self.I, self.S
        vec = self.vec
        m = k.mark()
        self.alloc_psum(6, 0)
        diag = k.sbuf("cv_diag", [128, 6, 5, 128], BF16)
        for c in range(6):
            for j in range(5):
                k.ts(diag[:, c, j, :], self.cF(C_ID), vec[:, V_CONVW + c * 5 + j:V_CONVW + c * 5 + j + 1], ALU.mult,
                     eng="dve")
        cbrow = k.sbuf("cv_cbrow", [128, 768], BF16)
        k.copy(cbrow[:], self.rowp[:, R_CONVB:R_CONVB + 768])
        xins = [k.sbuf("cv_xin%d" % i, [128, 6, 516], BF16) for i in range(2)]
        ofm = [k.sbuf("cv_ofm%d" % i, [128, 512], BF16) for i in range(2)]
        oxs = [k.sbuf("cv_oxs%d" % i, [128, 512], BF16) for i in range(2)]
        obt = [k.sbuf("cv_obt%d" % i, [128, 128], BF16) for i in range(2)]
        xv = S["xbcT"].rearrange("(c p) t -> p c t", p=128)
        cnt = 0
        for ti, (t0, n, s) in enumerate(TILES):
            xin = xins[ti % 2]
            s0, s1 = (0, CTX) if s == 1 else (CTX, T)
            lo, hi = t0 - 2, t0 + n + 2
            clo, chi = max(lo, s0), min(hi, s1)
            if lo < s0:
                k.memset(xin[:, :, 0:2], 0.0)
            if hi > s1:
                k.memset(xin[:, :, n + 2:n + 4], 0.0)
            k.dma("sp", xin[:, :, clo - lo:chi - lo], xv[:, :, clo:chi])
            for c in (4, 5):
                ps = self.pf[cnt % 2]; o = ofm[cnt % 2]; cnt += 1
                for j in range(5):
                    k.mm(ps[:, 0:n], diag[:, c, j, :], xin[:, c, j:j + n], start=(j == 0), stop=(j == 4))
                k.act(o[:, 0:n], ps[:, 0:n], AF.Silu, bias=vec[:, V_CONVB + c:V_CONVB + c + 1], scale=1.0)
                k.dma("pool", (S["BT"] if c == 4 else S["CT"])[:, t0:t0 + n], o[:, 0:n])
            for jt in range(n // 128):
                c0 = t0 + jt * 128
                pa = self.pf[2 + jt % 2]; pb = self.pf[4 + jt % 2]
                for c in range(5):
                    dstp = pa[:, c * 128:(c + 1) * 128] if c < 4 else pb[:, 0:128]
                    for j in range(5):
                        k.mm(dstp, xin[:, c, jt * 128 + j:jt * 128 + j + 128], diag[:, c, j, :], start=(j == 0), stop=False)
                    k.mm(dstp, self.cB(C_E0), cbrow[:, c * 128:(c + 1) * 128], start=False, stop=True)
                ox = oxs[jt % 2]; ob = obt[jt % 2]
                k.act(ox[:], pa[:], AF.Silu)
                k.dma("pool", S["xs"][c0:c0 + 128, :], ox[:])
                k.act(ob[:], pb[:, 0:128], AF.Silu)
                k.dma("pool", S["Bt"][c0:c0 + 128, :], ob[:])
        k.release(m)

    def rms_to_mixT(self, src, ngrp, gsz, nwoff, row0, c, tg):
        k, S = self.k, self.S
        ssq, rs, gn, mixc, junk = tg
        for g in range(ngrp):
            k.act(junk[:, 0:gsz], src[:, g * gsz:(g + 1) * gsz], AF.Square, accum_out=ssq[:, g:g + 1])
        k.act(rs[:, 0:ngrp], ssq[:, 0:ngrp], AF.Sqrt, bias=self.epsb[:, 0:1], scale=1.0 / gsz)
        k.op("dve", lambda e: e.reciprocal(rs[:, 0:ngrp], rs[:, 0:ngrp]), reads=[rs[:, 0:ngrp]], writes=[rs[:, 0:ngrp]])
        k.tt(gn[:].rearrange("p (g f) -> p g f", g=ngrp), src[:].rearrange("p (g f) -> p g f", g=ngrp),
             bc(rs[:, 0:ngrp], 2, gsz), ALU.mult)
        pt = self.pt[c % len(self.pt)]
        k.tr(pt[:, 512:640], gn[:, 0:128], self.cB(C_ID))
        for kk in range(4):
            k.tr(pt[:, kk * 128:(kk + 1) * 128], gn[:, kk * 128:(kk + 1) * 128], self.cB(C_ID))
        for kk in range(4):
            k.act(mixc[:, kk, :], pt[:, kk * 128:(kk + 1) * 128], AF.Copy, scale=self.vec[:, nwoff + kk:nwoff + kk + 1])
        mv = S["mixT"].rearrange("(k p) t -> p k t", p=128)
        k.dma("pool", mv[:, row0 // 128:row0 // 128 + 4, c * 128:(c + 1) * 128], mixc[:])

    def phase_attn(self, l, last):
        k, I, S = self.k, self.I, self.S
        m = k.mark()
        self.alloc_psum(4, 2)
        Kt = k.sbuf("at_K", [128, 2, T], BF16)
        Qt = k.sbuf("at_Q", [128, 8, T], BF16)
        k.memset(Kt[:], 0.0)
        k.memset(Qt[:], 0.0)
        Va = k.sbuf("at_V", [128, NCH, 2, 65], BF16)
        esk = k.sbuf("at_es", [128, 8], F32)
        for g in range(2):
            k.dma("sp", Kt[0:64, g, :], S["kT"][g * 64:(g + 1) * 64, :])
        for h in range(8):
            k.dma("sp", Qt[0:64, h, :], S["qT"][h * 64:(h + 1) * 64, :])
        k.memset(Va[:], 1.0)
        for c in range(NCH):
            k.dma("sp", Va[:, c, :, 0:64], S["v"][c * 128:(c + 1) * 128, :].rearrange("p (g d) -> p g d", g=2))
        k.act(esk[:], self.rowp[:, R_SINK:R_SINK + 8], AF.Exp)
        ET = [[k.sbuf("at_E%d_%d" % (i, j), [128, 4, 128], BF16) for j in range(5)] for i in range(2)]
        attn = [k.sbuf("at_o%d" % i, [128, 512], F32) for i in range(2)]
        den = k.sbuf("at_den", [128, 4], F32)
        tg = (k.sbuf("at_ssq", [128, 2], F32), k.sbuf("at_rs", [128, 2], F32), k.sbuf("at_gn", [128, 512], BF16),
              k.sbuf("at_mix", [128, 4, 128], BF16), k.sbuf("at_junk", [128, 512], F32))
        blocks = list(range(0 if not last else 2, NCH))
        it = 0
        for c in blocks:
            if c < 2:
                keys = [(0, None), (1, None)]
            else:
                keys = [(0, None), (1, None)]
                if c > 2:
                    keys.append((c - 1, C_U1))
                keys.append((c, None))
                if c < NCH - 1:
                    keys.append((c + 1, C_U0))
            at = attn[c % 2]
            for g in range(2):
                ets = ET[it % 2]
                pv = self.pf[2 + it % 2]
                pvv = pv[:].rearrange("p (r d) -> p r d", r=4)
                for idx, (kc, mk) in enumerate(keys):
                    ps = self.pf[idx % 2]
                    psv = ps[:].rearrange("p (r q) -> p r q", r=4)
                    k.mm(psv, Kt[:, g, kc * 128:(kc + 1) * 128], Qt[:, 4 * g:4 * g + 4, c * 128:(c + 1) * 128])
                    k.act(ets[idx][:], psv, AF.Exp, scale=0.125)
                    if mk is not None:
                        k.tt(ets[idx][:], ets[idx][:], bc(self.cB(mk), 1, 4), ALU.mult)
                for r in range(4):
                    for idx, (kc, mk) in enumerate(keys):
                        k.mm(pvv[:, r, 0:65], ets[idx][:, r, :], Va[:, kc, g, :], start=(idx == 0), stop=(idx == len(keys) - 1))
                k.tt(den[:], pvv[:, :, 64], esk[:, 4 * g:4 * g + 4], ALU.add)
                k.op("dve", lambda e: e.reciprocal(den[:], den[:]), reads=[den[:]], writes=[den[:]])
                k.tt(at[:, g * 256:(g + 1) * 256].rearrange("p (r d) -> p r d", r=4), pvv[:, :, 0:64], bc(den[:], 2, 64), ALU.mult)
                it += 1
            self.rms_to_mixT(at, 1, 512, V_ANW, 0, c, tg)
        k.release(m)

    def phase_ssd(self, l, last):
        k, I, S = self.k, self.I, self.S
        m = k.mark()
        self.alloc_psum(6, 2)
        rowp = self.rowp
        abc = k.sbuf("sd_a", [128, 16], F32)
        k.act(abc[:], rowp[:, R_ALOG:R_ALOG + 16], AF.Exp)
        k.ts(abc[:], abc[:], -1.0, ALU.mult)
        xsb = [k.sbuf("sd_xs%d" % i, [128, 512], BF16) for i in range(2)]
        btb = [k.sbuf("sd_bt%d" % i, [128, 128], BF16) for i in range(2)]
        BTc = [k.sbuf("sd_BT%d" % i, [128, 256], BF16) for i in range(2)]
        CTc = [k.sbuf("sd_CT%d" % i, [128, 256], BF16) for i in range(2)]
        for t_ in BTc + CTc:
            k.memset(t_[:], 0.0)
        dtb = [k.sbuf("sd_dt%d" % i, [128, 16], F32) for i in range(2)]
        yfb = [k.sbuf("sd_yf%d" % i, [128, 512], F32) for i in range(2)]
        zsb = [k.sbuf("sd_zs%d" % i, [128, 512], BF16) for i in range(2)]
        dta = k.sbuf("sd_dta", [128, 8], F32)
        DU = k.sbuf("sd_DU", [128, 1024], BF16)
        E = k.sbuf("sd_E", [128, 1024], BF16)
        GM = k.sbuf("sd_GM", [128, 256], BF16)
        ST = k.sbuf("sd_ST", [128, 1024], BF16)
        xdt = k.sbuf("sd_xdt", [128, 512], BF16)
        xw = k.sbuf("sd_xw", [128, 512], BF16)
        ecs = k.sbuf("sd_ecs", [128, 8], F32)
        dec = k.sbuf("sd_dec", [128, 8], F32)
        tmp = k.sbuf("sd_tmp", [128, 512], F32)
        ys = [k.sbuf("sd_y%d" % i, [128, 512], F32) for i in range(2)]
        H = k.sbuf("sd_H", [128, 256], F32)
        Hbf = k.sbuf("sd_Hbf", [128, 256], BF16)
        gg = k.sbuf("sd_g", [128, 512], F32)
        tg = (k.sbuf("sd_ssq", [128, 2], F32), k.sbuf("sd_rs", [128, 2], F32), k.sbuf("sd_gn", [128, 512], BF16),
              k.sbuf("sd_mix", [128, 4, 128], BF16), k.sbuf("sd_junk", [128, 512], F32))
        pE0, pE1, pG, pY, pYO, pS7 = self.pf
        pX = pG[:, 256:512]

        def v3(ap, h):
            return ap.rearrange("p (h q) -> p h q", h=h)

        def v4(ap):
            return ap.rearrange("p (g r i) -> p g r i", g=2, r=4)
        for d in range(2):
            k.memset(H[:], 0.0)
            k.memset(Hbf[:], 0.0)
            order = list(range(NCH)) if d == 0 else [1, 0] + list(range(NCH - 1, 1, -1))
            cU = C_U0 if d == 0 else C_U1
            cL = C_L0 if d == 0 else C_L1
            iend = 127 if d == 0 else 0
            for it, c in enumerate(order):
                c0 = c * 128
                xs_, bt_, BT_, CT_, dt_ = xsb[it % 2], btb[it % 2], BTc[it % 2], CTc[it % 2], dtb[it % 2]
                k.dma("sp", xs_[:], S["xs"][c0:c0 + 128, :])
                k.dma("sp", bt_[:], S["Bt"][c0:c0 + 128, :])
                for g in range(2):
                    k.dma("sp", BT_[g * 64:(g + 1) * 64, g * 128:(g + 1) * 128], S["BT"][g * 64:(g + 1) * 64, c0:c0 + 128])
                    k.dma("sp", CT_[g * 64:(g + 1) * 64, g * 128:(g + 1) * 128], S["CT"][g * 64:(g + 1) * 64, c0:c0 + 128])
                k.dma("sp", dt_[:], S["dt"][c0:c0 + 128, :])
                if d == 1:
                    yf_, zs_ = yfb[it % 2], zsb[it % 2]
                    k.dma("sp", yf_[:], S["yf"][c0:c0 + 128, :])
                    k.dma("sp", zs_[:], S["zs"][c0:c0 + 128, :])
                dtd = dt_[:, d * 8:(d + 1) * 8]
                k.tt(dta[:], dtd, abc[:, d * 8:(d + 1) * 8], ALU.mult)
                k.tt(v3(DU[:], 8), bc(self.cB(cU), 1, 8), bc(dta[:], 2, 128), ALU.mult)
                k.mm(pE0[:], self.cB(cL), DU[:, 0:512])
                k.mm(pE1[:], self.cB(cL), DU[:, 512:1024])
                k.act(E[:, 0:512], pE0[:], AF.Exp)
                k.act(E[:, 512:1024], pE1[:], AF.Exp)
                k.mm(pX[:, 0:8], self.cF(cU), dta[:])
                k.mm(pX[:, 8:16], self.cF(C_ONE), dta[:])
                k.act(ecs[:], pX[:, 0:8], AF.Exp)
                k.act(dec[:], pX[:, 8:16], AF.Exp)
                for g in range(2):
                    k.mm(pG[:, g * 128:(g + 1) * 128], BT_[:, g * 128:(g + 1) * 128], CT_[:, g * 128:(g + 1) * 128])
                k.tt(v3(GM[:], 2), v3(pG[:, 0:256], 2), bc(self.cB(cU), 1, 2), ALU.mult)
                k.tt(v4(ST[:]), v4(E[:]), bc(v3(GM[:], 2), 2, 4), ALU.mult)
                k.tt(v3(xdt[:], 8), v3(xs_[:], 8), bc(dtd, 2, 64), ALU.mult)
                k.tt(v3(xw[:], 8), v3(xdt[:], 8), bc(v3(E[:], 8)[:, :, iend], 2, 64), ALU.mult)
                for h in range(8):
                    k.mm(pY[:, h * 64:(h + 1) * 64], ST[:, h * 128:(h + 1) * 128], xdt[:, h * 64:(h + 1) * 64])
                for g in range(2):
                    k.mm(pYO[:, g * 256:(g + 1) * 256], CT_[:, g * 128:(g + 1) * 128], Hbf[:, :])
                y = ys[it % 2]
                k.tt(v3(tmp[:], 8), v3(pYO[:], 8), bc(ecs[:], 2, 64), ALU.mult)
                k.tt(y[:], pY[:], tmp[:], ALU.add)
                for g in range(2):
                    k.mm(pS7[:, g * 256:(g + 1) * 256], bt_[:, :], xw[:, g * 256:(g + 1) * 256])
                for g in range(2):
                    gs = slice(g * 64, (g + 1) * 64)
                    k.tt(v3(H[gs, :], 4), v3(H[gs, :], 4), bc(dec[gs, 4 * g:4 * g + 4], 2, 64), ALU.mult)
                    k.tt(H[gs, :], H[gs, :], pS7[gs, g * 256:(g + 1) * 256], ALU.add)
                k.copy(Hbf[:], H[:], eng="act")
                if d == 0:
                    k.dma("pool", S["yf"][c0:c0 + 128, :], y[:])
                else:
                    if "yb" in self.debug:
                        k.dma("pool", S["yb"][c0:c0 + 128, :], y[:])
                    k.tt(y[:], y[:], yf_[:], ALU.add)
                    k.tt(v3(tmp[:], 8), v3(xs_[:], 8), bc(rowp[:, R_DSKIP:R_DSKIP + 8], 2, 64), ALU.mult)
                    k.tt(y[:], y[:], tmp[:], ALU.add)
                    if "dg1" in self.debug:
                        k.dma("pool", S["dg1"][c0:c0 + 128, :], y[:])
                    k.tt(gg[:], y[:], zs_[:], ALU.mult)
                    if "dg1" in self.debug:
                        k.dma("pool", S["dg2"][c0:c0 + 128, :], gg[:])
                    self.rms_to_mixT(gg, 2, 256, V_SNW, 512, c, tg)
                    if "dg1" in self.debug:
                        k.dma("pool", S["dg3"][c0:c0 + 128, :], tg[2][:])
        k.release(m)

    def phase_outproj(self, l, last):
        k, I, S = self.k, self.I, self.S
        m = k.mark()
        self.alloc_psum(6, 2)
        wout = k.sbuf("op_w", [128, 8, D], BF16)
        wv = I["w_out"][l].rearrange("(k p) n -> p k n", p=128)
        for kk in range(8):
            k.dma("pool", wout[:, kk, :], wv[:, kk, :])
        wr = k.sbuf("op_wr", [128, 8, NEXP], F32)
        k.dma("sp", wr[:], I["w_router"][l].rearrange("(k p) e -> p k e", p=128))
        mixs = [k.sbuf("op_mix%d" % i, [128, 8, 512], BF16) for i in range(2)]
        xts = [k.sbuf("op_x%d" % i, [128, 8, 512], F32) for i in range(2)]
        h2f = k.sbuf("op_h2f", [128, 8, 512], F32)
        h2b = k.sbuf("op_h2b", [128, 8, 512], BF16)
        sq = k.sbuf("op_sq", [128, 8, 512], BF16)
        rstd = k.sbuf("op_rstd", [128, 512], F32)
        rows = [k.sbuf("op_rows%d" % i, [128, D], BF16) for i in range(2)]
        negm = k.sbuf("op_negm", [128, 1], F32)
        e4 = k.sbuf("op_e4", [128, 4], F32)
        gsum = k.sbuf("op_gsum", [128, 1], F32)
        maskb = k.sbuf("op_mask", [128, NEXP], BF16)
        xTv = S["xT"].rearrange("(k p) t -> p k t", p=128)
        mTv = S["mixT"].rearrange("(k p) t -> p k t", p=128)
        for ti, (t0, n, s) in enumerate(TILES):
            if last and s == 1:
                continue
            mix = mixs[ti % 2]; xt = xts[ti % 2]
            k.dma("sp", mix[:, :, 0:n], mTv[:, :, t0:t0 + n])
            k.dma("sp", xt[:, :, 0:n], xTv[:, :, t0:t0 + n])
            for mc in range(8):
                ps = self.pf[mc % 2]
                for kk in range(8):
                    k.mm(ps[:, 0:n], wout[:, kk, mc * 128:(mc + 1) * 128], mix[:, kk, 0:n], start=(kk == 0), stop=(kk == 7))
                k.stt(xt[:, mc, 0:n], ps[:, 0:n], self.mod[:, 16 + mc, s:s + 1], xt[:, mc, 0:n], ALU.mult, ALU.add)
            k.dma("pool", xTv[:, :, t0:t0 + n], xt[:, :, 0:n])
            self.norm_mod(xt, n, s, self.A2, 24, h2f, sq, rstd, self.pf[5])
            k.copy(h2b[:, :, 0:n], h2f[:, :, 0:n], eng="act")
            for j in range(n // 128):
                c = (t0 + j * 128) // 128
                pl = self.pf[2 + j % 2]
                for kk in range(8):
                    k.mm(pl[:, 0:NEXP], h2f[:, kk, j * 128:(j + 1) * 128], wr[:, kk, :], start=(kk == 0), stop=(kk == 7))
                lg = self.lgs[:, c, :]
                t8 = self.top8s[:, c, :]
                k.tt(lg, pl[:, 0:NEXP], self.rowp[:, R_BR:R_BR + NEXP], ALU.add)
                k.op("dve", lambda e, t8=t8, lg=lg: e.max(t8, lg), reads=[lg], writes=[t8])
                k.ts(negm[:], t8[:, 0:1], -1.0, ALU.mult)
                k.act(e4[:], t8[:, 0:4], AF.Exp, bias=negm[:, 0:1], scale=1.0, accum_out=gsum[:, 0:1])
                k.op("dve", lambda e: e.reciprocal(gsum[:], gsum[:]), reads=[gsum[:]], writes=[gsum[:]])
                k.ts(self.gates[:, c, :], e4[:], gsum[:, 0:1], ALU.mult)
                k.ts(maskb[:], lg, t8[:, 3:4], ALU.is_ge)
                pr = self.pf[4]
                k.mm(pr[:, 0:NEXP], self.cB(C_L1), maskb[:])
                k.mm(pr[:, NEXP:2 * NEXP], self.cB(C_ONE), maskb[:])
                k.tt(self.rks[:, c, :], pr[:, 0:NEXP], self.base[:], ALU.add)
                k.tt(self.base[:], self.base[:], pr[:, NEXP:2 * NEXP], ALU.add)
                pt = self.pt[j % 2]; rw = rows[j % 2]
                for kk in range(8):
                    k.tr(pt[:, kk * 128:(kk + 1) * 128], h2b[:, kk, j * 128:(j + 1) * 128], self.cB(C_ID))
                k.copy(rw[:, 0:512], pt[:, 0:512], eng="act")
                k.copy(rw[:, 512:1024], pt[:, 512:1024], eng="dve")
                k.dma("pool", S["h2"][c * 128:(c + 1) * 128, :], rw[:])
        k.release(m)

    def phase_route(self, l, last):
        k, I, S = self.k, self.I, self.S
        m = k.mark()
        chunks = list(range(2 if last else 0, NCH))
        NB = len(chunks) * 4 + NEXP
        self.NB = NB
        cm = k.sbuf("rt_cm", [128, NEXP], F32)
        gt = k.sbuf("rt_gt", [128, NEXP], F32)
        pd = k.sbuf("rt_pd", [128, NEXP], F32)
        ca = k.sbuf("rt_ca", [128, NEXP], F32)
        cbb = k.sbuf("rt_cb", [128, NEXP], F32)
        pstart = k.sbuf("rt_ps", [128, NEXP], F32)
        thr = k.sbuf("rt_thr", [128, 256], F32)
        k.ts(thr[:], self.iot[:, 0:256], 128.0, ALU.mult)
        cmp2 = k.sbuf("rt_cmp2", [128, NEXP, NCH], F32)
        k.tt(cmp2[:], bc(self.base[:], 2, NCH), bc(thr[:, 0:NCH], 1, NEXP), ALU.is_gt)
        k.op("dve", lambda e: e.reduce_sum(pd[:], cmp2[:], AX.X), reads=[cmp2[:]], writes=[pd[:]])
        k.ts(pd[:], pd[:], 128.0, ALU.mult)
        a, b = ca, cbb
        k.copy(a[:], pd[:])
        for sft in (1, 2, 4, 8, 16):
            k.copy(b[:, 0:sft], a[:, 0:sft])
            k.tt(b[:, sft:NEXP], a[:, sft:NEXP], a[:, 0:NEXP - sft], ALU.add)
            a, b = b, a
        pend = a
        k.tt(pstart[:], pend[:], pd[:], ALU.subtract)
        BIG = float(2 ** 30)
        cmp_ = k.sbuf("rt_cmp", [128, NB, NEXP], F32)
        blkf = k.sbuf("rt_blkf", [128, NB], F32)
        k.tt(cmp_[:], bc(pend[:, :], 1, NB), bc(thr[:, 0:NB], 2, NEXP), ALU.is_le)
        k.op("dve", lambda e: e.reduce_sum(blkf[:, 0:NB], cmp_[:], AX.X), reads=[cmp_[:]], writes=[blkf[:, 0:NB]])
        k.ts(blkf[:], blkf[:], float(NEXP - 1), ALU.min)
        k.copy(self.blkf[:, 0:NB], blkf[:, 0:NB])
        k.memset(self.flg[:, 0:1], 1.0)
        k.tt(self.flg[:, 1:NB], blkf[:, 1:NB], blkf[:, 0:NB - 1], ALU.not_equal)
        k.ts(self.nfl[:, 0:NB], self.flg[:, 0:NB], -BIG, ALU.mult, BIG, ALU.add)
        Dm = k.sbuf("rt_D", [128, NEXP], F32)
        oh = k.sbuf("rt_oh", [128, NEXP], F32)
        dstf = k.sbuf("rt_dstf", [128, 4], F32)
        rows = [k.sbuf("rt_rows%d" % i, [128, D], BF16) for i in range(2)]
        ixs = [k.sbuf("rt_ix%d" % i, [128, 1], I32) for i in range(8)]
        for ci, c in enumerate(chunks):
            rw = rows[ci % 2]
            k.dma("sp", rw[:], S["h2"][c * 128:(c + 1) * 128, :])
            k.tt(Dm[:], self.rks[:, c, :], pstart[:], ALU.add)
            for kq in range(4):
                k.ts(oh[:], self.lgs[:, c, :], self.top8s[:, c, kq:kq + 1], ALU.is_equal)
                k.tt(oh[:], oh[:], Dm[:], ALU.mult)
                k.op("dve", lambda e, kq=kq: e.reduce_sum(dstf[:, kq:kq + 1], oh[:], AX.X), reads=[oh[:]], writes=[dstf[:, kq:kq + 1]])
            k.copy(self.dsts[:, c, :], dstf[:])
            if "d_dsts" in self.debug:
                continue
            for kq in range(4):
                ixt = ixs[(ci * 4 + kq) % 8]
                k.copy(ixt[:], dstf[:, kq:kq + 1])
                ix = ixt[:, 0:1]
                k.dma_custom("pool", lambda e, ix=ix, rw=rw: e.indirect_dma_start(
                    out=S["buf"], out_offset=bass.IndirectOffsetOnAxis(ap=ix, axis=0), in_=rw[:], in_offset=None),
                    reads=[ix, rw[:]], writes=[S["buf"]])
        if "d_dsts" in self.debug:
            k.dma("sp", S["d_dsts"], self.dsts[:].rearrange("p c q -> p (c q)"))
            k.dma("sp", S["d_blkf"][:, 0:NB], self.blkf[:, 0:NB])
            k.dma("sp", S["d_base"], self.base[:])
            k.dma("sp", S["d_gates"], self.gates[:].rearrange("p c q -> p (c q)"))
            k.dma("sp", S["d_pend"], pend[:])
        k.release(m)

    def phase_moe(self, l, last):
        k, I, S = self.k, self.I, self.S
        m = k.mark()
        self.alloc_psum(6, 2)
        NB = self.NB
        wgu = k.sbuf("me_wgu", [128, 8, 2 * D], BF16)
        wd = k.sbuf("me_wd", [128, 8, D], BF16)
        bgu = k.sbuf("me_bgu", [128, 2 * D], BF16)
        bd = k.sbuf("me_bd", [128, D], BF16)
        rows = [k.sbuf("me_rows%d" % i, [128, D], BF16) for i in range(2)]
        rT = [k.sbuf("me_rT%d" % i, [128, 8, 128], BF16) for i in range(2)]
        gsb = k.sbuf("me_g", [128, D], F32)
        tsb = k.sbuf("me_t", [128, D], F32)
        lsb = k.sbuf("me_l", [128, D], F32)
        actb = k.sbuf("me_act", [128, D], BF16)
        aT = k.sbuf("me_aT", [128, 8, 128], BF16)
        osb = [k.sbuf("me_o%d" % i, [128, D], BF16) for i in range(2)]
        ones_row = self.cb[0:1, C_ONE:C_ONE + 128]
        wg2 = I["w_gate_up"]
        wd2 = I["w_down"]
        bg2 = I["b_gate_up"]
        bd2 = I["b_down"]
        c8 = k.sbuf("me_c8", [128, 8], F32)
        idf = k.sbuf("me_idf", [128, 9], F32)
        ixw = [k.sbuf("me_ixw%d" % i, [128, 1], I32) for i in range(18)]
        e1k = k.sbuf("me_e1k", [128, 1], F32)
        k.ts(idf[:, 0:1], self.iot[:, 256:257], 8.0, ALU.mult, float(l * NEXP * 1024), ALU.add)
        k.ts(c8[:], self.iot[:, 0:8], idf[:, 0:1], ALU.add)
        st = self.pool_regs

        def gath(e, out, src, ix, big):
            if "rw" not in st:
                st["rw"] = e.alloc_register("bnd_w")
                st["rb"] = e.alloc_register("bnd_b")
                e.reg_mov(st["rw"], DEPTH * NEXP * D - 1)
                e.reg_mov(st["rb"], DEPTH * NEXP - 1)
            return e.indirect_dma_start(out=out, out_offset=None, in_=src, in_offset=bass.IndirectOffsetOnAxis(ap=ix, axis=0),
                                        bounds_check=(st["rw"] if big else st["rb"]), oob_is_err=False)
        for b in range(NB):
            k.ts(e1k[:], self.blkf[:, b:b + 1], 1024.0, ALU.mult)
            k.ts(idf[:, 0:8], c8[:], e1k[:, 0:1], ALU.add)
            k.ts(idf[:, 8:9], self.blkf[:, b:b + 1], float(l * NEXP), ALU.add)
            k.ts(idf[:], idf[:], self.flg[:, b:b + 1], ALU.mult, self.nfl[:, b:b + 1], ALU.add)
            sl = (b % 2) * 9
            for j in range(9):
                k.copy(ixw[sl + j][:], idf[:, j:j + 1])
            bix = ixw[sl + 8][:, 0:1]
            k.dma_custom("pool", lambda e, bix=bix: gath(e, bgu[:], bg2, bix, False), reads=[bix, I["b_gate_up"]], writes=[bgu[:]])
            k.dma_custom("pool", lambda e, bix=bix: gath(e, bd[:], bd2, bix, False), reads=[bix, I["b_down"]], writes=[bd[:]])
            for kk in range(8):
                wix = ixw[sl + kk][:, 0:1]
                k.dma_custom("pool", lambda e, wix=wix, kk=kk: gath(e, wgu[:, kk, :], wg2, wix, True),
                             reads=[wix, I["w_gate_up"]], writes=[wgu[:, kk, :]])
            for kk in range(8):
                wix = ixw[sl + kk][:, 0:1]
                k.dma_custom("pool", lambda e, wix=wix, kk=kk: gath(e, wd[:, kk, :], wd2, wix, True),
                             reads=[wix, I["w_down"]], writes=[wd[:, kk, :]])
            rw = rows[b % 2]; rt = rT[b % 2]
            k.dma("sp", rw[:], S["buf"][b * 128:(b + 1) * 128, :])
            pt = self.pt[0]
            for kk in range(8):
                k.tr(pt[:, kk * 128:(kk + 1) * 128], rw[:].rearrange("p (c k) -> p k c", k=8)[:, kk, :], self.cB(C_ID))
            k.copy(rt[:, 0:4, :], pt[:, 0:512].rearrange("p (a t) -> p a t", a=4), eng="act")
            k.copy(rt[:, 4:8, :], pt[:, 512:1024].rearrange("p (a t) -> p a t", a=4), eng="dve")
            for nq in range(4):
                ps = self.pf[nq]
                for kk in range(8):
                    k.mm(ps[:], rt[:, kk, :], wgu[:, kk, nq * 512:(nq + 1) * 512], start=(kk == 0), stop=False)
                k.mm(ps[:], self.cB(C_E0), bgu[:, nq * 512:(nq + 1) * 512], start=False, stop=True)
            for h in range(2):
                k.ts(gsb[:, h * 512:(h + 1) * 512], self.pf[h][:], 7.0, ALU.min)
                k.ts(lsb[:, h * 512:(h + 1) * 512], self.pf[2 + h][:], 7.0, ALU.min, -7.0, ALU.max)
            k.act(tsb[:], gsb[:], AF.Silu, scale=ALPHA)
            k.act(lsb[:], lsb[:], AF.Identity, bias=self.ialb[:, 0:1], scale=1.0 / ALPHA)
            k.tt(actb[:], tsb[:], lsb[:], ALU.mult)
            pt2 = self.pt[1]
            for kk in range(8):
                k.tr(pt2[:, kk * 128:(kk + 1) * 128], actb[:].rearrange("p (c k) -> p k c", k=8)[:, kk, :], self.cB(C_ID))
            k.copy(aT[:, 0:4, :], pt2[:, 0:512].rearrange("p (a t) -> p a t", a=4), eng="act")
            k.copy(aT[:, 4:8, :], pt2[:, 512:1024].rearrange("p (a t) -> p a t", a=4), eng="dve")
            o = osb[b % 2]
            for nq in range(2):
                ps = self.pf[4 + nq]
                for kk in range(8):
                    k.mm(ps[:], aT[:, kk, :], wd[:, kk, nq * 512:(nq + 1) * 512], start=(kk == 0), stop=False)
                k.mm(ps[:], self.cB(C_E0), bd[:, nq * 512:(nq + 1) * 512], start=False, stop=True)
                k.copy(o[:, nq * 512:(nq + 1) * 512], ps[:], eng=("act" if nq == 0 else "dve"))
            k.dma("sp", S["obuf"][b * 128:(b + 1) * 128, :], o[:])
        k.release(m)

    def phase_combine(self, l, last):
        k, I, S = self.k, self.I, self.S
        m = k.mark()
        self.alloc_psum(0, 2)
        chunks = list(range(2 if last else 0, NCH))
        gk = [[k.sbuf("cb_g%d_%d" % (i, j), [128, D], BF16) for j in range(4)] for i in range(2)]
        cixs = [k.sbuf("cb_ix%d" % i, [128, 1], I32) for i in range(8)]
        f = k.sbuf("cb_f", [128, D], F32)
        fb = k.sbuf("cb_fb", [128, D], BF16)
        xts = [k.sbuf("cb_x%d" % i, [128, 8, 128], F32) for i in range(2)]
        tmp = k.sbuf("cb_tmp", [128, 8, 128], F32)
        xTv = S["xT"].rearrange("(k p) t -> p k t", p=128)
        for ci, c in enumerate(chunks):
            s = 1 if c < 2 else 0
            g4 = gk[ci % 2]
            xt = xts[ci % 2]
            k.dma("sp", xt[:], xTv[:, :, c * 128:(c + 1) * 128])
            for kq in range(4):
                ixt = cixs[(ci * 4 + kq) % 8]
                k.copy(ixt[:], self.dsts[:, c, kq:kq + 1])
                ix = ixt[:, 0:1]
                gt = g4[kq]
                k.dma_custom("pool", lambda e, ix=ix, gt=gt: e.indirect_dma_start(
                    out=gt[:], out_offset=None, in_=S["obuf"], in_offset=bass.IndirectOffsetOnAxis(ap=ix, axis=0)),
                    reads=[ix, S["obuf"]], writes=[gt[:]])
            k.ts(f[:], g4[0][:], self.gates[:, c, 0:1], ALU.mult)
            for kq in range(1, 4):
                k.stt(f[:] if kq < 3 else fb[:], g4[kq][:], self.gates[:, c, kq:kq + 1], f[:], ALU.mult, ALU.add)
            pt = self.pt[ci % 2]
            for kk in range(8):
                k.tr(pt[:, kk * 128:(kk + 1) * 128], fb[:, kk * 128:(kk + 1) * 128], self.cB(C_ID))
            k.tt(tmp[:], pt[:].rearrange("p (a t) -> p a t", a=8), bc(self.mod[:, 40:48, s], 2, 128), ALU.mult)
            k.tt(xt[:], xt[:], tmp[:], ALU.add)
            k.dma("sp", xTv[:, :, c * 128:(c + 1) * 128], xt[:])
        k.release(m)

    def phase_final(self):
        k, I, S = self.k, self.I, self.S
        m = k.mark()
        self.alloc_psum(4, 0)
        xts = [k.sbuf("fn_x%d" % i, [128, 8, 128], F32) for i in range(2)]
        sq = k.sbuf("fn_sq", [128, 8, 128], BF16)
        rstd = k.sbuf("fn_rstd", [128, 128], F32)
        xn = k.sbuf("fn_xn", [128, 8, 128], F32)
        ob = [k.sbuf("fn_o%d" % i, [128, D], F32) for i in range(2)]
        xTv = S["xT"].rearrange("(k p) t -> p k t", p=128)
        for c in range(2, NCH):
            xt = xts[c % 2]
            k.dma("sp", xt[:], xTv[:, :, c * 128:(c + 1) * 128])
            k.act(sq[:], xt[:], AF.Square)
            ps = self.pf[0]
            for kk in range(8):
                k.mm(ps[:, 0:128], self.cB(C_ONE), sq[:, kk, :], start=(kk == 0), stop=(kk == 7))
            k.act(rstd[:], ps[:, 0:128], AF.Sqrt, bias=self.epsb[:, 0:1], scale=1.0 / D)
            k.op("dve", lambda e: e.reciprocal(rstd[:], rstd[:]), reads=[rstd[:]], writes=[rstd[:]])
            for kk in range(8):
                k.stt(xn[:, kk, :], xt[:, kk, :], self.gv[:, kk:kk + 1], rstd[:], ALU.mult, ALU.mult,
                      eng="dve")
            o = ob[c % 2]
            for h in range(2):
                p = self.pf[1 + (c % 2) * 0 + h]
                for j in range(4):
                    kk = h * 4 + j
                    k.mm(p[:, j * 128:(j + 1) * 128], xn[:, kk, :], self.cF(C_ID))
                k.copy(o[:, h * 512:(h + 1) * 512], p[:], eng=("act" if h else "dve"))
            k.dma("pool", self.out[(c - 2) * 128:(c - 1) * 128, :], o[:])
        k.release(m)

    def build(self):
        k, I = self.k, self.I
        self.vec = k.sbuf("vec_sb", [128, NVEC], F32)
        self.rowp = k.sbuf("rowp_sb", [128, NROW], F32)
        self.mod = k.sbuf("mod", [128, 48, 2], F32)
        self.A1 = k.sbuf("A1", [128, 8, 2], F32)
        self.A2 = k.sbuf("A2", [128, 8, 2], F32)
        self.epsb = k.sbuf("epsb", [128, 1], F32)
        self.oneb = k.sbuf("oneb", [128, 1], F32)
        self.nm_tmp = [k.sbuf("nmtmp%d" % i, [128, 512], F32) for i in range(2)]
        self.iot = k.sbuf("iot_sb", [128, 257], F32)
        self.lgs = k.sbuf("lgs", [128, NCH, NEXP], F32)
        self.top8s = k.sbuf("top8s", [128, NCH, 8], F32)
        self.rks = k.sbuf("rks", [128, NCH, NEXP], F32)
        self.gates = k.sbuf("gates", [128, NCH, 4], F32)
        self.dsts = k.sbuf("dsts", [128, NCH, 4], I32)
        self.base = k.sbuf("base", [128, NEXP], F32)
        self.blkf = k.sbuf("blkf", [128, NBLK], F32)
        self.flg = k.sbuf("flg", [128, NBLK], F32)
        self.nfl = k.sbuf("nfl", [128, NBLK], F32)
        k.memset(self.epsb[:], EPS)
        k.memset(self.oneb[:], 1.0)
        self.ialb = k.sbuf("ialb", [128, 1], F32)
        k.memset(self.ialb[:], 1.0 / ALPHA)
        k.dma("sp", self.iot[:], I["iot"])
        stop = self.stop_after
        seq = [("x0", None)]
        for l in range(self.nlayers):
            for ph in ("ada", "inproj", "conv", "attn", "ssd", "outproj", "route", "moe", "combine"):
                seq.append((ph, l))
        seq.append(("final", None))
        for ph, l in seq:
            last = (l == DEPTH - 1)
            if ph == "x0":
                self.phase_x0()
            elif ph == "final":
                self.phase_final()
            elif ph == "ada":
                k.dma("sp", self.vec[:], I["vec"][l])
                k.dma("sp", self.rowp[:], I["rowp"][l])
                k.memset(self.base[:], 0.0)
                self.phase_ada(l)
            elif ph in ("inproj", "conv", "ada"):
                getattr(self, "phase_" + ph)(l)
            else:
                getattr(self, "phase_" + ph)(l, last)
            if stop is not None and (ph, l) == tuple(stop):
                break
        k.barrier()
        k.wait_all("sp")
        k.emit()
        k.close()
        return self.nc


def _host_consts():
    idn = np.eye(128, dtype=np.float32)
    perm = np.zeros((128, 128), np.float32)
    for p in range(128):
        perm[p, p + 16 if (p % 32) < 16 else p - 16] = 1.0
    kk = np.arange(128)
    U0 = (kk[:, None] <= kk[None, :]).astype(np.float32)
    U1 = (kk[:, None] >= kk[None, :]).astype(np.float32)
    L0 = (kk[:, None] > kk[None, :]).astype(np.float32)
    L1 = (kk[:, None] < kk[None, :]).astype(np.float32)
    ones = np.ones((128, 128), np.float32)
    e0 = np.zeros((128, 128), np.float32)
    e0[0, :] = 1.0
    cst = np.concatenate([idn, perm, U0, U1, L0, L1, ones, e0], axis=1)
    t = np.arange(S_LAT)
    pos = np.stack([t // 64, t % 64], axis=0).astype(np.float64)
    inv = 10000.0 ** (-np.arange(16, dtype=np.float64) / 16.0)
    rope = np.zeros((128, 2, S_LAT), np.float32)
    for p in range(128):
        d = p % 64
        a, r, f = d // 32, (d % 32) // 16, d % 16
        ang = pos[a] * inv[f]
        rope[p, 0] = np.cos(ang)
        rope[p, 1] = np.sin(ang) * (-1.0 if r == 0 else 1.0)
    iot = np.zeros((128, 257), np.float32)
    iot[:, 0:256] = np.arange(256, dtype=np.float32)[None, :]
    iot[:, 256] = np.arange(128, dtype=np.float32)
    return cst, rope, iot


def _fm(v, kcols):
    return np.ascontiguousarray(np.asarray(v, np.float32).reshape(kcols, 128).T)


def _host_layout(inputs):
    vec = np.zeros((DEPTH, 128, NVEC), np.float32)
    rowp = np.zeros((DEPTH, 128, NROW), np.float32)
    for l in range(DEPTH):
        vec[l, :, V_NMW:V_NMW + 8] = _fm(inputs["norm_mix_w"][l], 8)
        vec[l, :, V_NFW:V_NFW + 8] = _fm(inputs["norm_ffn_w"][l], 8)
        vec[l, :, V_BADA:V_BADA + 48] = _fm(inputs["b_ada"][l], 48)
        vec[l, :, V_CONVB:V_CONVB + 6] = _fm(inputs["conv_b"][l], 6)
        cw = np.asarray(inputs["conv_w"][l], np.float32)
        vec[l, :, V_CONVW:V_CONVW + 30] = cw.reshape(5, 6, 128).transpose(2, 1, 0).reshape(128, 30)
        vec[l, :, V_SNW:V_SNW + 4] = _fm(inputs["ssm_norm_w"][l], 4)
        vec[l, :, V_ANW:V_ANW + 4] = _fm(inputs["attn_norm_w"][l], 4)
        row = np.concatenate([np.asarray(inputs["dt_bias"][l], np.float32).reshape(16),
                              np.asarray(inputs["a_log"][l], np.float32).reshape(16),
                              np.asarray(inputs["d_skip"][l], np.float32).reshape(8),
                              np.asarray(inputs["attn_sinks"][l], np.float32).reshape(8),
                              np.asarray(inputs["b_router"][l], np.float32).reshape(32),
                              np.asarray(inputs["conv_b"][l], np.float32).reshape(768)])
        rowp[l] = np.broadcast_to(row[None, :], (128, NROW))
    return vec, rowp


_CACHE = {}


def kernel(**inputs):
    dbg = inputs.pop("_debug", None)
    inputs = {k_: np.asarray(v) for k_, v in inputs.items()}
    debug = tuple(dbg["dump"]) if dbg is not None else ()
    stop = dbg["stop"] if dbg is not None else None
    ncores = int(dbg.get("ncores", 8)) if dbg is not None else 8
    prog = Prog(debug=debug, stop_after=stop)
    nc = prog.build()
    cst, rope, iot = _host_consts()
    vec, rowp = _host_layout(inputs)
    f32 = lambda a: np.ascontiguousarray(np.asarray(a, np.float32))
    shared = {"w_ada": f32(inputs["w_ada"]), "w_in": f32(inputs["w_in"]), "w_out": f32(inputs["w_out"]),
              "w_router": f32(inputs["w_router"]), "w_gate_up": f32(inputs["w_gate_up"]).reshape(DEPTH * NEXP * D, 2 * D),
              "b_gate_up": f32(inputs["b_gate_up"]).reshape(DEPTH * NEXP, 2 * D),
              "w_down": f32(inputs["w_down"]).reshape(DEPTH * NEXP * D, D), "b_down": f32(inputs["b_down"]).reshape(DEPTH * NEXP, D),
              "vec": vec, "rowp": rowp, "cst": cst, "rope": rope, "iot": iot}
    in_maps = []
    for b in range(ncores):
        gvec = np.concatenate([_fm(inputs["final_norm_w"], 8), _fm(inputs["c"][b], 8), _fm(inputs["c_ctx"], 8)], axis=1)
        mp = dict(shared)
        mp["x"] = f32(inputs["x"][b]); mp["ctx"] = f32(inputs["ctx"][b]); mp["gvec"] = np.ascontiguousarray(gvec)
        in_maps.append(mp)
    res = run_bass_kernel_spmd(nc, in_maps, core_ids=list(range(ncores)))
    if dbg is not None:
        dbg["results"] = res.results
    out = np.stack([np.asarray(r["out"], np.float32) for r in res.results], axis=0)
    return out
```

```python
import numpy as np
import concourse.bass as bass
import concourse.mybir as mybir

F32 = mybir.dt.float32
BF16 = mybir.dt.bfloat16
I32 = mybir.dt.int32
AF = mybir.ActivationFunctionType
ALU = mybir.AluOpType
AX = mybir.AxisListType

ENGS = ("pe", "act", "dve", "pool", "sp")
NRING = 12


def _is_psum(ap):
    return type(ap.tensor).__name__ == "PSumTensorHandle"


def _region(ap):
    shape = list(ap.tensor.shape)
    if _is_psum(ap):
        return tuple((0, int(d) - 1) for d in shape)
    lo = int(ap.offset)
    hi = lo
    for step, cnt in ap.ap:
        step = int(step); cnt = int(cnt)
        if step >= 0:
            hi += step * (cnt - 1)
        else:
            lo += step * (cnt - 1)
    strides = [1] * len(shape)
    for i in range(len(shape) - 2, -1, -1):
        strides[i] = strides[i + 1] * int(shape[i + 1])
    reg = []
    l, h = lo, hi
    for i, s in enumerate(strides):
        a, l = divmod(l, s)
        b, h = divmod(h, s)
        if b < a or (i > 0 and reg and reg[-1][0] != reg[-1][1] and False):
            a, b = 0, int(shape[i]) - 1
        reg.append((a, b))
    for i in range(len(reg)):
        a, b = reg[i]
        if b < a:
            reg[i] = (0, int(shape[i]) - 1)
    return tuple(reg)


def _overlap(r1, r2):
    for (a0, a1), (b0, b1) in zip(r1, r2):
        if a1 < b0 or b1 < a0:
            return False
    return True


def _contains(outer, inner):
    for (a0, a1), (b0, b1) in zip(outer, inner):
        if b0 < a0 or b1 > a1:
            return False
    return True


class Rec:
    __slots__ = ("reg", "writer", "readers")

    def __init__(self, reg, writer, readers):
        self.reg = reg
        self.writer = writer
        self.readers = readers


class K:
    def __init__(self, nc):
        self.nc = nc
        self.ops = {e: [] for e in ENGS}
        self.cnt = {e: 0 for e in ENGS}
        self.clock = {e: {} for e in ENGS}
        self.recs = {}
        self.sems = {}
        self._ctx = []
        for e in ENGS:
            self.sems[e] = self._enter(nc.semaphore("s_" + e))
        self.ring = {}
        self.ring_pos = {}
        self.ring_val = {}
        for q in ("sp", "pool", "act"):
            self.ring[q] = []
            for i in range(NRING):
                key = "d_%s_%d" % (q, i)
                self.sems[key] = self._enter(nc.semaphore(key))
                self.ring[q].append(key)
            self.ring_pos[q] = 0
            self.ring_val[q] = [0] * NRING
        self.n_wait = 0
        self.tcount = 0

    def _enter(self, guard):
        v = guard.__enter__()
        self._ctx.append(guard)
        return v

    def sbuf(self, name, shape, dtype):
        self.tcount += 1
        return self._enter(self.nc.sbuf_tensor("%s_%d" % (name, self.tcount), list(shape), dtype))

    def psum(self, name, shape, dtype=F32):
        self.tcount += 1
        return self._enter(self.nc.psum_tensor("%s_%d" % (name, self.tcount), list(shape), dtype))

    def dram(self, name, shape, dtype, kind="Internal"):
        return self.nc.dram_tensor(name, list(shape), dtype, kind=kind)

    def _need(self, eng, ev, skip_self_pe):
        if ev is None:
            return
        key, val = ev
        if key == eng and eng == "pe" and skip_self_pe:
            return
        if self.clock[eng].get(key, 0) >= val:
            return
        self.clock[eng][key] = val
        sem = self.sems[key]
        self.ops[eng].append(("w", sem, val))
        self.n_wait += 1

    def _deps(self, eng, reads, writes):
        need = []
        for ap in reads:
            name = ap.tensor.name
            reg = _region(ap)
            for r in self.recs.get(name, ()):
                if r.writer is not None and _overlap(r.reg, reg):
                    need.append(r.writer)
        for ap in writes:
            name = ap.tensor.name
            reg = _region(ap)
            for r in self.recs.get(name, ()):
                if _overlap(r.reg, reg):
                    if r.writer is not None:
                        need.append(r.writer)
                    for k, v in r.readers.items():
                        need.append((k, v))
        return need

    def _record(self, ev, reads, writes):
        key, val = ev
        for ap in reads:
            name = ap.tensor.name
            reg = _region(ap)
            lst = self.recs.setdefault(name, [])
            for r in lst:
                if r.reg == reg:
                    if r.readers.get(key, 0) < val:
                        r.readers[key] = val
                    break
            else:
                lst.append(Rec(reg, None, {key: val}))
        for ap in writes:
            name = ap.tensor.name
            reg = _region(ap)
            lst = self.recs.setdefault(name, [])
            lst[:] = [r for r in lst if not _contains(reg, r.reg)]
            lst.append(Rec(reg, ev, {}))

    def op(self, eng, fn, reads=(), writes=()):
        writes = list(writes) + [ap for ap in reads if _is_psum(ap)]
        reads = [ap for ap in reads if not _is_psum(ap)]
        for ev in self._deps(eng, reads, writes):
            self._need(eng, ev, True)
        self.cnt[eng] += 1
        ev = (eng, self.cnt[eng])
        self.ops[eng].append(("o", fn, self.sems[eng], 1))
        self._record(ev, reads, writes)
        return ev

    def dma(self, q, out, in_, **kw):
        for ev in self._deps(q, [in_], [out]):
            self._need(q, ev, False)
        i = self.ring_pos[q]
        self.ring_pos[q] = (i + 1) % NRING
        key = self.ring[q][i]
        prev = self.ring_val[q][i]
        if prev:
            self._need(q, (key, prev), False)
        val = prev + 16
        self.ring_val[q][i] = val
        ev = (key, val)

        def fn(e, out=out, in_=in_, kw=kw):
            return e.dma_start(out=out, in_=in_, **kw)
        self.ops[q].append(("o", fn, self.sems[key], 16))
        self._record(ev, [in_], [out])
        return ev

    def dma_custom(self, q, fn, reads, writes):
        for ev in self._deps(q, reads, writes):
            self._need(q, ev, False)
        i = self.ring_pos[q]
        self.ring_pos[q] = (i + 1) % NRING
        key = self.ring[q][i]
        prev = self.ring_val[q][i]
        if prev:
            self._need(q, (key, prev), False)
        val = prev + 16
        self.ring_val[q][i] = val
        ev = (key, val)
        self.ops[q].append(("o", fn, self.sems[key], 16))
        self._record(ev, reads, writes)
        return ev

    def mark(self):
        return len(self._ctx)

    def pe_drain(self):
        if self.cnt["pe"]:
            self.ops["pe"].append(("w", self.sems["pe"], self.cnt["pe"]))

    def barrier(self):
        for e in ENGS:
            self.wait_all(e)
        self.recs.clear()

    def release(self, m):
        self.barrier()
        while len(self._ctx) > m:
            self._ctx.pop().__exit__(None, None, None)

    def wait_all(self, eng):
        for e in ENGS:
            if e != eng and self.cnt[e]:
                self._need(eng, (e, self.cnt[e]), False)
        for q in self.ring:
            for i, key in enumerate(self.ring[q]):
                if self.ring_val[q][i]:
                    self._need(eng, (key, self.ring_val[q][i]), False)

    def emit(self):
        nc = self.nc
        engobj = {"pe": "tensor", "act": "scalar", "dve": "vector", "pool": "gpsimd", "sp": "sync"}
        with nc.Block() as block:
            for e in ENGS:
                lst = self.ops[e]
                if not lst:
                    continue

                def body(eng, lst=lst):
                    for it in lst:
                        if it[0] == "w":
                            eng.wait_ge(it[1], it[2])
                        else:
                            it[1](eng).then_inc(it[2], it[3])
                getattr(block, engobj[e])(body)

    def close(self):
        for g in reversed(self._ctx):
            g.__exit__(None, None, None)

    def mm(self, out, lhsT, rhs, start=True, stop=True, **kw):
        return self.op("pe", lambda e: e.matmul(out, lhsT, rhs, start=start, stop=stop, **kw),
                       reads=[lhsT, rhs], writes=[out])

    def tr(self, out, in_, ident):
        return self.op("pe", lambda e: e.transpose(out, in_, ident), reads=[in_, ident], writes=[out])

    def act(self, out, in_, func, bias=None, scale=None, accum_out=None, eng="act"):
        kw = {}
        reads = [in_]
        writes = [out]
        if bias is not None:
            kw["bias"] = bias
            if not isinstance(bias, (int, float)):
                reads.append(bias)
        if scale is not None:
            kw["scale"] = scale
            if not isinstance(scale, (int, float)):
                reads.append(scale)
        if accum_out is not None:
            kw["accum_out"] = accum_out
            writes.append(accum_out)
        return self.op(eng, lambda e: e.activation(out, in_, func, **kw), reads=reads, writes=writes)

    def tt(self, out, in0, in1, op, eng="dve"):
        return self.op(eng, lambda e: e.tensor_tensor(out, in0, in1, op), reads=[in0, in1], writes=[out])

    def ts(self, out, in0, s1, op0, s2=None, op1=None, eng="dve", accum_out=None):
        reads = [in0]
        if not isinstance(s1, (int, float)):
            reads.append(s1)
        if s2 is not None and not isinstance(s2, (int, float)):
            reads.append(s2)
        writes = [out]
        kw = {}
        if accum_out is not None:
            kw["accum_out"] = accum_out
            writes.append(accum_out)
        if op1 is None:
            return self.op(eng, lambda e: e.tensor_scalar(out, in0, s1, None, op0, **kw), reads=reads, writes=writes)
        return self.op(eng, lambda e: e.tensor_scalar(out, in0, s1, s2, op0, op1, **kw), reads=reads, writes=writes)

    def stt(self, out, in0, scalar, in1, op0, op1, eng="dve"):
        reads = [in0, in1]
        if not isinstance(scalar, (int, float)):
            reads.append(scalar)
        return self.op(eng, lambda e: e.scalar_tensor_tensor(out, in0, scalar, in1, op0, op1),
                       reads=reads, writes=[out])

    def copy(self, out, in_, eng="dve"):
        if eng == "act":
            return self.op("act", lambda e: e.copy(out, in_), reads=[in_], writes=[out])
        return self.op(eng, lambda e: e.tensor_copy(out, in_), reads=[in_], writes=[out])

    def memset(self, ap, val, eng="dve"):
        return self.op(eng, lambda e: e.memset(ap, val), reads=[], writes=[ap])


from concourse.bass_utils import run_bass_kernel_spmd

T = 4352
NCH = 34
CTX = 256
S_LAT = 4096
D = 1024
DEPTH = 2
NEXP = 32
EPS = 1e-6
ALPHA = 1.702
TILES = [(0, 256, 1)] + [(256 + 512 * i, 512, 0) for i in range(8)]
BLK = 256
NBLK = (T * 4) // BLK + NEXP
V_NMW, V_NFW, V_BADA, V_CONVB, V_CONVW, V_SNW, V_ANW = 0, 8, 16, 64, 70, 100, 104
NVEC = 108
R_DTB, R_ALOG, R_DSKIP, R_SINK, R_BR = 0, 16, 32, 40, 48
R_CONVB = 80
NROW = 848
C_ID, C_PERM, C_U0, C_U1, C_L0, C_L1, C_ONE, C_E0 = 0, 128, 256, 384, 512, 640, 768, 896
NCST = 1024


def bc(ap, axis, n):
    shp = list(ap.shape)
    shp.insert(axis, n)
    return ap.unsqueeze(axis).broadcast_to(shp)


class Prog:
    def __init__(self, debug=None, nlayers=DEPTH, stop_after=None):
        self.debug = debug or ()
        self.nlayers = nlayers
        self.stop_after = stop_after
        nc = bass.Bass("TRN2", target_bir_lowering=False)
        self.nc = nc
        self.k = K(nc)
        k = self.k
        I = {}

        def inp(name, shape):
            I[name] = nc.dram_tensor(name, list(shape), F32, kind="ExternalInput").ap()
        inp("x", [S_LAT, D]); inp("ctx", [CTX, D])
        inp("w_ada", [DEPTH, D, 6 * D]); inp("w_in", [DEPTH, D, 2064]); inp("w_out", [DEPTH, D, D])
        inp("w_router", [DEPTH, D, NEXP]); inp("w_gate_up", [DEPTH * NEXP * D, 2 * D])
        inp("b_gate_up", [DEPTH * NEXP, 2 * D]); inp("w_down", [DEPTH * NEXP * D, D]); inp("b_down", [DEPTH * NEXP, D])
        inp("vec", [DEPTH, 128, NVEC]); inp("gvec", [128, 24]); inp("rowp", [DEPTH, 128, NROW])
        inp("cst", [128, NCST]); inp("rope", [128, 2, S_LAT]); inp("iot", [128, 257])
        self.I = I
        self.out = nc.dram_tensor("out", [S_LAT, D], F32, kind="ExternalOutput").ap()
        self.S = {}

        def scr(name, shape, dt):
            kind = "ExternalOutput" if name in self.debug else "Internal"
            self.S[name] = nc.dram_tensor("s_" + name, list(shape), dt, kind=kind).ap()
        scr("xT", [D, T], F32)
        scr("qT", [512, T], BF16); scr("kT", [128, T], BF16); scr("v", [T, 128], BF16)
        scr("zs", [T, 512], BF16); scr("dt", [T, 16], F32); scr("xbcT", [768, T], BF16)
        scr("xs", [T, 512], BF16); scr("Bt", [T, 128], BF16); scr("BT", [128, T], BF16); scr("CT", [128, T], BF16)
        scr("yf", [T, 512], F32); scr("mixT", [D, T], BF16)
        if "yb" in self.debug:
            scr("yb", [T, 512], F32)
        if "d_dsts" in self.debug:
            scr("d_dsts", [128, NCH * 4], I32); scr("d_blkf", [128, NBLK], F32); scr("d_base", [128, NEXP], F32)
            scr("d_gates", [128, NCH * 4], F32); scr("d_pend", [128, NEXP], F32)
        if "dg1" in self.debug:
            scr("dg1", [T, 512], F32); scr("dg2", [T, 512], F32); scr("dg3", [T, 512], BF16)
        scr("h2", [T, D], BF16); scr("gat", [T, 4], F32); scr("dst", [T, 4], I32)
        scr("buf", [NBLK * BLK, D], BF16); scr("obuf", [NBLK * BLK, D], BF16)
        scr("blke", [1, 2 * NBLK], I32)
        self.psum_gen = 0
        self.pool_regs = {}
        self.cf = k.sbuf("cstf", [128, NCST], F32)
        self.cb = k.sbuf("cstb", [128, NCST], BF16)
        self.gv = k.sbuf("gvec_sb", [128, 24], F32)
        k.dma("sp", self.cf[:], I["cst"])
        k.dma("pool", self.cb[:], I["cst"])
        k.dma("sp", self.gv[:], I["gvec"])

    def alloc_psum(self, nf, nt):
        k = self.k
        self.psum_gen += 1
        self.pf = [k.psum("pf%d_%d" % (self.psum_gen, i), [128, 512], F32) for i in range(nf)]
        self.pt = [k.psum("pt%d_%d" % (self.psum_gen, i), [128, 1024], BF16) for i in range(nt)]

    def cB(self, off, n=128, p=128):
        return self.cb[0:p, off:off + n]

    def cF(self, off, n=128, p=128):
        return self.cf[0:p, off:off + n]

    def phase_x0(self):
        k, I, S = self.k, self.I, self.S
        m = k.mark()
        self.alloc_psum(2, 0)
        xin = [k.sbuf("x0in%d" % i, [128, D], F32) for i in range(2)]
        xo = [k.sbuf("x0o%d" % i, [128, 8, 128], F32) for i in range(2)]
        xTv = S["xT"].rearrange("(k p) t -> p k t", p=128)
        for c in range(NCH):
            src = I["ctx"][c * 128:(c + 1) * 128, :] if c < 2 else I["x"][(c - 2) * 128:(c - 1) * 128, :]
            xi = xin[c % 2]; o = xo[c % 2]
            k.dma("sp", xi[:], src)
            for h in range(2):
                p = self.pf[h]
                for j in range(4):
                    kk = h * 4 + j
                    k.mm(p[:, j * 128:(j + 1) * 128], xi[:, kk * 128:(kk + 1) * 128], self.cF(C_ID))
                k.copy(o[:, h * 4:(h + 1) * 4, :], p[:].rearrange("p (j t) -> p j t", j=4), eng=("dve" if h else "act"))
            k.dma("pool", xTv[:, :, c * 128:(c + 1) * 128], o[:])
        k.release(m)

    def phase_ada(self, l):
        k, I = self.k, self.I
        vec = self.vec
        m = k.mark()
        self.alloc_psum(1, 0)
        sc = k.sbuf("ada_sc", [128, 8, 2], F32)
        k.act(sc[:, :, 0], self.gv[:, 8:16], AF.Silu)
        k.act(sc[:, :, 1], self.gv[:, 16:24], AF.Silu)
        wa = [k.sbuf("ada_w%d" % i, [128, 8, 512], F32) for i in range(2)]
        pm = self.pf[0]
        pmv = pm[:, 0:96].rearrange("p (j s) -> p j s", s=2)
        wv = I["w_ada"][l].rearrange("(k p) n -> p k n", p=128)
        for g in range(12):
            w = wa[g % 2]
            k.dma("sp", w[:], wv[:, :, g * 512:(g + 1) * 512])
            for jj in range(4):
                j = g * 4 + jj
                for kk in range(8):
                    k.mm(pmv[:, j, :], w[:, kk, jj * 128:(jj + 1) * 128], sc[:, kk, :], start=(kk == 0), stop=(kk == 7))
        mod = self.mod
        k.tt(mod[:], pmv, bc(vec[:, V_BADA:V_BADA + 48], 2, 2), ALU.add)
        for (A, base, nw) in ((self.A1, 8, V_NMW), (self.A2, 32, V_NFW)):
            k.ts(A[:], mod[:, base:base + 8, :], 1.0, ALU.add)
            k.tt(A[:], A[:], bc(vec[:, nw:nw + 8], 2, 2), ALU.mult)
        k.release(m)

    def norm_mod(self, xt, n, s, A, Boff, hT, sq, rstd, ps):
        k = self.k
        k.act(sq[:, :, 0:n], xt[:, :, 0:n], AF.Square)
        for kk in range(8):
            k.mm(ps[:, 0:n], self.cB(C_ONE), sq[:, kk, 0:n], start=(kk == 0), stop=(kk == 7))
        k.act(rstd[:, 0:n], ps[:, 0:n], AF.Sqrt, bias=self.epsb[:, 0:1], scale=1.0 / D)
        k.op("dve", lambda e: e.reciprocal(rstd[:, 0:n], rstd[:, 0:n]), reads=[rstd[:, 0:n]], writes=[rstd[:, 0:n]])
        for kk in range(8):
            tp = self.nm_tmp[kk % 2]
            k.stt(tp[:, 0:n], xt[:, kk, 0:n], A[:, kk, s:s + 1], rstd[:, 0:n], ALU.mult, ALU.mult)
            k.act(hT[:, kk, 0:n], tp[:, 0:n], AF.Identity, bias=self.mod[:, Boff + kk, s:s + 1], scale=1.0)

    def phase_inproj(self, l):
        k, I, S = self.k, self.I, self.S
        m = k.mark()
        self.alloc_psum(6, 0)
        win = k.sbuf("win", [128, 8, 2064], BF16)
        wv = I["w_in"][l].rearrange("(k p) n -> p k n", p=128)
        for kk in range(8):
            for c0_ in (0, 1024, 2048):
                c1_ = min(c0_ + 1024, 2064)
                k.dma("pool", win[:, kk, c0_:c1_], wv[:, kk, c0_:c1_])
        rope = k.sbuf("rope_sb", [128, 2, S_LAT], F32)
        k.dma("sp", rope[:], I["rope"])
        xts = [k.sbuf("ip_x%d" % i, [128, 8, 512], F32) for i in range(2)]
        hTs = [k.sbuf("ip_h%d" % i, [128, 8, 512], BF16) for i in range(2)]
        sq = k.sbuf("ip_sq", [128, 8, 512], BF16)
        rstd = k.sbuf("ip_rstd", [128, 512], F32)
        qb = [k.sbuf("ip_qb%d" % i, [128, 512], BF16) for i in range(2)]
        t1 = [k.sbuf("ip_t1%d" % i, [128, 512], F32) for i in range(2)]
        t2 = [k.sbuf("ip_t2%d" % i, [128, 512], F32) for i in range(2)]
        ob = [k.sbuf("ip_ob%d" % i, [128, 512], BF16) for i in range(3)]
        zsb = [k.sbuf("ip_zs%d" % i, [128, 512], BF16) for i in range(2)]
        vsb = [k.sbuf("ip_v%d" % i, [128, 128], BF16) for i in range(2)]
        dtt = [k.sbuf("ip_dt%d" % i, [128, 16], F32) for i in range(2)]
        xTv = S["xT"].rearrange("(k p) t -> p k t", p=128)
        cnt = 0
        for ti, (t0, n, s) in enumerate(TILES):
            xt = xts[ti % 2]; hT = hTs[ti % 2]
            k.dma("sp", xt[:, :, 0:n], xTv[:, :, t0:t0 + n])
            self.norm_mod(xt, n, s, self.A1, 0, hT, sq, rstd, self.pf[5])
            fm = [("q", c, c * 128) for c in range(4)] + [("k", 0, 512)] + [("xbc", c, 1280 + c * 128) for c in range(6)]
            for (nm, c, col) in fm:
                ps = self.pf[cnt % 2]; cnt += 1
                for kk in range(8):
                    k.mm(ps[:, 0:n], win[:, kk, col:col + 128], hT[:, kk, 0:n], start=(kk == 0), stop=(kk == 7))
                o = ob[cnt % 3]
                if nm == "xbc":
                    k.copy(o[:, 0:n], ps[:, 0:n], eng="act")
                    k.dma("pool", S["xbcT"][c * 128:(c + 1) * 128, t0:t0 + n], o[:, 0:n])
                    continue
                dst = S["qT"][c * 128:(c + 1) * 128, t0:t0 + n] if nm == "q" else S["kT"][:, t0:t0 + n]
                if s == 1:
                    k.copy(o[:, 0:n], ps[:, 0:n], eng="act")
                else:
                    q_ = qb[cnt % 2]; t_ = t1[cnt % 2]
                    p0 = t0 - CTX
                    k.copy(q_[:, 0:n], ps[:, 0:n], eng="act")
                    pp = self.pf[2 + cnt % 2]
                    k.mm(pp[:, 0:n], self.cB(C_PERM), q_[:, 0:n])
                    t2_ = t2[cnt % 2]
                    k.tt(t_[:, 0:n], q_[:, 0:n], rope[:, 0, p0:p0 + n], ALU.mult)
                    k.tt(t2_[:, 0:n], pp[:, 0:n], rope[:, 1, p0:p0 + n], ALU.mult)
                    k.tt(o[:, 0:n], t_[:, 0:n], t2_[:, 0:n], ALU.add)
                k.dma("pool", dst, o[:, 0:n])
            for j in range(n // 128):
                c0 = t0 + j * 128
                pz = self.pf[4]; pv = self.pf[2 + j % 2]
                for kk in range(8):
                    k.mm(pz[:, :], hT[:, kk, j * 128:(j + 1) * 128], win[:, kk, 768:1280], start=(kk == 0), stop=(kk == 7))
                for kk in range(8):
                    k.mm(pv[:, 0:128], hT[:, kk, j * 128:(j + 1) * 128], win[:, kk, 640:768], start=(kk == 0), stop=(kk == 7))
                for kk in range(8):
                    k.mm(pv[:, 128:144], hT[:, kk, j * 128:(j + 1) * 128], win[:, kk, 2048:2064], start=(kk == 0), stop=(kk == 7))
                z_ = zsb[j % 2]; v_ = vsb[j % 2]; d_ = dtt[j % 2]
                k.act(z_[:], pz[:], AF.Silu)
                k.dma("pool", S["zs"][c0:c0 + 128, :], z_[:])
                k.copy(v_[:], pv[:, 0:128], eng="dve")
                k.dma("pool", S["v"][c0:c0 + 128, :], v_[:])
                k.tt(d_[:], pv[:, 128:144], self.rowp[:, R_DTB:R_DTB + 16], ALU.add)
                k.act(d_[:], d_[:], AF.Exp)
                k.act(d_[:], d_[:], AF.Ln, bias=self.oneb[:, 0:1], scale=1.0)
                k.dma("pool", S["dt"][c0:c0 + 128, :], d_[:])
        k.release(m)

    def phase_conv(self, l):
        k, I, S = self.k, self.I, self.S
        vec = self.vec
        m = k.mark()
        self.alloc_psum(6, 0)
        diag = k.sbuf("cv_diag", [128, 6, 5, 128], BF16)
        for c in range(6):
            for j in range(5):
                k.ts(diag[:, c, j, :], self.cF(C_ID), vec[:, V_CONVW + c * 5 + j:V_CONVW + c * 5 + j + 1], ALU.mult,
                     eng="dve")
        cbrow = k.sbuf("cv_cbrow", [128, 768], BF16)
        k.copy(cbrow[:], self.rowp[:, R_CONVB:R_CONVB + 768])
        xins = [k.sbuf("cv_xin%d" % i, [128, 6, 516], BF16) for i in range(2)]
        ofm = [k.sbuf("cv_ofm%d" % i, [128, 512], BF16) for i in range(2)]
        oxs = [k.sbuf("cv_oxs%d" % i, [128, 512], BF16) for i in range(2)]
        obt = [k.sbuf("cv_obt%d" % i, [128, 128], BF16) for i in range(2)]
        xv = S["xbcT"].rearrange("(c p) t -> p c t", p=128)
        cnt = 0
        for ti, (t0, n, s) in enumerate(TILES):
            xin = xins[ti % 2]
            s0, s1 = (0, CTX) if s == 1 else (CTX, T)
            lo, hi = t0 - 2, t0 + n + 2
            clo, chi = max(lo, s0), min(hi, s1)
            if lo < s0:
                k.memset(xin[:, :, 0:2], 0.0)
            if hi > s1:
                k.memset(xin[:, :, n + 2:n + 4], 0.0)
            k.dma("sp", xin[:, :, clo - lo:chi - lo], xv[:, :, clo:chi])
            for c in (4, 5):
                ps = self.pf[cnt % 2]; o = ofm[cnt % 2]; cnt += 1
                for j in range(5):
                    k.mm(ps[:, 0:n], diag[:, c, j, :], xin[:, c, j:j + n], start=(j == 0), stop=(j == 4))
                k.act(o[:, 0:n], ps[:, 0:n], AF.Silu, bias=vec[:, V_CONVB + c:V_CONVB + c + 1], scale=1.0)
                k.dma("pool", (S["BT"] if c == 4 else S["CT"])[:, t0:t0 + n], o[:, 0:n])
            for jt in range(n // 128):
                c0 = t0 + jt * 128
                pa = self.pf[2 + jt % 2]; pb = self.pf[4 + jt % 2]
                for c in range(5):
                    dstp = pa[:, c * 128:(c + 1) * 128] if c < 4 else pb[:, 0:128]
                    for j in range(5):
                        k.mm(dstp, xin[:, c, jt * 128 + j:jt * 128 + j + 128], diag[:, c, j, :], start=(j == 0), stop=False)
                    k.mm(dstp, self.cB(C_E0), cbrow[:, c * 128:(c + 1) * 128], start=False, stop=True)
                ox = oxs[jt % 2]; ob = obt[jt % 2]
                k.act(ox[:], pa[:], AF.Silu)
                k.dma("pool", S["xs"][c0:c0 + 128, :], ox[:])
                k.act(ob[:], pb[:, 0:128], AF.Silu)
                k.dma("pool", S["Bt"][c0:c0 + 128, :], ob[:])
        k.release(m)

    def rms_to_mixT(self, src, ngrp, gsz, nwoff, row0, c, tg):
        k, S = self.k, self.S
        ssq, rs, gn, mixc, junk = tg
        for g in range(ngrp):
            k.act(junk[:, 0:gsz], src[:, g * gsz:(g + 1) * gsz], AF.Square, accum_out=ssq[:, g:g + 1])
        k.act(rs[:, 0:ngrp], ssq[:, 0:ngrp], AF.Sqrt, bias=self.epsb[:, 0:1], scale=1.0 / gsz)
        k.op("dve", lambda e: e.reciprocal(rs[:, 0:ngrp], rs[:, 0:ngrp]), reads=[rs[:, 0:ngrp]], writes=[rs[:, 0:ngrp]])
        k.tt(gn[:].rearrange("p (g f) -> p g f", g=ngrp), src[:].rearrange("p (g f) -> p g f", g=ngrp),
             bc(rs[:, 0:ngrp], 2, gsz), ALU.mult)
        pt = self.pt[c % len(self.pt)]
        k.tr(pt[:, 512:640], gn[:, 0:128], self.cB(C_ID))
        for kk in range(4):
            k.tr(pt[:, kk * 128:(kk + 1) * 128], gn[:, kk * 128:(kk + 1) * 128], self.cB(C_ID))
        for kk in range(4):
            k.act(mixc[:, kk, :], pt[:, kk * 128:(kk + 1) * 128], AF.Copy, scale=self.vec[:, nwoff + kk:nwoff + kk + 1])
        mv = S["mixT"].rearrange("(k p) t -> p k t", p=128)
        k.dma("pool", mv[:, row0 // 128:row0 // 128 + 4, c * 128:(c + 1) * 128], mixc[:])

    def phase_attn(self, l, last):
        k, I, S = self.k, self.I, self.S
        m = k.mark()
        self.alloc_psum(4, 2)
        Kt = k.sbuf("at_K", [128, 2, T], BF16)
        Qt = k.sbuf("at_Q", [128, 8, T], BF16)
        k.memset(Kt[:], 0.0)
        k.memset(Qt[:], 0.0)
        Va = k.sbuf("at_V", [128, NCH, 2, 65], BF16)
        esk = k.sbuf("at_es", [128, 8], F32)
        for g in range(2):
            k.dma("sp", Kt[0:64, g, :], S["kT"][g * 64:(g + 1) * 64, :])
        for h in range(8):
            k.dma("sp", Qt[0:64, h, :], S["qT"][h * 64:(h + 1) * 64, :])
        k.memset(Va[:], 1.0)
        for c in range(NCH):
            k.dma("sp", Va[:, c, :, 0:64], S["v"][c * 128:(c + 1) * 128, :].rearrange("p (g d) -> p g d", g=2))
        k.act(esk[:], self.rowp[:, R_SINK:R_SINK + 8], AF.Exp)
        ET = [[k.sbuf("at_E%d_%d" % (i, j), [128, 4, 128], BF16) for j in range(5)] for i in range(2)]
        attn = [k.sbuf("at_o%d" % i, [128, 512], F32) for i in range(2)]
        den = k.sbuf("at_den", [128, 4], F32)
        tg = (k.sbuf("at_ssq", [128, 2], F32), k.sbuf("at_rs", [128, 2], F32), k.sbuf("at_gn", [128, 512], BF16),
              k.sbuf("at_mix", [128, 4, 128], BF16), k.sbuf("at_junk", [128, 512], F32))
        blocks = list(range(0 if not last else 2, NCH))
        it = 0
        for c in blocks:
            if c < 2:
                keys = [(0, None), (1, None)]
            else:
                keys = [(0, None), (1, None)]
                if c > 2:
                    keys.append((c - 1, C_U1))
                keys.append((c, None))
                if c < NCH - 1:
                    keys.append((c + 1, C_U0))
            at = attn[c % 2]
            for g in range(2):
                ets = ET[it % 2]
                pv = self.pf[2 + it % 2]
                pvv = pv[:].rearrange("p (r d) -> p r d", r=4)
                for idx, (kc, mk) in enumerate(keys):
                    ps = self.pf[idx % 2]
                    psv = ps[:].rearrange("p (r q) -> p r q", r=4)
                    k.mm(psv, Kt[:, g, kc * 128:(kc + 1) * 128], Qt[:, 4 * g:4 * g + 4, c * 128:(c + 1) * 128])
                    k.act(ets[idx][:], psv, AF.Exp, scale=0.125)
                    if mk is not None:
                        k.tt(ets[idx][:], ets[idx][:], bc(self.cB(mk), 1, 4), ALU.mult)
                for r in range(4):
                    for idx, (kc, mk) in enumerate(keys):
                        k.mm(pvv[:, r, 0:65], ets[idx][:, r, :], Va[:, kc, g, :], start=(idx == 0), stop=(idx == len(keys) - 1))
                k.tt(den[:], pvv[:, :, 64], esk[:, 4 * g:4 * g + 4], ALU.add)
                k.op("dve", lambda e: e.reciprocal(den[:], den[:]), reads=[den[:]], writes=[den[:]])
                k.tt(at[:, g * 256:(g + 1) * 256].rearrange("p (r d) -> p r d", r=4), pvv[:, :, 0:64], bc(den[:], 2, 64), ALU.mult)
                it += 1
            self.rms_to_mixT(at, 1, 512, V_ANW, 0, c, tg)
        k.release(m)

    def phase_ssd(self, l, last):
        k, I, S = self.k, self.I, self.S
        m = k.mark()
        self.alloc_psum(6, 2)
        rowp = self.rowp
        abc = k.sbuf("sd_a", [128, 16], F32)
        k.act(abc[:], rowp[:, R_ALOG:R_ALOG + 16], AF.Exp)
        k.ts(abc[:], abc[:], -1.0, ALU.mult)
        xsb = [k.sbuf("sd_xs%d" % i, [128, 512], BF16) for i in range(2)]
        btb = [k.sbuf("sd_bt%d" % i, [128, 128], BF16) for i in range(2)]
        BTc = [k.sbuf("sd_BT%d" % i, [128, 256], BF16) for i in range(2)]
        CTc = [k.sbuf("sd_CT%d" % i, [128, 256], BF16) for i in range(2)]
        for t_ in BTc + CTc:
            k.memset(t_[:], 0.0)
        dtb = [k.sbuf("sd_dt%d" % i, [128, 16], F32) for i in range(2)]
        yfb = [k.sbuf("sd_yf%d" % i, [128, 512], F32) for i in range(2)]
        zsb = [k.sbuf("sd_zs%d" % i, [128, 512], BF16) for i in range(2)]
        dta = k.sbuf("sd_dta", [128, 8], F32)
        DU = k.sbuf("sd_DU", [128, 1024], BF16)
        E = k.sbuf("sd_E", [128, 1024], BF16)
        GM = k.sbuf("sd_GM", [128, 256], BF16)
        ST = k.sbuf("sd_ST", [128, 1024], BF16)
        xdt = k.sbuf("sd_xdt", [128, 512], BF16)
        xw = k.sbuf("sd_xw", [128, 512], BF16)
        ecs = k.sbuf("sd_ecs", [128, 8], F32)
        dec = k.sbuf("sd_dec", [128, 8], F32)
        tmp = k.sbuf("sd_tmp", [128, 512], F32)
        ys = [k.sbuf("sd_y%d" % i, [128, 512], F32) for i in range(2)]
        H = k.sbuf("sd_H", [128, 256], F32)
        Hbf = k.sbuf("sd_Hbf", [128, 256], BF16)
        gg = k.sbuf("sd_g", [128, 512], F32)
        tg = (k.sbuf("sd_ssq", [128, 2], F32), k.sbuf("sd_rs", [128, 2], F32), k.sbuf("sd_gn", [128, 512], BF16),
              k.sbuf("sd_mix", [128, 4, 128], BF16), k.sbuf("sd_junk", [128, 512], F32))
        pE0, pE1, pG, pY, pYO, pS7 = self.pf
        pX = pG[:, 256:512]

        def v3(ap, h):
            return ap.rearrange("p (h q) -> p h q", h=h)

        def v4(ap):
            return ap.rearrange("p (g r i) -> p g r i", g=2, r=4)
        for d in range(2):
            k.memset(H[:], 0.0)
            k.memset(Hbf[:], 0.0)
            order = list(range(NCH)) if d == 0 else [1, 0] + list(range(NCH - 1, 1, -1))
            cU = C_U0 if d == 0 else C_U1
            cL = C_L0 if d == 0 else C_L1
            iend = 127 if d == 0 else 0
            for it, c in enumerate(order):
                c0 = c * 128
                xs_, bt_, BT_, CT_, dt_ = xsb[it % 2], btb[it % 2], BTc[it % 2], CTc[it % 2], dtb[it % 2]
                k.dma("sp", xs_[:], S["xs"][c0:c0 + 128, :])
                k.dma("sp", bt_[:], S["Bt"][c0:c0 + 128, :])
                for g in range(2):
                    k.dma("sp", BT_[g * 64:(g + 1) * 64, g * 128:(g + 1) * 128], S["BT"][g * 64:(g + 1) * 64, c0:c0 + 128])
                    k.dma("sp", CT_[g * 64:(g + 1) * 64, g * 128:(g + 1) * 128], S["CT"][g * 64:(g + 1) * 64, c0:c0 + 128])
                k.dma("sp", dt_[:], S["dt"][c0:c0 + 128, :])
                if d == 1:
                    yf_, zs_ = yfb[it % 2], zsb[it % 2]
                    k.dma("sp", yf_[:], S["yf"][c0:c0 + 128, :])
                    k.dma("sp", zs_[:], S["zs"][c0:c0 + 128, :])
                dtd = dt_[:, d * 8:(d + 1) * 8]
                k.tt(dta[:], dtd, abc[:, d * 8:(d + 1) * 8], ALU.mult)
                k.tt(v3(DU[:], 8), bc(self.cB(cU), 1, 8), bc(dta[:], 2, 128), ALU.mult)
                k.mm(pE0[:], self.cB(cL), DU[:, 0:512])
                k.mm(pE1[:], self.cB(cL), DU[:, 512:1024])
                k.act(E[:, 0:512], pE0[:], AF.Exp)
                k.act(E[:, 512:1024], pE1[:], AF.Exp)
                k.mm(pX[:, 0:8], self.cF(cU), dta[:])
                k.mm(pX[:, 8:16], self.cF(C_ONE), dta[:])
                k.act(ecs[:], pX[:, 0:8], AF.Exp)
                k.act(dec[:], pX[:, 8:16], AF.Exp)
                for g in range(2):
                    k.mm(pG[:, g * 128:(g + 1) * 128], BT_[:, g * 128:(g + 1) * 128], CT_[:, g * 128:(g + 1) * 128])
                k.tt(v3(GM[:], 2), v3(pG[:, 0:256], 2), bc(self.cB(cU), 1, 2), ALU.mult)
                k.tt(v4(ST[:]), v4(E[:]), bc(v3(GM[:], 2), 2, 4), ALU.mult)
                k.tt(v3(xdt[:], 8), v3(xs_[:], 8), bc(dtd, 2, 64), ALU.mult)
                k.tt(v3(xw[:], 8), v3(xdt[:], 8), bc(v3(E[:], 8)[:, :, iend], 2, 64), ALU.mult)
                for h in range(8):
                    k.mm(pY[:, h * 64:(h + 1) * 64], ST[:, h * 128:(h + 1) * 128], xdt[:, h * 64:(h + 1) * 64])
                for g in range(2):
                    k.mm(pYO[:, g * 256:(g + 1) * 256], CT_[:, g * 128:(g + 1) * 128], Hbf[:, :])
                y = ys[it % 2]
                k.tt(v3(tmp[:], 8), v3(pYO[:], 8), bc(ecs[:], 2, 64), ALU.mult)
                k.tt(y[:], pY[:], tmp[:], ALU.add)
                for g in range(2):
                    k.mm(pS7[:, g * 256:(g + 1) * 256], bt_[:, :], xw[:, g * 256:(g + 1) * 256])
                for g in range(2):
                    gs = slice(g * 64, (g + 1) * 64)
                    k.tt(v3(H[gs, :], 4), v3(H[gs, :], 4), bc(dec[gs, 4 * g:4 * g + 4], 2, 64), ALU.mult)
                    k.tt(H[gs, :], H[gs, :], pS7[gs, g * 256:(g + 1) * 256], ALU.add)
                k.copy(Hbf[:], H[:], eng="act")
                if d == 0:
                    k.dma("pool", S["yf"][c0:c0 + 128, :], y[:])
                else:
                    if "yb" in self.debug:
                        k.dma("pool", S["yb"][c0:c0 + 128, :], y[:])
                    k.tt(y[:], y[:], yf_[:], ALU.add)
                    k.tt(v3(tmp[:], 8), v3(xs_[:], 8), bc(rowp[:, R_DSKIP:R_DSKIP + 8], 2, 64), ALU.mult)
                    k.tt(y[:], y[:], tmp[:], ALU.add)
                    if "dg1" in self.debug:
                        k.dma("pool", S["dg1"][c0:c0 + 128, :], y[:])
                    k.tt(gg[:], y[:], zs_[:], ALU.mult)
                    if "dg1" in self.debug:
                        k.dma("pool", S["dg2"][c0:c0 + 128, :], gg[:])
                    self.rms_to_mixT(gg, 2, 256, V_SNW, 512, c, tg)
                    if "dg1" in self.debug:
                        k.dma("pool", S["dg3"][c0:c0 + 128, :], tg[2][:])
        k.release(m)

    def phase_outproj(self, l, last):
        k, I, S = self.k, self.I, self.S
        m = k.mark()
        self.alloc_psum(6, 2)
        wout = k.sbuf("op_w", [128, 8, D], BF16)
        wv = I["w_out"][l].rearrange("(k p) n -> p k n", p=128)
        for kk in range(8):
            k.dma("pool", wout[:, kk, :], wv[:, kk, :])
        wr = k.sbuf("op_wr", [128, 8, NEXP], F32)
        k.dma("sp", wr[:], I["w_router"][l].rearrange("(k p) e -> p k e", p=128))
        mixs = [k.sbuf("op_mix%d" % i, [128, 8, 512], BF16) for i in range(2)]
        xts = [k.sbuf("op_x%d" % i, [128, 8, 512], F32) for i in range(2)]
        h2f = k.sbuf("op_h2f", [128, 8, 512], F32)
        h2b = k.sbuf("op_h2b", [128, 8, 512], BF16)
        sq = k.sbuf("op_sq", [128, 8, 512], BF16)
        rstd = k.sbuf("op_rstd", [128, 512], F32)
        rows = [k.sbuf("op_rows%d" % i, [128, D], BF16) for i in range(2)]
        negm = k.sbuf("op_negm", [128, 1], F32)
        e4 = k.sbuf("op_e4", [128, 4], F32)
        gsum = k.sbuf("op_gsum", [128, 1], F32)
        maskb = k.sbuf("op_mask", [128, NEXP], BF16)
        xTv = S["xT"].rearrange("(k p) t -> p k t", p=128)
        mTv = S["mixT"].rearrange("(k p) t -> p k t", p=128)
        for ti, (t0, n, s) in enumerate(TILES):
            if last and s == 1:
                continue
            mix = mixs[ti % 2]; xt = xts[ti % 2]
            k.dma("sp", mix[:, :, 0:n], mTv[:, :, t0:t0 + n])
            k.dma("sp", xt[:, :, 0:n], xTv[:, :, t0:t0 + n])
            for mc in range(8):
                ps = self.pf[mc % 2]
                for kk in range(8):
                    k.mm(ps[:, 0:n], wout[:, kk, mc * 128:(mc + 1) * 128], mix[:, kk, 0:n], start=(kk == 0), stop=(kk == 7))
                k.stt(xt[:, mc, 0:n], ps[:, 0:n], self.mod[:, 16 + mc, s:s + 1], xt[:, mc, 0:n], ALU.mult, ALU.add)
            k.dma("pool", xTv[:, :, t0:t0 + n], xt[:, :, 0:n])
            self.norm_mod(xt, n, s, self.A2, 24, h2f, sq, rstd, self.pf[5])
            k.copy(h2b[:, :, 0:n], h2f[:, :, 0:n], eng="act")
            for j in range(n // 128):
                c = (t0 + j * 128) // 128
                pl = self.pf[2 + j % 2]
                for kk in range(8):
                    k.mm(pl[:, 0:NEXP], h2f[:, kk, j * 128:(j + 1) * 128], wr[:, kk, :], start=(kk == 0), stop=(kk == 7))
                lg = self.lgs[:, c, :]
                t8 = self.top8s[:, c, :]
                k.tt(lg, pl[:, 0:NEXP], self.rowp[:, R_BR:R_BR + NEXP], ALU.add)
                k.op("dve", lambda e, t8=t8, lg=lg: e.max(t8, lg), reads=[lg], writes=[t8])
                k.ts(negm[:], t8[:, 0:1], -1.0, ALU.mult)
                k.act(e4[:], t8[:, 0:4], AF.Exp, bias=negm[:, 0:1], scale=1.0, accum_out=gsum[:, 0:1])
                k.op("dve", lambda e: e.reciprocal(gsum[:], gsum[:]), reads=[gsum[:]], writes=[gsum[:]])
                k.ts(self.gates[:, c, :], e4[:], gsum[:, 0:1], ALU.mult)
                k.ts(maskb[:], lg, t8[:, 3:4], ALU.is_ge)
                pr = self.pf[4]
                k.mm(pr[:, 0:NEXP], self.cB(C_L1), maskb[:])
                k.mm(pr[:, NEXP:2 * NEXP], self.cB(C_ONE), maskb[:])
                k.tt(self.rks[:, c, :], pr[:, 0:NEXP], self.base[:], ALU.add)
                k.tt(self.base[:], self.base[:], pr[:, NEXP:2 * NEXP], ALU.add)
                pt = self.pt[j % 2]; rw = rows[j % 2]
                for kk in range(8):
                    k.tr(pt[:, kk * 128:(kk + 1) * 128], h2b[:, kk, j * 128:(j + 1) * 128], self.cB(C_ID))
                k.copy(rw[:, 0:512], pt[:, 0:512], eng="act")
                k.copy(rw[:, 512:1024], pt[:, 512:1024], eng="dve")
                k.dma("pool", S["h2"][c * 128:(c + 1) * 128, :], rw[:])
        k.release(m)

    def phase_route(self, l, last):
        k, I, S = self.k, self.I, self.S
        m = k.mark()
        chunks = list(range(2 if last else 0, NCH))
        NB = (len(chunks) * 128 * 4) // BLK + NEXP
        NCK = (T + BLK - 1) // BLK
        self.NB = NB
        cm = k.sbuf("rt_cm", [128, NEXP], F32)
        gt = k.sbuf("rt_gt", [128, NEXP], F32)
        pd = k.sbuf("rt_pd", [128, NEXP], F32)
        ca = k.sbuf("rt_ca", [128, NEXP], F32)
        cbb = k.sbuf("rt_cb", [128, NEXP], F32)
        pstart = k.sbuf("rt_ps", [128, NEXP], F32)
        thr = k.sbuf("rt_thr", [128, 256], F32)
        k.ts(thr[:], self.iot[:, 0:256], float(BLK), ALU.mult)
        cmp2 = k.sbuf("rt_cmp2", [128, NEXP, NCK], F32)
        k.tt(cmp2[:], bc(self.base[:], 2, NCK), bc(thr[:, 0:NCK], 1, NEXP), ALU.is_gt)
        k.op("dve", lambda e: e.reduce_sum(pd[:], cmp2[:], AX.X), reads=[cmp2[:]], writes=[pd[:]])
        k.ts(pd[:], pd[:], float(BLK), ALU.mult)
        a, b = ca, cbb
        k.copy(a[:], pd[:])
        for sft in (1, 2, 4, 8, 16):
            k.copy(b[:, 0:sft], a[:, 0:sft])
            k.tt(b[:, sft:NEXP], a[:, sft:NEXP], a[:, 0:NEXP - sft], ALU.add)
            a, b = b, a
        pend = a
        k.tt(pstart[:], pend[:], pd[:], ALU.subtract)
        BIG = float(2 ** 30)
        cmp_ = k.sbuf("rt_cmp", [128, NB, NEXP], F32)
        blkf = k.sbuf("rt_blkf", [128, NB], F32)
        k.tt(cmp_[:], bc(pend[:, :], 1, NB), bc(thr[:, 0:NB], 2, NEXP), ALU.is_le)
        k.op("dve", lambda e: e.reduce_sum(blkf[:, 0:NB], cmp_[:], AX.X), reads=[cmp_[:]], writes=[blkf[:, 0:NB]])
        k.ts(blkf[:], blkf[:], float(NEXP - 1), ALU.min)
        k.copy(self.blkf[:, 0:NB], blkf[:, 0:NB])
        k.memset(self.flg[:, 0:1], 1.0)
        k.tt(self.flg[:, 1:NB], blkf[:, 1:NB], blkf[:, 0:NB - 1], ALU.not_equal)
        k.ts(self.nfl[:, 0:NB], self.flg[:, 0:NB], -BIG, ALU.mult, BIG, ALU.add)
        Dm = k.sbuf("rt_D", [128, NEXP], F32)
        oh = k.sbuf("rt_oh", [128, NEXP], F32)
        dstf = k.sbuf("rt_dstf", [128, 4], F32)
        rows = [k.sbuf("rt_rows%d" % i, [128, D], BF16) for i in range(2)]
        ixs = [k.sbuf("rt_ix%d" % i, [128, 1], I32) for i in range(8)]
        for ci, c in enumerate(chunks):
            rw = rows[ci % 2]
            k.dma("sp", rw[:], S["h2"][c * 128:(c + 1) * 128, :])
            k.tt(Dm[:], self.rks[:, c, :], pstart[:], ALU.add)
            for kq in range(4):
                k.ts(oh[:], self.lgs[:, c, :], self.top8s[:, c, kq:kq + 1], ALU.is_equal)
                k.tt(oh[:], oh[:], Dm[:], ALU.mult)
                k.op("dve", lambda e, kq=kq: e.reduce_sum(dstf[:, kq:kq + 1], oh[:], AX.X), reads=[oh[:]], writes=[dstf[:, kq:kq + 1]])
            k.copy(self.dsts[:, c, :], dstf[:])
            if "d_dsts" in self.debug:
                continue
            for kq in range(4):
                ixt = ixs[(ci * 4 + kq) % 8]
                k.copy(ixt[:], dstf[:, kq:kq + 1])
                ix = ixt[:, 0:1]
                k.dma_custom("pool", lambda e, ix=ix, rw=rw: e.indirect_dma_start(
                    out=S["buf"], out_offset=bass.IndirectOffsetOnAxis(ap=ix, axis=0), in_=rw[:], in_offset=None),
                    reads=[ix, rw[:]], writes=[S["buf"]])
        if "d_dsts" in self.debug:
            k.dma("sp", S["d_dsts"], self.dsts[:].rearrange("p c q -> p (c q)"))
            k.dma("sp", S["d_blkf"][:, 0:NB], self.blkf[:, 0:NB])
            k.dma("sp", S["d_base"], self.base[:])
            k.dma("sp", S["d_gates"], self.gates[:].rearrange("p c q -> p (c q)"))
            k.dma("sp", S["d_pend"], pend[:])
        k.release(m)

    def phase_moe(self, l, last):
        k, I, S = self.k, self.I, self.S
        m = k.mark()
        self.alloc_psum(6, 2)
        NB = self.NB
        wgu = k.sbuf("me_wgu", [128, 8, 2 * D], BF16)
        wd = k.sbuf("me_wd", [128, 8, D], BF16)
        bgu = k.sbuf("me_bgu", [128, 2 * D], BF16)
        bd = k.sbuf("me_bd", [128, D], BF16)
        rows = [k.sbuf("me_rows%d" % i, [128, D], BF16) for i in range(2)]
        rT = [k.sbuf("me_rT%d" % i, [128, 8, 128], BF16) for i in range(2)]
        gsb = k.sbuf("me_g", [128, D], F32)
        tsb = k.sbuf("me_t", [128, D], F32)
        lsb = k.sbuf("me_l", [128, D], F32)
        actb = k.sbuf("me_act", [128, D], BF16)
        aT = k.sbuf("me_aT", [128, 8, 128], BF16)
        osb = [k.sbuf("me_o%d" % i, [128, D], BF16) for i in range(2)]
        ones_row = self.cb[0:1, C_ONE:C_ONE + 128]
        wg2 = I["w_gate_up"]
        wd2 = I["w_down"]
        bg2 = I["b_gate_up"]
        bd2 = I["b_down"]
        c8 = k.sbuf("me_c8", [128, 8], F32)
        idf = k.sbuf("me_idf", [128, 9], F32)
        ixw = [k.sbuf("me_ixw%d" % i, [128, 1], I32) for i in range(18)]
        e1k = k.sbuf("me_e1k", [128, 1], F32)
        k.ts(idf[:, 0:1], self.iot[:, 256:257], 8.0, ALU.mult, float(l * NEXP * 1024), ALU.add)
        k.ts(c8[:], self.iot[:, 0:8], idf[:, 0:1], ALU.add)
        st = self.pool_regs

        def gath(e, out, src, ix, big):
            if "rw" not in st:
                st["rw"] = e.alloc_register("bnd_w")
                st["rb"] = e.alloc_register("bnd_b")
                e.reg_mov(st["rw"], DEPTH * NEXP * D - 1)
                e.reg_mov(st["rb"], DEPTH * NEXP - 1)
            return e.indirect_dma_start(out=out, out_offset=None, in_=src, in_offset=bass.IndirectOffsetOnAxis(ap=ix, axis=0),
                                        bounds_check=(st["rw"] if big else st["rb"]), oob_is_err=False)
        for b in range(NB):
            k.ts(e1k[:], self.blkf[:, b:b + 1], 1024.0, ALU.mult)
            k.ts(idf[:, 0:8], c8[:], e1k[:, 0:1], ALU.add)
            k.ts(idf[:, 8:9], self.blkf[:, b:b + 1], float(l * NEXP), ALU.add)
            k.ts(idf[:], idf[:], self.flg[:, b:b + 1], ALU.mult, self.nfl[:, b:b + 1], ALU.add)
            sl = (b % 2) * 9
            for j in range(9):
                k.copy(ixw[sl + j][:], idf[:, j:j + 1])
            bix = ixw[sl + 8][:, 0:1]
            k.dma_custom("pool", lambda e, bix=bix: gath(e, bgu[:], bg2, bix, False), reads=[bix, I["b_gate_up"]], writes=[bgu[:]])
            k.dma_custom("pool", lambda e, bix=bix: gath(e, bd[:], bd2, bix, False), reads=[bix, I["b_down"]], writes=[bd[:]])
            for kk in range(8):
                wix = ixw[sl + kk][:, 0:1]
                k.dma_custom("pool", lambda e, wix=wix, kk=kk: gath(e, wgu[:, kk, :], wg2, wix, True),
                             reads=[wix, I["w_gate_up"]], writes=[wgu[:, kk, :]])
            for kk in range(8):
                wix = ixw[sl + kk][:, 0:1]
                k.dma_custom("pool", lambda e, wix=wix, kk=kk: gath(e, wd[:, kk, :], wd2, wix, True),
                             reads=[wix, I["w_down"]], writes=[wd[:, kk, :]])
            for sub in range(BLK // 128):
                bb = b * (BLK // 128) + sub
                rw = rows[bb % 2]; rt = rT[bb % 2]
                k.dma("sp", rw[:], S["buf"][bb * 128:(bb + 1) * 128, :])
                pt = self.pt[0]
                for kk in range(8):
                    k.tr(pt[:, kk * 128:(kk + 1) * 128], rw[:].rearrange("p (c k) -> p k c", k=8)[:, kk, :], self.cB(C_ID))
                k.copy(rt[:, 0:4, :], pt[:, 0:512].rearrange("p (a t) -> p a t", a=4), eng="act")
                k.copy(rt[:, 4:8, :], pt[:, 512:1024].rearrange("p (a t) -> p a t", a=4), eng="dve")
                for nq in range(4):
                    ps = self.pf[nq]
                    for kk in range(8):
                        k.mm(ps[:], rt[:, kk, :], wgu[:, kk, nq * 512:(nq + 1) * 512], start=(kk == 0), stop=False)
                    k.mm(ps[:], self.cB(C_E0), bgu[:, nq * 512:(nq + 1) * 512], start=False, stop=True)
                for h in range(2):
                    k.ts(gsb[:, h * 512:(h + 1) * 512], self.pf[h][:], 7.0, ALU.min)
                    k.ts(lsb[:, h * 512:(h + 1) * 512], self.pf[2 + h][:], 7.0, ALU.min, -7.0, ALU.max)
                k.act(tsb[:], gsb[:], AF.Silu, scale=ALPHA)
                k.act(lsb[:], lsb[:], AF.Identity, bias=self.ialb[:, 0:1], scale=1.0 / ALPHA)
                k.tt(actb[:], tsb[:], lsb[:], ALU.mult)
                pt2 = self.pt[1]
                for kk in range(8):
                    k.tr(pt2[:, kk * 128:(kk + 1) * 128], actb[:].rearrange("p (c k) -> p k c", k=8)[:, kk, :], self.cB(C_ID))
                k.copy(aT[:, 0:4, :], pt2[:, 0:512].rearrange("p (a t) -> p a t", a=4), eng="act")
                k.copy(aT[:, 4:8, :], pt2[:, 512:1024].rearrange("p (a t) -> p a t", a=4), eng="dve")
                o = osb[bb % 2]
                for nq in range(2):
                    ps = self.pf[4 + nq]
                    for kk in range(8):
                        k.mm(ps[:], aT[:, kk, :], wd[:, kk, nq * 512:(nq + 1) * 512], start=(kk == 0), stop=False)
                    k.mm(ps[:], self.cB(C_E0), bd[:, nq * 512:(nq + 1) * 512], start=False, stop=True)
                    k.copy(o[:, nq * 512:(nq + 1) * 512], ps[:], eng=("act" if nq == 0 else "dve"))
                k.dma("sp", S["obuf"][bb * 128:(bb + 1) * 128, :], o[:])
        k.release(m)

    def phase_combine(self, l, last):
        k, I, S = self.k, self.I, self.S
        m = k.mark()
        self.alloc_psum(0, 2)
        chunks = list(range(2 if last else 0, NCH))
        gk = [[k.sbuf("cb_g%d_%d" % (i, j), [128, D], BF16) for j in range(4)] for i in range(2)]
        cixs = [k.sbuf("cb_ix%d" % i, [128, 1], I32) for i in range(8)]
        f = k.sbuf("cb_f", [128, D], F32)
        fb = k.sbuf("cb_fb", [128, D], BF16)
        xts = [k.sbuf("cb_x%d" % i, [128, 8, 128], F32) for i in range(2)]
        tmp = k.sbuf("cb_tmp", [128, 8, 128], F32)
        xTv = S["xT"].rearrange("(k p) t -> p k t", p=128)
        for ci, c in enumerate(chunks):
            s = 1 if c < 2 else 0
            g4 = gk[ci % 2]
            xt = xts[ci % 2]
            k.dma("sp", xt[:], xTv[:, :, c * 128:(c + 1) * 128])
            for kq in range(4):
                ixt = cixs[(ci * 4 + kq) % 8]
                k.copy(ixt[:], self.dsts[:, c, kq:kq + 1])
                ix = ixt[:, 0:1]
                gt = g4[kq]
                k.dma_custom("pool", lambda e, ix=ix, gt=gt: e.indirect_dma_start(
                    out=gt[:], out_offset=None, in_=S["obuf"], in_offset=bass.IndirectOffsetOnAxis(ap=ix, axis=0)),
                    reads=[ix, S["obuf"]], writes=[gt[:]])
            k.ts(f[:], g4[0][:], self.gates[:, c, 0:1], ALU.mult)
            for kq in range(1, 4):
                k.stt(f[:] if kq < 3 else fb[:], g4[kq][:], self.gates[:, c, kq:kq + 1], f[:], ALU.mult, ALU.add)
            pt = self.pt[ci % 2]
            for kk in range(8):
                k.tr(pt[:, kk * 128:(kk + 1) * 128], fb[:, kk * 128:(kk + 1) * 128], self.cB(C_ID))
            k.tt(tmp[:], pt[:].rearrange("p (a t) -> p a t", a=8), bc(self.mod[:, 40:48, s], 2, 128), ALU.mult)
            k.tt(xt[:], xt[:], tmp[:], ALU.add)
            k.dma("sp", xTv[:, :, c * 128:(c + 1) * 128], xt[:])
        k.release(m)

    def phase_final(self):
        k, I, S = self.k, self.I, self.S
        m = k.mark()
        self.alloc_psum(4, 0)
        xts = [k.sbuf("fn_x%d" % i, [128, 8, 128], F32) for i in range(2)]
        sq = k.sbuf("fn_sq", [128, 8, 128], BF16)
        rstd = k.sbuf("fn_rstd", [128, 128], F32)
        xn = k.sbuf("fn_xn", [128, 8, 128], F32)
        ob = [k.sbuf("fn_o%d" % i, [128, D], F32) for i in range(2)]
        xTv = S["xT"].rearrange("(k p) t -> p k t", p=128)
        for c in range(2, NCH):
            xt = xts[c % 2]
            k.dma("sp", xt[:], xTv[:, :, c * 128:(c + 1) * 128])
            k.act(sq[:], xt[:], AF.Square)
            ps = self.pf[0]
            for kk in range(8):
                k.mm(ps[:, 0:128], self.cB(C_ONE), sq[:, kk, :], start=(kk == 0), stop=(kk == 7))
            k.act(rstd[:], ps[:, 0:128], AF.Sqrt, bias=self.epsb[:, 0:1], scale=1.0 / D)
            k.op("dve", lambda e: e.reciprocal(rstd[:], rstd[:]), reads=[rstd[:]], writes=[rstd[:]])
            for kk in range(8):
                k.stt(xn[:, kk, :], xt[:, kk, :], self.gv[:, kk:kk + 1], rstd[:], ALU.mult, ALU.mult,
                      eng="dve")
            o = ob[c % 2]
            for h in range(2):
                p = self.pf[1 + (c % 2) * 0 + h]
                for j in range(4):
                    kk = h * 4 + j
                    k.mm(p[:, j * 128:(j + 1) * 128], xn[:, kk, :], self.cF(C_ID))
                k.copy(o[:, h * 512:(h + 1) * 512], p[:], eng=("act" if h else "dve"))
            k.dma("pool", self.out[(c - 2) * 128:(c - 1) * 128, :], o[:])
        k.release(m)

    def build(self):
        k, I = self.k, self.I
        self.vec = k.sbuf("vec_sb", [128, NVEC], F32)
        self.rowp = k.sbuf("rowp_sb", [128, NROW], F32)
        self.mod = k.sbuf("mod", [128, 48, 2], F32)
        self.A1 = k.sbuf("A1", [128, 8, 2], F32)
        self.A2 = k.sbuf("A2", [128, 8, 2], F32)
        self.epsb = k.sbuf("epsb", [128, 1], F32)
        self.oneb = k.sbuf("oneb", [128, 1], F32)
        self.nm_tmp = [k.sbuf("nmtmp%d" % i, [128, 512], F32) for i in range(2)]
        self.iot = k.sbuf("iot_sb", [128, 257], F32)
        self.lgs = k.sbuf("lgs", [128, NCH, NEXP], F32)
        self.top8s = k.sbuf("top8s", [128, NCH, 8], F32)
        self.rks = k.sbuf("rks", [128, NCH, NEXP], F32)
        self.gates = k.sbuf("gates", [128, NCH, 4], F32)
        self.dsts = k.sbuf("dsts", [128, NCH, 4], I32)
        self.base = k.sbuf("base", [128, NEXP], F32)
        self.blkf = k.sbuf("blkf", [128, NBLK], F32)
        self.flg = k.sbuf("flg", [128, NBLK], F32)
        self.nfl = k.sbuf("nfl", [128, NBLK], F32)
        k.memset(self.epsb[:], EPS)
        k.memset(self.oneb[:], 1.0)
        self.ialb = k.sbuf("ialb", [128, 1], F32)
        k.memset(self.ialb[:], 1.0 / ALPHA)
        k.dma("sp", self.iot[:], I["iot"])
        stop = self.stop_after
        seq = [("x0", None)]
        for l in range(self.nlayers):
            for ph in ("ada", "inproj", "conv", "attn", "ssd", "outproj", "route", "moe", "combine"):
                seq.append((ph, l))
        seq.append(("final", None))
        for ph, l in seq:
            last = (l == DEPTH - 1)
            if ph == "x0":
                self.phase_x0()
            elif ph == "final":
                self.phase_final()
            elif ph == "ada":
                k.dma("sp", self.vec[:], I["vec"][l])
                k.dma("sp", self.rowp[:], I["rowp"][l])
                k.memset(self.base[:], 0.0)
                self.phase_ada(l)
            elif ph in ("inproj", "conv", "ada"):
                getattr(self, "phase_" + ph)(l)
            else:
                getattr(self, "phase_" + ph)(l, last)
            if stop is not None and (ph, l) == tuple(stop):
                break
        k.barrier()
        k.wait_all("sp")
        k.emit()
        k.close()
        return self.nc


def _host_consts():
    idn = np.eye(128, dtype=np.float32)
    perm = np.zeros((128, 128), np.float32)
    for p in range(128):
        perm[p, p + 16 if (p % 32) < 16 else p - 16] = 1.0
    kk = np.arange(128)
    U0 = (kk[:, None] <= kk[None, :]).astype(np.float32)
    U1 = (kk[:, None] >= kk[None, :]).astype(np.float32)
    L0 = (kk[:, None] > kk[None, :]).astype(np.float32)
    L1 = (kk[:, None] < kk[None, :]).astype(np.float32)
    ones = np.ones((128, 128), np.float32)
    e0 = np.zeros((128, 128), np.float32)
    e0[0, :] = 1.0
    cst = np.concatenate([idn, perm, U0, U1, L0, L1, ones, e0], axis=1)
    t = np.arange(S_LAT)
    pos = np.stack([t // 64, t % 64], axis=0).astype(np.float64)
    inv = 10000.0 ** (-np.arange(16, dtype=np.float64) / 16.0)
    rope = np.zeros((128, 2, S_LAT), np.float32)
    for p in range(128):
        d = p % 64
        a, r, f = d // 32, (d % 32) // 16, d % 16
        ang = pos[a] * inv[f]
        rope[p, 0] = np.cos(ang)
        rope[p, 1] = np.sin(ang) * (-1.0 if r == 0 else 1.0)
    iot = np.zeros((128, 257), np.float32)
    iot[:, 0:256] = np.arange(256, dtype=np.float32)[None, :]
    iot[:, 256] = np.arange(128, dtype=np.float32)
    return cst, rope, iot


def _fm(v, kcols):
    return np.ascontiguousarray(np.asarray(v, np.float32).reshape(kcols, 128).T)


def _host_layout(inputs):
    vec = np.zeros((DEPTH, 128, NVEC), np.float32)
    rowp = np.zeros((DEPTH, 128, NROW), np.float32)
    for l in range(DEPTH):
        vec[l, :, V_NMW:V_NMW + 8] = _fm(inputs["norm_mix_w"][l], 8)
        vec[l, :, V_NFW:V_NFW + 8] = _fm(inputs["norm_ffn_w"][l], 8)
        vec[l, :, V_BADA:V_BADA + 48] = _fm(inputs["b_ada"][l], 48)
        vec[l, :, V_CONVB:V_CONVB + 6] = _fm(inputs["conv_b"][l], 6)
        cw = np.asarray(inputs["conv_w"][l], np.float32)
        vec[l, :, V_CONVW:V_CONVW + 30] = cw.reshape(5, 6, 128).transpose(2, 1, 0).reshape(128, 30)
        vec[l, :, V_SNW:V_SNW + 4] = _fm(inputs["ssm_norm_w"][l], 4)
        vec[l, :, V_ANW:V_ANW + 4] = _fm(inputs["attn_norm_w"][l], 4)
        row = np.concatenate([np.asarray(inputs["dt_bias"][l], np.float32).reshape(16),
                              np.asarray(inputs["a_log"][l], np.float32).reshape(16),
                              np.asarray(inputs["d_skip"][l], np.float32).reshape(8),
                              np.asarray(inputs["attn_sinks"][l], np.float32).reshape(8),
                              np.asarray(inputs["b_router"][l], np.float32).reshape(32),
                              np.asarray(inputs["conv_b"][l], np.float32).reshape(768)])
        rowp[l] = np.broadcast_to(row[None, :], (128, NROW))
    return vec, rowp


_CACHE = {}


def kernel(**inputs):
    dbg = inputs.pop("_debug", None)
    inputs = {k_: np.asarray(v) for k_, v in inputs.items()}
    debug = tuple(dbg["dump"]) if dbg is not None else ()
    stop = dbg["stop"] if dbg is not None else None
    ncores = int(dbg.get("ncores", 8)) if dbg is not None else 8
    prog = Prog(debug=debug, stop_after=stop)
    nc = prog.build()
    cst, rope, iot = _host_consts()
    vec, rowp = _host_layout(inputs)
    f32 = lambda a: np.ascontiguousarray(np.asarray(a, np.float32))
    shared = {"w_ada": f32(inputs["w_ada"]), "w_in": f32(inputs["w_in"]), "w_out": f32(inputs["w_out"]),
              "w_router": f32(inputs["w_router"]), "w_gate_up": f32(inputs["w_gate_up"]).reshape(DEPTH * NEXP * D, 2 * D),
              "b_gate_up": f32(inputs["b_gate_up"]).reshape(DEPTH * NEXP, 2 * D),
              "w_down": f32(inputs["w_down"]).reshape(DEPTH * NEXP * D, D), "b_down": f32(inputs["b_down"]).reshape(DEPTH * NEXP, D),
              "vec": vec, "rowp": rowp, "cst": cst, "rope": rope, "iot": iot}
    in_maps = []
    for b in range(ncores):
        gvec = np.concatenate([_fm(inputs["final_norm_w"], 8), _fm(inputs["c"][b], 8), _fm(inputs["c_ctx"], 8)], axis=1)
        mp = dict(shared)
        mp["x"] = f32(inputs["x"][b]); mp["ctx"] = f32(inputs["ctx"][b]); mp["gvec"] = np.ascontiguousarray(gvec)
        in_maps.append(mp)
    res = run_bass_kernel_spmd(nc, in_maps, core_ids=list(range(ncores)))
    if dbg is not None:
        dbg["results"] = res.results
    out = np.stack([np.asarray(r["out"], np.float32) for r in res.results], axis=0)
    return out
```
